# Optimizing a Trainium2 kernel written in Bass

```python
import math
import jax, jax.numpy as jnp
from jax import lax
import numpy as np

D_MODEL = 2048
BATCH = 4
SEQ = 8192
DEPTH = 1

GRID_W = 64
CTX_LEN = 256
D_MIX = D_MODEL
W_LRU = D_MIX // 2
W_HY = D_MIX - W_LRU
IN_COLS = 2 * W_LRU + 3 * W_HY
LRU_BLOCKS = 8
LRU_BW = W_LRU // LRU_BLOCKS
LRU_CONV = 4
LRU_PAD = (2, 1)
LRU_C = 8.0
HY_ORDER = 2
HY_CONV = 3
HY_PAD = (1, 1)
HY_BANDS = 16
HY_EMB = 1 + 2 * HY_BANDS
HY_FH = 64
HY_TARGET = 1e-2
HY_FAST = 0.3
HY_SLOW = 1.5
HY_FILTER_STD = 0.02
N_EXPERTS = 32
TOP_K = 4
D_FF = D_MODEL
SWIGLU_LIMIT = 7.0
SWIGLU_ALPHA = 1.702
MOE_BLOCK = 256
N_MOD = 6
EPS = 1e-6

kernel_name = 'hybrid_rglru_hyena_moe_dit'

F32 = jnp.float32


def rmsnorm(x, g):
    xf = x.astype(F32)
    y = xf * lax.rsqrt(jnp.mean(xf * xf, axis=-1, keepdims=True) + EPS)
    return (y * g.astype(F32)).astype(x.dtype)


def dwconv(u, w, b, pad):
    y = lax.conv_general_dilated(u, w[:, None, :].astype(u.dtype), window_strides=(1,),
                                 padding=[pad], dimension_numbers=('NWC', 'WIO', 'NWC'),
                                 feature_group_count=u.shape[-1])
    return y + b.astype(u.dtype)


def to_col_major(u, rows):
    b, s, ch = u.shape
    return u.reshape(b, rows, GRID_W, ch).transpose(0, 2, 1, 3).reshape(b, s, ch)


def from_col_major(u, rows):
    b, s, ch = u.shape
    return u.reshape(b, GRID_W, rows, ch).transpose(0, 2, 1, 3).reshape(b, s, ch)


def rglru_coeffs(u, wa, ba, wi, bi, lam):
    bsz, L, w = u.shape
    ub = u.reshape(bsz, L, LRU_BLOCKS, LRU_BW)
    r = jax.nn.sigmoid((jnp.einsum('blnd,nde->blne', ub, wa).reshape(bsz, L, w) + ba).astype(F32))
    i = jax.nn.sigmoid((jnp.einsum('blnd,nde->blne', ub, wi).reshape(bsz, L, w) + bi).astype(F32))
    log_a = -LRU_C * r * jax.nn.softplus(-lam.astype(F32))
    a = jnp.exp(log_a)
    b = jnp.sqrt(-jnp.expm1(2.0 * log_a)) * (i * u.astype(F32))
    return a, b


def _lin_combine(e1, e2):
    a1, b1 = e1
    a2, b2 = e2
    return a1 * a2, a2 * b1 + b2


def linear_scan(a, b, h0, reverse):
    a_cum, b_cum = lax.associative_scan(_lin_combine, (a, b), axis=1, reverse=reverse)
    return a_cum * h0[:, None, :] + b_cum


def rglru_group(r_c, r_x, g_c, g_x, need_ctx, conv_w, conv_b, wa, ba, wi, bi, lam):
    u_c = dwconv(r_c, conv_w, conv_b, LRU_PAD)
    u_x = dwconv(r_x, conv_w, conv_b, LRU_PAD)
    hs_c, hs_x = [], []
    for d, rev in enumerate((False, True)):
        a_c, b_c = rglru_coeffs(u_c, wa[d], ba[d], wi[d], bi[d], lam[d])
        h_c = linear_scan(a_c, b_c, jnp.zeros_like(b_c[:, 0]), rev)
        h_last = h_c[:, 0] if rev else h_c[:, -1]
        a_x, b_x = rglru_coeffs(u_x, wa[d], ba[d], wi[d], bi[d], lam[d])
        hs_x.append(linear_scan(a_x, b_x, h_last, rev))
        hs_c.append(h_c)
    y_x = ((hs_x[0] + hs_x[1]) * jax.nn.gelu(g_x.astype(F32))).astype(r_x.dtype)
    if not need_ctx:
        return None, y_x
    y_c = ((hs_c[0] + hs_c[1]) * jax.nn.gelu(g_c.astype(F32))).astype(r_c.dtype)
    return y_c, y_x


def hyena_filters(L, w1, b1, w2, b2, w3, b3, freq, w4):
    t = jnp.linspace(0.0, 1.0, L, dtype=F32)[:, None]
    w = 2.0 * math.pi * jnp.arange(L, dtype=F32)[:, None] / L
    f = jnp.linspace(1e-4, HY_BANDS - 1, HY_BANDS, dtype=F32)[None, :]
    z = jnp.concatenate([t, jnp.cos(f * w), -jnp.sin(f * w)], axis=-1)
    fr = freq.astype(F32)
    hdn = jnp.sin(fr * (z @ w1.astype(F32) + b1.astype(F32)))
    hdn = jnp.sin(fr * (hdn @ w2.astype(F32) + b2.astype(F32)))
    hdn = jnp.sin(fr * (hdn @ w3.astype(F32) + b3.astype(F32)))
    h = (hdn @ w4.astype(F32)).reshape(L, HY_ORDER, 2, W_HY)
    max_decay = math.log(HY_TARGET) / HY_FAST
    min_decay = math.log(HY_TARGET) / HY_SLOW
    deltas = jnp.abs(jnp.linspace(min_decay, max_decay, W_HY, dtype=F32))
    decay = jnp.exp(-t * deltas[None, :])
    return h * decay[:, None, None, :]


def bidir_long_conv(u, h_fwd, h_bwd, d):
    L = u.shape[1]
    k = jnp.concatenate([h_fwd, jnp.zeros_like(h_fwd[:1]), h_bwd[:0:-1]], axis=0)
    kf = jnp.fft.rfft(k, axis=0)
    uf = jnp.fft.rfft(u.astype(F32), n=2 * L, axis=1)
    y = jnp.fft.irfft(uf * kf[None], n=2 * L, axis=1)[:, :L]
    return (y + u.astype(F32) * d.astype(F32)).astype(u.dtype)


def hyena_group(p, conv_w, conv_b, filt, hy_d):
    u = dwconv(p, conv_w, conv_b, HY_PAD)
    v, x1, x2 = jnp.split(u, 3, axis=-1)
    z = x1 * bidir_long_conv(v, filt[:, 0, 0], filt[:, 0, 1], hy_d[0])
    z = x2 * bidir_long_conv(z, filt[:, 1, 0], filt[:, 1, 1], hy_d[1])
    return z


def token_mixer(a_c, a_x, need_ctx, w_in, lru_conv_w, lru_conv_b, lru_wa, lru_ba, lru_wi, lru_bi,
                lru_lambda, hy_conv_w, hy_conv_b, hy_fparams, hy_d, gn_lru, gn_hy, w_out):
    seq = a_x.shape[1]
    rows = seq // GRID_W
    p_c = a_c @ w_in
    p_x = a_x @ w_in
    lru_c, lru_x = rglru_group(p_c[..., :W_LRU], p_x[..., :W_LRU],
                               p_c[..., W_LRU:2 * W_LRU], p_x[..., W_LRU:2 * W_LRU], need_ctx,
                               lru_conv_w, lru_conv_b, lru_wa, lru_ba, lru_wi, lru_bi, lru_lambda)
    filt_x = hyena_filters(seq, *hy_fparams)
    hy_x = from_col_major(hyena_group(to_col_major(p_x[..., 2 * W_LRU:], rows), hy_conv_w, hy_conv_b,
                                      filt_x, hy_d), rows)
    out_x = jnp.concatenate([rmsnorm(lru_x, gn_lru), rmsnorm(hy_x, gn_hy)], axis=-1) @ w_out
    if not need_ctx:
        return None, out_x
    filt_c = hyena_filters(a_c.shape[1], *hy_fparams)
    hy_c = hyena_group(p_c[..., 2 * W_LRU:], hy_conv_w, hy_conv_b, filt_c, hy_d)
    out_c = jnp.concatenate([rmsnorm(lru_c, gn_lru), rmsnorm(hy_c, gn_hy)], axis=-1) @ w_out
    return out_c, out_x


def moe_ffn(h, router_w, router_b, w_gu, b_gu, w_down, b_down):
    n_tok = h.shape[0]
    n_assign = n_tok * TOP_K
    n_blocks = -(-n_assign // MOE_BLOCK) + N_EXPERTS
    cap = n_blocks * MOE_BLOCK
    logits = (h @ router_w + router_b).astype(F32)
    top_val, top_idx = lax.top_k(logits, TOP_K)
    gate = jax.nn.softmax(top_val, axis=-1)
    e_flat = top_idx.reshape(-1)
    tok_flat = jnp.arange(n_assign, dtype=jnp.int32) // TOP_K
    order = jnp.argsort(e_flat)
    e_sorted = e_flat[order]
    counts = jnp.bincount(e_flat, length=N_EXPERTS)
    starts = jnp.cumsum(counts) - counts
    padded = (counts + MOE_BLOCK - 1) // MOE_BLOCK * MOE_BLOCK
    pends = jnp.cumsum(padded)
    pstarts = pends - padded
    dest = pstarts[e_sorted] + jnp.arange(n_assign, dtype=jnp.int32) - starts[e_sorted]
    slot_tok = jnp.zeros((cap,), jnp.int32).at[dest].set(tok_flat[order])
    slot_gate = jnp.zeros((cap,), F32).at[dest].set(gate.reshape(-1)[order])
    block_start = jnp.arange(n_blocks, dtype=jnp.int32) * MOE_BLOCK
    block_expert = jnp.minimum(jnp.searchsorted(pends, block_start, side='right'), N_EXPERTS - 1)

    def run_block(args):
        tok, e = args
        xb = h[tok]
        gu = xb @ w_gu[e] + b_gu[e]
        x_glu = jnp.minimum(gu[:, :D_FF], SWIGLU_LIMIT)
        x_lin = jnp.clip(gu[:, D_FF:], -SWIGLU_LIMIT, SWIGLU_LIMIT)
        act = x_glu * jax.nn.sigmoid(SWIGLU_ALPHA * x_glu) * (x_lin + 1)
        return act @ w_down[e] + b_down[e]

    y = lax.map(run_block, (slot_tok.reshape(n_blocks, MOE_BLOCK), block_expert))
    y = y.reshape(cap, -1) * slot_gate[:, None].astype(y.dtype)
    return jax.ops.segment_sum(y, slot_tok, num_segments=n_tok)


def setup_inputs(seed: int = 0) -> dict:
    key = jax.random.key(seed)
    ks = iter(jax.random.split(key, 40))

    def nrm(shape, std):
        return jax.random.normal(next(ks), shape, F32) * std

    def gain(shape):
        return 1.0 + nrm(shape, 0.05)

    L_ = DEPTH
    a0 = jax.random.uniform(next(ks), (L_, 2, W_LRU), F32, 0.9, 0.999)
    s0 = a0 ** (1.0 / LRU_C)
    lam = jnp.log(s0) - jnp.log1p(-s0)
    return {
        'x': nrm((BATCH, SEQ, D_MODEL), 1.0),
        'c': nrm((BATCH, D_MODEL), 1.0),
        'ctx': nrm((BATCH, CTX_LEN, D_MODEL), 1.0),
        'c_ctx': nrm((D_MODEL,), 1.0),
        'w_mod': nrm((L_, D_MODEL, N_MOD * D_MODEL), 0.5 * D_MODEL ** -0.5),
        'b_mod': nrm((L_, N_MOD * D_MODEL), 0.01),
        'norm1_g': gain((L_, D_MODEL)),
        'norm2_g': gain((L_, D_MODEL)),
        'w_in': nrm((L_, D_MODEL, IN_COLS), D_MODEL ** -0.5),
        'lru_conv_w': nrm((L_, LRU_CONV, W_LRU), LRU_CONV ** -0.5),
        'lru_conv_b': nrm((L_, W_LRU), 0.01),
        'lru_wa': nrm((L_, 2, LRU_BLOCKS, LRU_BW, LRU_BW), LRU_BW ** -0.5),
        'lru_ba': nrm((L_, 2, W_LRU), 0.01),
        'lru_wi': nrm((L_, 2, LRU_BLOCKS, LRU_BW, LRU_BW), LRU_BW ** -0.5),
        'lru_bi': nrm((L_, 2, W_LRU), 0.01),
        'lru_lambda': lam,
        'hy_conv_w': nrm((L_, HY_CONV, 3 * W_HY), HY_CONV ** -0.5),
        'hy_conv_b': nrm((L_, 3 * W_HY), 0.01),
        'hy_w1': nrm((L_, HY_EMB, HY_FH), HY_EMB ** -0.5),
        'hy_b1': nrm((L_, HY_FH), 0.1),
        'hy_w2': nrm((L_, HY_FH, HY_FH), HY_FH ** -0.5),
        'hy_b2': nrm((L_, HY_FH), 0.1),
        'hy_w3': nrm((L_, HY_FH, HY_FH), HY_FH ** -0.5),
        'hy_b3': nrm((L_, HY_FH), 0.1),
        'hy_freq': gain((L_, HY_FH)),
        'hy_w4': nrm((L_, HY_FH, HY_ORDER * 2 * W_HY), HY_FILTER_STD * (2.0 / HY_FH) ** 0.5),
        'hy_d': nrm((L_, HY_ORDER, W_HY), 0.5),
        'gn_lru': gain((L_, W_LRU)),
        'gn_hy': gain((L_, W_HY)),
        'w_out': nrm((L_, D_MIX, D_MODEL), D_MIX ** -0.5),
        'router_w': nrm((L_, D_MODEL, N_EXPERTS), D_MODEL ** -0.5),
        'router_b': nrm((L_, N_EXPERTS), 0.01),
        'exp_w_gu': nrm((L_, N_EXPERTS, D_MODEL, 2 * D_FF), D_MODEL ** -0.5),
        'exp_b_gu': nrm((L_, N_EXPERTS, 2 * D_FF), 0.01),
        'exp_w_down': nrm((L_, N_EXPERTS, D_FF, D_MODEL), D_FF ** -0.5),
        'exp_b_down': nrm((L_, N_EXPERTS, D_MODEL), 0.01),
        'final_g': gain((D_MODEL,)),
    }


def reference(x, c, ctx, c_ctx, w_mod, b_mod, norm1_g, norm2_g, w_in, lru_conv_w, lru_conv_b,
              lru_wa, lru_ba, lru_wi, lru_bi, lru_lambda, hy_conv_w, hy_conv_b, hy_w1, hy_b1,
              hy_w2, hy_b2, hy_w3, hy_b3, hy_freq, hy_w4, hy_d, gn_lru, gn_hy, w_out,
              router_w, router_b, exp_w_gu, exp_b_gu, exp_w_down, exp_b_down, final_g):
    hx = x
    hc = ctx
    silu_c = jax.nn.silu(c)
    silu_cc = jax.nn.silu(c_ctx)
    for l in range(DEPTH):
        need_ctx = l < DEPTH - 1
        mod_x = (silu_c @ w_mod[l] + b_mod[l])[:, None, :]
        mod_c = (silu_cc @ w_mod[l] + b_mod[l])[None, None, :]
        sh1x, sc1x, g1x, sh2x, sc2x, g2x = jnp.split(mod_x, N_MOD, axis=-1)
        sh1c, sc1c, g1c, sh2c, sc2c, g2c = jnp.split(mod_c, N_MOD, axis=-1)
        a_x = rmsnorm(hx, norm1_g[l]) * (1 + sc1x) + sh1x
        a_c = rmsnorm(hc, norm1_g[l]) * (1 + sc1c) + sh1c
        hy_fparams = (hy_w1[l], hy_b1[l], hy_w2[l], hy_b2[l], hy_w3[l], hy_b3[l], hy_freq[l], hy_w4[l])
        out_c, out_x = token_mixer(a_c, a_x, need_ctx, w_in[l], lru_conv_w[l], lru_conv_b[l],
                                   lru_wa[l], lru_ba[l], lru_wi[l], lru_bi[l], lru_lambda[l],
                                   hy_conv_w[l], hy_conv_b[l], hy_fparams, hy_d[l],
                                   gn_lru[l], gn_hy[l], w_out[l])
        hx = hx + g1x * out_x
        m_x = rmsnorm(hx, norm2_g[l]) * (1 + sc2x) + sh2x
        hx = hx + g2x * moe_ffn(m_x.reshape(-1, D_MODEL), router_w[l], router_b[l], exp_w_gu[l],
                                exp_b_gu[l], exp_w_down[l], exp_b_down[l]).reshape(hx.shape)
        if need_ctx:
            hc = hc + g1c * out_c
            m_c = rmsnorm(hc, norm2_g[l]) * (1 + sc2c) + sh2c
            hc = hc + g2c * moe_ffn(m_c.reshape(-1, D_MODEL), router_w[l], router_b[l], exp_w_gu[l],
                                    exp_b_gu[l], exp_w_down[l], exp_b_down[l]).reshape(hc.shape)
    return rmsnorm(hx, final_g)
```

```python
import numpy as np
from contextlib import ExitStack
import ml_dtypes
import concourse.bass as bass
import concourse.mybir as mybir
from concourse.bass_utils import run_bass_kernel_spmd

F32 = mybir.dt.float32
BF16 = mybir.dt.bfloat16
ALU = mybir.AluOpType
AF = mybir.ActivationFunctionType

S = 8192
CTX = 256
D = 2048
ST = S + CTX
NCORES = 8
SL = S // 2
EPS = 1e-6


class Trk:
    NDS = 8

    def __init__(self, nc):
        self.nc = nc
        self.es = ExitStack()
        self.engs = {'pe': nc.tensor, 'act': nc.scalar, 'dve': nc.vector, 'pool': nc.gpsimd, 'sp': nc.sync}
        self.sem = {e: self.es.enter_context(nc.semaphore('c_' + e)) for e in self.engs}
        self.cnt = {e: 0 for e in self.engs}
        self.dsem = {q: [self.es.enter_context(nc.semaphore(f'd_{q}{i}')) for i in range(self.NDS)]
                     for q in ('sp', 'pool')}
        self.duse = {q: [0] * self.NDS for q in self.dsem}
        self.drr = {q: 0 for q in self.dsem}
        self.waited = {e: {} for e in self.engs}
        self.lastw = {}
        self.readers = {}
        self.ntile = 0
        self.scopes = [self.es]

    def tile(self, shape, dt, name=None):
        self.ntile += 1
        return self.scopes[-1].enter_context(self.nc.sbuf_tensor(f'{name or "t"}_{self.ntile}', list(shape), dt))

    def ptile(self, shape, dt, name=None):
        self.ntile += 1
        return self.scopes[-1].enter_context(self.nc.psum_tensor(f'{name or "p"}_{self.ntile}', list(shape), dt))

    def push(self):
        self.scopes.append(ExitStack())

    def pop(self):
        self.barrier()
        self.scopes.pop().close()

    def _need(self, E, tok, waits):
        if tok is None:
            return
        kind, a, v = tok
        if kind == 'e':
            if a == E and E == 'pe':
                return
            key = ('e', a)
        else:
            key = ('d', a[0], a[1])
        if self.waited[E].get(key, -1) >= v:
            return
        self.waited[E][key] = v
        waits.append((key, v))

    def _deps(self, E, reads, writes):
        waits = []
        for b in reads:
            self._need(E, self.lastw.get(b), waits)
        for b in writes:
            self._need(E, self.lastw.get(b), waits)
            for t in self.readers.get(b, ()):
                self._need(E, t, waits)
        return waits

    def _commit(self, tok, reads, writes):
        for b in reads:
            self.readers.setdefault(b, []).append(tok)
        for b in writes:
            self.lastw[b] = tok
            self.readers[b] = []

    def _semof(self, key):
        return self.sem[key[1]] if key[0] == 'e' else self.dsem[key[1]][key[2]]

    def _emit(self, E, waits, fn, inc):
        eng = self.engs[E]
        for key, v in waits:
            eng.wait_ge(self._semof(key), v)
        if fn is None:
            return
        ins = fn(eng)
        if inc[0] == 'e':
            ins.then_inc(self.sem[E], 1)
        else:
            ins.then_inc(self.dsem[inc[1]][inc[2]], 16)

    def op(self, E, fn, reads=(), writes=()):
        waits = self._deps(E, reads, writes)
        self.cnt[E] += 1
        tok = ('e', E, self.cnt[E])
        self._emit(E, waits, fn, ('e', E))
        self._commit(tok, reads, writes)

    def dma(self, Q, fn, reads=(), writes=()):
        r = self.drr[Q]
        self.drr[Q] = (r + 1) % self.NDS
        waits = self._deps(Q, reads, writes)
        prev = self.duse[Q][r]
        if prev > 0:
            self._need(Q, ('d', (Q, r), 16 * prev), waits)
        self.duse[Q][r] += 1
        tok = ('d', (Q, r), 16 * self.duse[Q][r])
        self._emit(Q, waits, fn, ('d', Q, r))
        self._commit(tok, reads, writes)

    def barrier(self):
        for E in self.engs:
            waits = []
            for E2 in self.engs:
                if E2 != E and self.cnt[E2] > 0:
                    self._need(E, ('e', E2, self.cnt[E2]), waits)
            for q in self.dsem:
                for r in range(self.NDS):
                    if self.duse[q][r] > 0:
                        self._need(E, ('d', (q, r), 16 * self.duse[q][r]), waits)
            self._emit(E, waits, None, None)
        self.lastw = {}
        self.readers = {}

    def finish(self):
        self.barrier()
        while self.scopes:
            self.scopes.pop().close()


def fm(v, ntiles):
    return np.ascontiguousarray(np.asarray(v, np.float32).reshape(ntiles, 128).T)


def build(dbg=False):
    nc = bass.Bass("TRN2", target_bir_lowering=False)
    T = Trk(nc)

    def din(name, shape, dt=F32):
        return nc.dram_tensor(name, list(shape), dt, kind="ExternalInput").ap()

    def dout(name, shape, dt=F32):
        return nc.dram_tensor(name, list(shape), dt, kind="ExternalOutput").ap()

    def dscr(name, shape, dt):
        return nc.dram_tensor(name, list(shape), dt).ap()

    x_d = din("x", [S, D])
    ctx_d = din("ctx", [CTX, D])
    cT_d = din("cT", [128, 16, 2])
    wmod_d = din("w_mod", [D, 6 * D])
    bmodT_d = din("bmodT", [128, 96])
    n1gT_d = din("n1gT", [128, 16])
    n2gT_d = din("n2gT", [128, 16])
    fing_d = din("final_g", [1, D])
    win_d = din("w_in", [D, 5120])
    lcw_d = din("lcwT", [128, 8, 4])
    lcb_d = din("lcbT", [128, 8])
    lwa_d = din("lru_wa", [2, 8, 128, 128])
    lwi_d = din("lru_wi", [2, 8, 128, 128])
    lba_d = din("lbaT", [128, 2, 8])
    lbi_d = din("lbiT", [128, 2, 8])
    llam_d = din("llamT", [128, 2, 8])
    identf_d = din("identf", [128, 128])
    out_d = dout("out", [SL, D])
    xh_d = din("xh", [SL, D])
    offs_d = din("offs", [1, 8], mybir.dt.int32)
    dbg_d = {}
    if dbg:
        dbg_d['modT'] = dout("dbg_modT", [128, 96, 2])
        dbg_d['aT'] = dout("dbg_aT", [16, 128, ST], BF16)
        dbg_d['mix'] = dout("dbg_mix", [16, 128, S], BF16)
        dbg_d['kf'] = dout("dbg_kf", [2, 8, 128, 2, 65, 128], BF16)
    aT_d = dbg_d['aT'] if dbg else dscr("aT_s", [16, 128, ST], BF16)
    mix_d = dbg_d['mix'] if dbg else dscr("mix_s", [16, 128, S], BF16)
    kf_d = dbg_d['kf'] if dbg else dscr("kf_s", [2, 8, 128, 2, 65, 128], BF16)
    grow_d = dscr("grow_s", [2, D], F32)
    if dbg:
        dbg_d['hx1'] = dout("dbg_hx1", [SL, D]); dbg_d['mT'] = dout("dbg_mT", [16, 128, SL], BF16)
    hx1_d = dbg_d['hx1'] if dbg else dscr("hx1_s", [SL, D], F32)
    mT_d = dbg_d['mT'] if dbg else dscr("mT_s", [16, 128, SL], BF16)
    wout_d = din("w_out", [D, D]); gnT_d = din("gnT", [128, 16])
    wgu_d = din("w_gu", [32, D, 2 * D]); wdn_d = din("w_dn", [32, D, D])
    bguT_d = din("bguT", [128, 32, 32]); bd_d = din("b_dn", [32, D]); rw_d = din("router_w", [D, 32]); rb_d = din("router_b", [1, 32])
    if dbg:
        dbg_d['gT'] = dout("dbg_gT", [32, 1024])
    hcw_d = din("hcwT", [128, 24, 3]); hcb_d = din("hcbT", [128, 24])
    hw1_d = din("hy_w1", [33, 64]); hw2_d = din("hy_w2", [64, 64]); hw3_d = din("hy_w3", [64, 64])
    hb_d = din("hy_bT", [64, 4])
    hw4_d = din("hy_w4", [64, 4096]); hyd_d = din("hy_d", [2, 1024])
    zT_d = din("c_zT", [33, S]); dec_d = din("c_dec", [8, 128, S], BF16)
    identb_d = din("c_identb", [128, 128], BF16); f1_d = din("c_f1", [64, 130], BF16)
    twf_d = din("c_twf", [128, 130]); csn_d = din("c_csn", [128, 3, 128], BF16)
    csh_d = din("c_csh", [128, 2, 2, 128], BF16); twi_d = din("c_twi", [65, 2, 128]); eri_d = din("c_eri", [65, 2, 64], BF16)

    identf = T.tile([128, 128], F32, 'identf')
    T.dma('sp', lambda e: e.dma_start(out=identf[:], in_=identf_d), writes=['identf'])
    modT = T.tile([128, 96, 2], F32, 'modT')
    epsT = T.tile([128, 1], F32, 'epsT')
    oneT = T.tile([128, 1], F32, 'oneT')
    T.op('pool', lambda e: e.memset(epsT[:], EPS), writes=['epsT'])
    T.op('pool', lambda e: e.memset(oneT[:], 1.0), writes=['oneT'])
    A1 = T.tile([128, 16, 2], F32, 'A1')
    A2 = T.tile([128, 16, 2], F32, 'A2')

    T.push()
    cT = T.tile([128, 16, 2], F32, 'cT')
    sT = T.tile([128, 16, 2], F32, 'sT')
    bmodT = T.tile([128, 96], F32, 'bmodT')
    n1gT = T.tile([128, 16], F32, 'n1gT')
    T.dma('sp', lambda e: e.dma_start(out=cT[:], in_=cT_d), writes=['cT'])
    T.dma('sp', lambda e: e.dma_start(out=bmodT[:], in_=bmodT_d), writes=['bmodT'])
    T.dma('sp', lambda e: e.dma_start(out=n1gT[:], in_=n1gT_d), writes=['n1gT'])
    n2gT = T.tile([128, 16], F32, 'n2gT')
    T.dma('sp', lambda e: e.dma_start(out=n2gT[:], in_=n2gT_d), writes=['n2gT'])
    T.op('act', lambda e: e.activation(out=sT[:], in_=cT[:], func=AF.Silu), reads=['cT'], writes=['sT'])
    wm = [T.tile([128, 16, 512], F32, f'wm{i}') for i in range(2)]
    pm = [T.ptile([128, 4, 2], F32, f'pm{i}') for i in range(2)]
    for ch in range(24):
        w = wm[ch % 2]
        wk = f'wm{ch % 2}'
        pk = f'pm{ch % 2}'
        p = pm[ch % 2]
        src = wmod_d[:, ch * 512:(ch + 1) * 512].rearrange("(k p) f -> p k f", p=128)
        T.dma('sp', lambda e, w=w, src=src: e.dma_start(out=w[:], in_=src), writes=[wk])
        for j in range(4):
            for k in range(16):
                T.op('pe', lambda e, p=p, w=w, j=j, k=k: e.matmul(
                    p[:, j, :], lhsT=w[:, k, j * 128:(j + 1) * 128], rhs=sT[:, k, :],
                    start=(k == 0), stop=(k == 15)), reads=[wk, 'sT'], writes=[pk])
        for j in range(4):
            t = ch * 4 + j
            T.op('dve', lambda e, p=p, j=j, t=t: e.tensor_scalar(
                out=modT[:, t, :], in0=p[:, j, :], scalar1=bmodT[:, t:t + 1], scalar2=None, op0=ALU.add),
                reads=[pk, 'bmodT'], writes=['modT'])
    T.op('dve', lambda e: e.tensor_scalar(out=A1[:], in0=modT[:, 16:32, :], scalar1=1.0, scalar2=None, op0=ALU.add),
         reads=['modT'], writes=['A1'])
    for r in range(2):
        T.op('dve', lambda e, r=r: e.tensor_tensor(out=A1[:, :, r], in0=A1[:, :, r], in1=n1gT[:], op=ALU.mult),
             reads=['A1', 'n1gT'], writes=['A1'])
    T.op('dve', lambda e: e.tensor_scalar(out=A2[:], in0=modT[:, 64:80, :], scalar1=1.0, scalar2=None, op0=ALU.add),
         reads=['modT'], writes=['A2'])
    for r in range(2):
        T.op('dve', lambda e, r=r: e.tensor_tensor(out=A2[:, :, r], in0=A2[:, :, r], in1=n2gT[:], op=ALU.mult),
             reads=['A2', 'n2gT'], writes=['A2'])
    g12 = T.tile([128, 2, 16], F32, 'g12')
    T.op('dve', lambda e: e.tensor_copy(out=g12[:, 0, :], in_=modT[:, 32:48, 0]), reads=['modT'], writes=['g12'])
    T.op('dve', lambda e: e.tensor_copy(out=g12[:, 1, :], in_=modT[:, 80:96, 0]), reads=['modT'], writes=['g12'])
    T.dma('sp', lambda e: e.dma_start(out=grow_d.rearrange("r (t p) -> p r t", p=128), in_=g12[:], allow_slow_non_contiguous=True),
          reads=['g12'], writes=['grow_d'])
    if dbg:
        T.dma('sp', lambda e: e.dma_start(out=dbg_d['modT'], in_=modT[:]), reads=['modT'])
    T.pop()

    def norm_to_T(X, xk, A, B, row, dst, dk, col0, scr, ptr, pk_list, it, abk=('A1', 'modT')):
        junk, ss, XN = scr
        i2 = it % 2
        T.op('act', lambda e: e.activation(out=junk[i2][:], in_=X[:], func=AF.Square, accum_out=ss[i2][:]),
             reads=[xk], writes=[f'junk{i2}', f'ss{i2}'])
        T.op('act', lambda e: e.activation(out=ss[i2][:], in_=ss[i2][:], func=AF.Sqrt, scale=1.0 / D, bias=epsT[:, 0:1]),
             reads=[f'ss{i2}', 'epsT'], writes=[f'ss{i2}'])
        T.op('dve', lambda e: e.reciprocal(out=ss[i2][:], in_=ss[i2][:]), reads=[f'ss{i2}'], writes=[f'ss{i2}'])
        T.op('pool', lambda e: e.tensor_scalar(out=XN[i2][:], in0=X[:], scalar1=ss[i2][:, 0:1], scalar2=None, op0=ALU.mult),
             reads=[xk, f'ss{i2}'], writes=[f'XN{i2}'])
        for q in range(4):
            p = ptr[q % 2]
            pk = pk_list[q % 2]
            for jj in range(4):
                j = q * 4 + jj
                T.op('pe', lambda e, p=p, jj=jj, j=j: e.transpose(
                    out=p[:, jj * 128:(jj + 1) * 128], in_=XN[i2][:, j * 128:(j + 1) * 128], identity=identf[:]),
                    reads=[f'XN{i2}', 'identf'], writes=[pk])
            for jj in range(4):
                j = q * 4 + jj
                if jj % 2 == 0:
                    T.op('dve', lambda e, p=p, jj=jj, j=j: e.tensor_scalar(
                        out=dst[:, j, col0:col0 + 128], in0=p[:, jj * 128:(jj + 1) * 128],
                        scalar1=A[:, j, row:row + 1], scalar2=B[:, j, row:row + 1], op0=ALU.mult, op1=ALU.add),
                        reads=[pk] + list(abk), writes=[dk])
                else:
                    T.op('act', lambda e, p=p, jj=jj, j=j: e.activation(
                        out=dst[:, j, col0:col0 + 128], in_=p[:, jj * 128:(jj + 1) * 128], func=AF.Identity,
                        scale=A[:, j, row:row + 1], bias=B[:, j, row:row + 1]),
                        reads=[pk] + list(abk), writes=[dk])

    T.push()
    Xt = [T.tile([128, D], F32, f'X{i}') for i in range(2)]
    scr = ([T.tile([128, D], BF16, f'junk{i}') for i in range(2)],
           [T.tile([128, 1], F32, f'ss{i}') for i in range(2)],
           [T.tile([128, D], F32, f'XN{i}') for i in range(2)])
    ptr = [T.ptile([128, 512], F32, f'ptr{i}') for i in range(2)]
    aTg = [T.tile([128, 16, 512], BF16, f'aTg{i}') for i in range(2)]
    it = 0
    for g in range(17):
        nsub = 4 if g < 16 else 2
        ag = aTg[g % 2]
        agk = f'aTg{g % 2}'
        for sub in range(nsub):
            X = Xt[it % 2]
            xk = f'X{it % 2}'
            if g < 16:
                src = x_d[g * 512 + sub * 128: g * 512 + (sub + 1) * 128, :]
                row = 0
            else:
                src = ctx_d[sub * 128:(sub + 1) * 128, :]
                row = 1
            T.dma('sp', lambda e, X=X, src=src: e.dma_start(out=X[:], in_=src), writes=[xk])
            norm_to_T(X, xk, A1, modT[:, 0:16, :], row, ag, agk, sub * 128, scr, ptr, ['ptr0', 'ptr1'], it)
            it += 1
        ncol = nsub * 128
        dst = aT_d[:, :, g * 512: g * 512 + ncol].rearrange("j p t -> p j t")
        T.dma('sp', lambda e, ag=ag, dst=dst, ncol=ncol: e.dma_start(out=dst, in_=ag[:, :, 0:ncol]), reads=[agk], writes=['aT_d'])
    T.pop()

    def rev(ap2d, n):
        a = ap2d
        return bass.AP(tensor=a.tensor, offset=a.offset + (n - 1), ap=[list(a.ap[0]), [-1, n]])

    T.push()
    lcw = T.tile([128, 8, 4], F32, 'lcw'); lcb = T.tile([128, 8], F32, 'lcb')
    lba = T.tile([128, 2, 8], F32, 'lba'); lbi = T.tile([128, 2, 8], F32, 'lbi'); sca = T.tile([128, 2, 8], F32, 'sca')
    for t_, d_, k_ in ((lcw, lcw_d, 'lcw'), (lcb, lcb_d, 'lcb'), (lba, lba_d, 'lba'), (lbi, lbi_d, 'lbi'), (sca, llam_d, 'sca')):
        T.dma('sp', lambda e, t_=t_, d_=d_: e.dma_start(out=t_[:], in_=d_), writes=[k_])
    T.op('act', lambda e: e.activation(out=sca[:], in_=sca[:], func=AF.Exp, scale=-1.0), reads=['sca'], writes=['sca'])
    T.op('act', lambda e: e.activation(out=sca[:], in_=sca[:], func=AF.Ln, bias=oneT[:, 0:1]), reads=['sca', 'oneT'], writes=['sca'])
    T.op('dve', lambda e: e.tensor_scalar(out=sca[:], in0=sca[:], scalar1=-8.0, scalar2=None, op0=ALU.mult), reads=['sca'], writes=['sca'])

    RA = T.tile([128, ST], F32, 'RA'); UB = T.tile([128, ST], F32, 'UB')
    ub = T.tile([128, ST], BF16, 'ub'); gg = T.tile([128, S], BF16, 'gg')
    hs = T.tile([128, ST], F32, 'hs'); h2 = T.tile([128, ST], F32, 'h2')
    wrb = T.tile([128, 16, 128], BF16, 'wrb'); wgb = T.tile([128, 16, 128], BF16, 'wgb')
    gst = T.tile([128, 128], F32, 'gst')
    wab = [T.tile([128, 128], BF16, f'wab{d}') for d in range(2)]
    wib = [T.tile([128, 128], BF16, f'wib{d}') for d in range(2)]
    ag2 = [T.tile([128, 16, 512], BF16, 'ag0')]
    pp = [T.ptile([128, 512], F32, f'pp{i}') for i in range(4)]
    tmp = [T.tile([128, 512], F32, f'tmp{i}') for i in range(4)]

    for blk in range(8):
        for (wt, wk, c0) in ((wrb, 'wrb', blk * 128), (wgb, 'wgb', 1024 + blk * 128)):
            src = win_d[:, c0:c0 + 128].rearrange("(k p) f -> p k f", p=128)
            T.dma('pool', lambda e, src=src, wt=wt: e.dma_start(out=wt[:], in_=src), writes=[wk])
        for d in range(2):
            for (wt, wk, srcw) in ((wab[d], f'wab{d}', lwa_d), (wib[d], f'wib{d}', lwi_d)):
                T.dma('sp', lambda e, srcw=srcw, d=d: e.dma_start(out=gst[:], in_=srcw[d, blk]), writes=['gst'])
                T.op('dve', lambda e, wt=wt: e.tensor_copy(out=wt[:], in_=gst[:]), reads=['gst'], writes=[wk])
        for g in range(17):
            ncol = 512 if g < 16 else 256
            a = ag2[0]; ak = 'ag0'
            src = aT_d[:, :, g * 512: g * 512 + ncol].rearrange("j p t -> p j t")
            T.dma('sp', lambda e, a=a, src=src, ncol=ncol: e.dma_start(out=a[:, :, 0:ncol], in_=src), reads=['aT_d'], writes=[ak])
            p0 = pp[(2 * g) % 4]; p0k = f'pp{(2 * g) % 4}'
            for k in range(16):
                T.op('pe', lambda e, p0=p0, a=a, k=k, ncol=ncol: e.matmul(p0[:, 0:ncol], lhsT=wrb[:, k, :], rhs=a[:, k, 0:ncol],
                     start=(k == 0), stop=(k == 15)), reads=['wrb', ak], writes=[p0k])
            T.op('act', lambda e, p0=p0, g=g, ncol=ncol: e.activation(out=RA[:, g * 512: g * 512 + ncol], in_=p0[:, 0:ncol], func=AF.Copy),
                 reads=[p0k], writes=['RA'])
            if g < 16:
                p1 = pp[(2 * g + 1) % 4]; p1k = f'pp{(2 * g + 1) % 4}'
                for k in range(16):
                    T.op('pe', lambda e, p1=p1, a=a, k=k: e.matmul(p1[:, :], lhsT=wgb[:, k, :], rhs=a[:, k, :],
                         start=(k == 0), stop=(k == 15)), reads=['wgb', ak], writes=[p1k])
                T.op('act', lambda e, p1=p1, g=g: e.activation(out=gg[:, g * 512:(g + 1) * 512], in_=p1[:, :], func=AF.Gelu),
                     reads=[p1k], writes=['gg'])
        for (lo, n) in ((0, S), (S, CTX)):
            T.op('dve', lambda e, lo=lo, n=n: e.tensor_scalar(out=UB[:, lo:lo + n], in0=RA[:, lo:lo + n],
                 scalar1=lcw[:, blk, 2:3], scalar2=lcb[:, blk:blk + 1], op0=ALU.mult, op1=ALU.add),
                 reads=['RA', 'lcw', 'lcb'], writes=['UB'])
            for (tap, sh) in ((0, -2), (1, -1), (3, 1)):
                if sh < 0:
                    o_lo, o_n, i_lo = lo - sh, n + sh, lo
                else:
                    o_lo, o_n, i_lo = lo, n - sh, lo + sh
                T.op('dve', lambda e, tap=tap, o_lo=o_lo, o_n=o_n, i_lo=i_lo: e.scalar_tensor_tensor(
                    out=UB[:, o_lo:o_lo + o_n], in0=RA[:, i_lo:i_lo + o_n], scalar=lcw[:, blk, tap:tap + 1],
                    in1=UB[:, o_lo:o_lo + o_n], op0=ALU.mult, op1=ALU.add), reads=['RA', 'UB', 'lcw'], writes=['UB'])
        T.op('pool', lambda e: e.tensor_copy(out=ub[:], in_=UB[:]), reads=['UB'], writes=['ub'])
        for d in range(2):
            for g in range(17):
                ncol = 512 if g < 16 else 256
                c0 = g * 512
                pa = pp[(2 * g) % 4]; pak = f'pp{(2 * g) % 4}'
                pi_ = pp[(2 * g + 1) % 4]; pik = f'pp{(2 * g + 1) % 4}'
                T.op('pe', lambda e, pa=pa, c0=c0, ncol=ncol, d=d: e.matmul(pa[:, 0:ncol], lhsT=wab[d][:], rhs=ub[:, c0:c0 + ncol],
                     start=True, stop=True), reads=[f'wab{d}', 'ub'], writes=[pak])
                T.op('pe', lambda e, pi_=pi_, c0=c0, ncol=ncol, d=d: e.matmul(pi_[:, 0:ncol], lhsT=wib[d][:], rhs=ub[:, c0:c0 + ncol],
                     start=True, stop=True), reads=[f'wib{d}', 'ub'], writes=[pik])
                t0, t1, t2, t3 = tmp
                T.op('act', lambda e, pa=pa, ncol=ncol, d=d: e.activation(out=t0[:, 0:ncol], in_=pa[:, 0:ncol], func=AF.Sigmoid,
                     bias=lba[:, d, blk:blk + 1]), reads=[pak, 'lba'], writes=['tmp0'])
                T.op('act', lambda e, pi_=pi_, ncol=ncol, d=d: e.activation(out=t1[:, 0:ncol], in_=pi_[:, 0:ncol], func=AF.Sigmoid,
                     bias=lbi[:, d, blk:blk + 1]), reads=[pik, 'lbi'], writes=['tmp1'])
                T.op('act', lambda e, c0=c0, ncol=ncol, d=d: e.activation(out=RA[:, c0:c0 + ncol], in_=t0[:, 0:ncol], func=AF.Exp,
                     scale=sca[:, d, blk:blk + 1]), reads=['tmp0', 'sca'], writes=['RA'])
                T.op('act', lambda e, c0=c0, ncol=ncol: e.activation(out=t2[:, 0:ncol], in_=RA[:, c0:c0 + ncol], func=AF.Square),
                     reads=['RA'], writes=['tmp2'])
                T.op('act', lambda e, ncol=ncol: e.activation(out=t2[:, 0:ncol], in_=t2[:, 0:ncol], func=AF.Sqrt, scale=-1.0,
                     bias=oneT[:, 0:1]), reads=['tmp2', 'oneT'], writes=['tmp2'])
                T.op('dve', lambda e, c0=c0, ncol=ncol: e.tensor_tensor(out=t3[:, 0:ncol], in0=t1[:, 0:ncol], in1=ub[:, c0:c0 + ncol], op=ALU.mult),
                     reads=['tmp1', 'ub'], writes=['tmp3'])
                T.op('pool', lambda e, c0=c0, ncol=ncol: e.tensor_tensor(out=UB[:, c0:c0 + ncol], in0=t3[:, 0:ncol], in1=t2[:, 0:ncol], op=ALU.mult),
                     reads=['tmp3', 'tmp2'], writes=['UB'])
            H = hs if d == 0 else h2
            hk = 'hs' if d == 0 else 'h2'
            if d == 0:
                T.op('dve', lambda e: e.tensor_tensor_scan(out=H[:, S:ST], data0=RA[:, S:ST], data1=UB[:, S:ST], initial=0.0,
                     op0=ALU.mult, op1=ALU.add), reads=['RA', 'UB'], writes=[hk])
                T.op('dve', lambda e: e.tensor_tensor_scan(out=H[:, 0:S], data0=RA[:, 0:S], data1=UB[:, 0:S], initial=H[:, ST - 1:ST],
                     op0=ALU.mult, op1=ALU.add), reads=['RA', 'UB', hk], writes=[hk])
            else:
                T.op('dve', lambda e: e.tensor_tensor_scan(out=rev(H[:, S:ST], CTX), data0=rev(RA[:, S:ST], CTX), data1=rev(UB[:, S:ST], CTX),
                     initial=0.0, op0=ALU.mult, op1=ALU.add), reads=['RA', 'UB'], writes=[hk])
                T.op('dve', lambda e: e.tensor_tensor_scan(out=rev(H[:, 0:S], S), data0=rev(RA[:, 0:S], S), data1=rev(UB[:, 0:S], S),
                     initial=H[:, S:S + 1], op0=ALU.mult, op1=ALU.add), reads=['RA', 'UB', hk], writes=[hk])
        T.op('pool', lambda e: e.tensor_tensor(out=hs[:, 0:S], in0=hs[:, 0:S], in1=h2[:, 0:S], op=ALU.add), reads=['hs', 'h2'], writes=['hs'])
        T.op('dve', lambda e: e.tensor_tensor(out=hs[:, 0:S], in0=hs[:, 0:S], in1=gg[:, :], op=ALU.mult), reads=['hs', 'gg'], writes=['hs'])
        T.op('pool', lambda e: e.tensor_copy(out=gg[:, :], in_=hs[:, 0:S]), reads=['hs'], writes=['gg'])
        T.dma('sp', lambda e, blk=blk: e.dma_start(out=mix_d[blk], in_=gg[:, :]), reads=['gg'], writes=['mix_d'])
    T.pop()


    def bc_mid(ap2d, n):
        a = ap2d
        return bass.AP(tensor=a.tensor, offset=a.offset, ap=[list(a.ap[0]), [0, n], list(a.ap[1])])

    T.push()
    identb = T.tile([128, 128], BF16, 'identb'); f1m = T.tile([64, 130], BF16, 'f1m'); twf = T.tile([128, 130], F32, 'twf')
    csn = T.tile([128, 3, 128], BF16, 'csn'); csh = T.tile([128, 2, 2, 128], BF16, 'csh')
    twi = T.tile([65, 2, 128], F32, 'twi'); eri = T.tile([65, 2, 64], BF16, 'eri')
    for t_, d_, k_ in ((identb, identb_d, 'identb'), (f1m, f1_d, 'f1m'), (twf, twf_d, 'twf'), (csn, csn_d, 'csn'),
                       (csh, csh_d, 'csh'), (twi, twi_d, 'twi'), (eri, eri_d, 'eri')):
        T.dma('sp', lambda e, t_=t_, d_=d_: e.dma_start(out=t_[:], in_=d_), writes=[k_])
    UC = T.tile([128, 16384], BF16, 'UC')
    AY = T.tile([128, 2, 65, 128], BF16, 'AY')
    KF = T.tile([128, 2, 65, 128], BF16, 'KF')
    U = UC[0:64, :].rearrange("p (c n) -> p c n", c=128)
    CPP = UC[0:65, :].rearrange("p (r n c) -> p r n c", r=2, n=64)
    AYK = [f'AY{i}' for i in range(17)]
    KGS = [(4 * i, 4) for i in range(16)] + [(64, 1)]
    ftmp = [T.tile([128, 512], F32, f'ft{i}') for i in range(4)]

    def fft_forward(u, uk, mode, dbc=None):
        T.push()
        pt = [T.ptile([64, 8, 128], BF16, f'pt{i}') for i in range(2)]
        pa = [T.ptile([128, 3, 130], F32, f'pa{i}') for i in range(2)]
        px = [T.ptile([128, 512], F32, f'px{i}') for i in range(2)]
        tw = [T.tile([128, 3, 65], F32, f'tw{i}') for i in range(4)]
        for q in range(16):
            p = pt[q % 2]; pk = f'pt{q % 2}'
            for i in range(8):
                n2 = q * 8 + i
                T.op('pe', lambda e, p=p, i=i, n2=n2: e.transpose(out=p[:, i, :], in_=u[:, n2 * 64:(n2 + 1) * 64], identity=identb[:]),
                     reads=[uk, 'identb'], writes=[pk])
            dst = U[:, :, q * 8:(q + 1) * 8].rearrange("p c n -> p n c")
            if q % 2 == 0:
                T.op('act', lambda e, p=p, dst=dst: e.activation(out=dst, in_=p[:, :, :], func=AF.Copy), reads=[pk], writes=['UC'])
            else:
                T.op('dve', lambda e, p=p, dst=dst: e.tensor_copy(out=dst, in_=p[:, :, :]), reads=[pk], writes=['UC'])
        ngr = 43
        for gi in range(ngr):
            c0 = gi * 3; nch = min(3, 128 - c0)
            p = pa[gi % 2]; pk = f'pa{gi % 2}'
            for i in range(nch):
                T.op('pe', lambda e, p=p, i=i, c=c0 + i: e.matmul(p[:, i, :], lhsT=U[:, c, :], rhs=f1m[:, :], start=True, stop=True),
                     reads=['UC', 'f1m'], writes=[pk])
            Pr = p[:, 0:nch, 0:65]; Pi = p[:, 0:nch, 65:130]
            twr = bc_mid(twf[:, 0:65], nch); twim = bc_mid(twf[:, 65:130], nch)
            t1, t2, t3, t4 = [t[:, 0:nch, :] for t in tw]
            T.op('dve', lambda e, t1=t1, Pr=Pr, twr=twr: e.tensor_tensor(out=t1, in0=Pr, in1=twr, op=ALU.mult), reads=[pk, 'twf'], writes=['tw0'])
            T.op('dve', lambda e, t2=t2, Pi=Pi, twim=twim: e.tensor_tensor(out=t2, in0=Pi, in1=twim, op=ALU.mult), reads=[pk, 'twf'], writes=['tw1'])
            T.op('dve', lambda e, t3=t3, Pr=Pr, twim=twim: e.tensor_tensor(out=t3, in0=Pr, in1=twim, op=ALU.mult), reads=[pk, 'twf'], writes=['tw2'])
            T.op('dve', lambda e, t4=t4, Pi=Pi, twr=twr: e.tensor_tensor(out=t4, in0=Pi, in1=twr, op=ALU.mult), reads=[pk, 'twf'], writes=['tw3'])
            dr = AY[:, 0, :, c0:c0 + nch].rearrange("p k c -> p c k"); di = AY[:, 1, :, c0:c0 + nch].rearrange("p k c -> p c k")
            T.op('pool', lambda e, dr=dr, t1=t1, t2=t2: e.tensor_tensor(out=dr, in0=t1, in1=t2, op=ALU.subtract), reads=['tw0', 'tw1'], writes=AYK)
            T.op('pool', lambda e, di=di, t3=t3, t4=t4: e.tensor_tensor(out=di, in0=t3, in1=t4, op=ALU.add), reads=['tw2', 'tw3'], writes=AYK)
        for gi, (k0, nk) in enumerate(KGS):
            ncol = nk * 128
            pr = px[0]; prk = 'px0'
            pi_ = px[1]; pik = 'px1'
            Ar = AY[:, 0, k0:k0 + nk, :]; Ai = AY[:, 1, k0:k0 + nk, :]
            ak = AYK[gi]
            T.op('pe', lambda e, pr=pr, Ar=Ar, ncol=ncol: e.matmul(pr[:, 0:ncol], lhsT=csn[:, 0, :], rhs=Ar, start=True, stop=False), reads=[ak, 'csn'], writes=[prk])
            T.op('pe', lambda e, pr=pr, Ai=Ai, ncol=ncol: e.matmul(pr[:, 0:ncol], lhsT=csn[:, 1, :], rhs=Ai, start=False, stop=True), reads=[ak, 'csn'], writes=[prk])
            T.op('pe', lambda e, pi_=pi_, Ai=Ai, ncol=ncol: e.matmul(pi_[:, 0:ncol], lhsT=csn[:, 0, :], rhs=Ai, start=True, stop=False), reads=[ak, 'csn'], writes=[pik])
            T.op('pe', lambda e, pi_=pi_, Ar=Ar, ncol=ncol: e.matmul(pi_[:, 0:ncol], lhsT=csn[:, 2, :], rhs=Ar, start=False, stop=True), reads=[ak, 'csn'], writes=[pik])
            Xr = pr[:, 0:ncol].rearrange("p (k c) -> p k c", c=128); Xi = pi_[:, 0:ncol].rearrange("p (k c) -> p k c", c=128)
            Kr = KF[:, 0, k0:k0 + nk, :]; Ki = KF[:, 1, k0:k0 + nk, :]
            if mode == 'kf0':
                T.op('dve', lambda e, Kr=Kr, Xr=Xr, nk=nk: e.tensor_tensor(out=Kr, in0=Xr, in1=bc_mid(dbc[:, :], nk), op=ALU.add), reads=[prk, 'dbc'], writes=['KF'])
                T.op('act', lambda e, Ki=Ki, Xi=Xi: e.activation(out=Ki, in_=Xi, func=AF.Copy), reads=[pik], writes=['KF'])
            elif mode == 'kf1':
                T.op('dve', lambda e, Kr=Kr, Xr=Xr: e.tensor_tensor(out=Kr, in0=Kr, in1=Xr, op=ALU.add), reads=[prk, 'KF'], writes=['KF'])
                T.op('dve', lambda e, Ki=Ki, Xi=Xi: e.tensor_tensor(out=Ki, in0=Ki, in1=Xi, op=ALU.subtract), reads=[pik, 'KF'], writes=['KF'])
            else:
                f1_, f2_, f3_, f4_ = [t[:, 0:ncol].rearrange("p (k c) -> p k c", c=128) for t in ftmp]
                T.op('dve', lambda e, f1_=f1_, Xr=Xr, Kr=Kr: e.tensor_tensor(out=f1_, in0=Xr, in1=Kr, op=ALU.mult), reads=[prk, 'KF'], writes=['ft0'])
                T.op('dve', lambda e, f2_=f2_, Xi=Xi, Ki=Ki: e.tensor_tensor(out=f2_, in0=Xi, in1=Ki, op=ALU.mult), reads=[pik, 'KF'], writes=['ft1'])
                T.op('dve', lambda e, f3_=f3_, Xr=Xr, Ki=Ki: e.tensor_tensor(out=f3_, in0=Xr, in1=Ki, op=ALU.mult), reads=[prk, 'KF'], writes=['ft2'])
                T.op('dve', lambda e, f4_=f4_, Xi=Xi, Kr=Kr: e.tensor_tensor(out=f4_, in0=Xi, in1=Kr, op=ALU.mult), reads=[pik, 'KF'], writes=['ft3'])
                T.op('pool', lambda e, Ar=Ar, f1_=f1_, f2_=f2_: e.tensor_tensor(out=Ar, in0=f1_, in1=f2_, op=ALU.subtract), reads=['ft0', 'ft1'], writes=[ak])
                T.op('pool', lambda e, Ai=Ai, f3_=f3_, f4_=f4_: e.tensor_tensor(out=Ai, in0=f3_, in1=f4_, op=ALU.add), reads=['ft2', 'ft3'], writes=[ak])
        T.pop()

    def fft_inverse(gate, gk, dst, dk):
        T.push()
        pc = [T.ptile([65, 4, 128], F32, f'pc{i}') for i in range(2)]
        py = [T.ptile([128, 8, 64], F32, f'py{i}') for i in range(2)]
        tw = [T.tile([65, 4, 64], F32, f'iw{i}') for i in range(4)]
        for h in range(2):
            for gi in range(32):
                c0 = gi * 4
                p = pc[gi % 2]; pk = f'pc{gi % 2}'
                for i in range(4):
                    c = c0 + i
                    T.op('pe', lambda e, p=p, i=i, c=c: e.matmul(p[:, i, :], lhsT=AY[:, 0, :, c], rhs=csh[:, 0, h, :], start=True, stop=False),
                         reads=AYK + ['csh'], writes=[pk])
                    T.op('pe', lambda e, p=p, i=i, c=c: e.matmul(p[:, i, :], lhsT=AY[:, 1, :, c], rhs=csh[:, 1, h, :], start=False, stop=True),
                         reads=AYK + ['csh'], writes=[pk])
                Pr = p[:, :, 0:64]; Pi = p[:, :, 64:128]
                ct = bc_mid(twi[:, 0, h * 64:(h + 1) * 64], 4); st = bc_mid(twi[:, 1, h * 64:(h + 1) * 64], 4)
                t1, t2, t3, t4 = [t[:, :, :] for t in tw]
                T.op('dve', lambda e, t1=t1, Pr=Pr, ct=ct: e.tensor_tensor(out=t1, in0=Pr, in1=ct, op=ALU.mult), reads=[pk, 'twi'], writes=['iw0'])
                T.op('dve', lambda e, t2=t2, Pi=Pi, st=st: e.tensor_tensor(out=t2, in0=Pi, in1=st, op=ALU.mult), reads=[pk, 'twi'], writes=['iw1'])
                T.op('dve', lambda e, t3=t3, Pr=Pr, st=st: e.tensor_tensor(out=t3, in0=Pr, in1=st, op=ALU.mult), reads=[pk, 'twi'], writes=['iw2'])
                T.op('dve', lambda e, t4=t4, Pi=Pi, ct=ct: e.tensor_tensor(out=t4, in0=Pi, in1=ct, op=ALU.mult), reads=[pk, 'twi'], writes=['iw3'])
                dr = CPP[:, 0, :, c0:c0 + 4].rearrange("p n c -> p c n"); di = CPP[:, 1, :, c0:c0 + 4].rearrange("p n c -> p c n")
                T.op('pool', lambda e, dr=dr, t1=t1, t2=t2: e.tensor_tensor(out=dr, in0=t1, in1=t2, op=ALU.subtract), reads=['iw0', 'iw1'], writes=['UC'])
                T.op('pool', lambda e, di=di, t3=t3, t4=t4: e.tensor_tensor(out=di, in0=t3, in1=t4, op=ALU.add), reads=['iw2', 'iw3'], writes=['UC'])
            for q in range(8):
                p = py[q % 2]; pk = f'py{q % 2}'
                for i in range(8):
                    nl = q * 8 + i
                    T.op('pe', lambda e, p=p, i=i, nl=nl: e.matmul(p[:, i, :], lhsT=CPP[:, 0, nl, :], rhs=eri[:, 0, :], start=True, stop=False),
                         reads=['UC', 'eri'], writes=[pk])
                    T.op('pe', lambda e, p=p, i=i, nl=nl: e.matmul(p[:, i, :], lhsT=CPP[:, 1, nl, :], rhs=eri[:, 1, :], start=False, stop=True),
                         reads=['UC', 'eri'], writes=[pk])
                tok0 = (h * 64 + q * 8) * 64
                T.op('dve', lambda e, p=p, tok0=tok0: e.tensor_tensor(out=dst[:, tok0:tok0 + 512], in0=p[:, :, :].rearrange("p a b -> p (a b)"),
                     in1=gate[:, tok0:tok0 + 512], op=ALU.mult), reads=[pk, gk], writes=[dk])
        T.pop()

    T.push()
    h3T = T.tile([64, S], F32, 'h3T')
    hbT = T.tile([64, 4], F32, 'hbT'); frb = T.tile([64, 3], F32, 'frb')
    T.dma('sp', lambda e: e.dma_start(out=hbT[:], in_=hb_d), writes=['hbT'])
    for l in range(3):
        T.op('dve', lambda e, l=l: e.tensor_tensor(out=frb[:, l:l + 1], in0=hbT[:, l:l + 1], in1=hbT[:, 3:4], op=ALU.mult), reads=['hbT'], writes=['frb'])
    T.push()
    zT = T.tile([33, S], F32, 'zT')
    w1 = T.tile([33, 64], F32, 'w1'); w2 = T.tile([64, 64], F32, 'w2'); w3 = T.tile([64, 64], F32, 'w3')
    for t_, d_, k_ in ((zT, zT_d, 'zT'), (w1, hw1_d, 'w1'), (w2, hw2_d, 'w2'), (w3, hw3_d, 'w3')):
        T.dma('sp', lambda e, t_=t_, d_=d_: e.dma_start(out=t_[:], in_=d_), writes=[k_])
    pm_ = [T.ptile([64, 512], F32, f'pml{i}') for i in range(2)]
    ha = [T.tile([64, 512], F32, f'ha{i}') for i in range(2)]
    PI_ = 3.14159
    for cc in range(16):
        cs = slice(cc * 512, (cc + 1) * 512)
        cur = zT[:, cs]; curk = 'zT'
        for l, (wl, wk) in enumerate(((w1, 'w1'), (w2, 'w2'), (w3, 'w3'))):
            p = pm_[l % 2]; pk = f'pml{l % 2}'
            T.op('pe', lambda e, p=p, wl=wl, cur=cur: e.matmul(p[:, :], lhsT=wl[:, :], rhs=cur, start=True, stop=True), reads=[wk, curk], writes=[pk])
            dstt = ha[l % 2][:, :] if l < 2 else h3T[:, cs]
            dk = f'ha{l % 2}' if l < 2 else 'h3T'
            T.op('dve', lambda e, p=p, dstt=dstt, l=l: e.tensor_scalar(out=dstt, in0=p[:, :], scalar1=hbT[:, 3:4], scalar2=frb[:, l:l + 1],
                 op0=ALU.mult, op1=ALU.add), reads=[pk, 'hbT', 'frb'], writes=[dk])
            T.op('dve', lambda e, dstt=dstt: e.tensor_scalar(out=dstt, in0=dstt, scalar1=PI_, scalar2=-PI_, op0=ALU.min, op1=ALU.max), reads=[dk], writes=[dk])
            T.op('act', lambda e, dstt=dstt: e.activation(out=dstt, in_=dstt, func=AF.Sin), reads=[dk], writes=[dk])
            cur = dstt; curk = dk
    T.pop()
    dec = T.tile([128, S], BF16, 'dec'); hch = T.tile([128, S], BF16, 'hch')
    w4t = T.tile([64, 128], F32, 'w4t'); dbc = T.tile([128, 128], F32, 'dbc')
    pf = [T.ptile([128, 512], F32, f'pf{i}') for i in range(2)]
    for ch in range(8):
        T.dma('sp', lambda e, ch=ch: e.dma_start(out=dec[:], in_=dec_d[ch]), writes=['dec'])
        for o in range(2):
            T.dma('sp', lambda e, o=o: e.dma_start(out=dbc[:], in_=hyd_d[o:o + 1, ch * 128:(ch + 1) * 128].partition_broadcast(128)), writes=['dbc'])
            for d in range(2):
                col = o * 2048 + d * 1024 + ch * 128
                T.dma('sp', lambda e, col=col: e.dma_start(out=w4t[:], in_=hw4_d[:, col:col + 128]), writes=['w4t'])
                for cc in range(16):
                    p = pf[cc % 2]; pk = f'pf{cc % 2}'
                    T.op('pe', lambda e, p=p, cc=cc: e.matmul(p[:, :], lhsT=w4t[:, :], rhs=h3T[:, cc * 512:(cc + 1) * 512], start=True, stop=True),
                         reads=['w4t', 'h3T'], writes=[pk])
                    T.op('dve', lambda e, p=p, cc=cc: e.tensor_tensor(out=hch[:, cc * 512:(cc + 1) * 512], in0=p[:, :], in1=dec[:, cc * 512:(cc + 1) * 512],
                         op=ALU.mult), reads=[pk, 'dec'], writes=['hch'])
                if d == 1:
                    T.op('pool', lambda e: e.memset(hch[:, 0:1], 0.0), reads=['hch'], writes=['hch'])
                fft_forward(hch, 'hch', 'kf0' if d == 0 else 'kf1', dbc)
            T.dma('sp', lambda e, o=o: e.dma_start(out=kf_d[o, ch], in_=KF[:]), reads=['KF'], writes=['kf_d'])
    T.pop()


    T.push()
    Bb = [T.tile([128, S], BF16, f'B{i}') for i in range(4)]
    hcw = T.tile([128, 24, 3], F32, 'hcw'); hcb = T.tile([128, 24], F32, 'hcb')
    T.dma('sp', lambda e: e.dma_start(out=hcw[:], in_=hcw_d), writes=['hcw'])
    T.dma('sp', lambda e: e.dma_start(out=hcb[:], in_=hcb_d), writes=['hcb'])
    hag = T.tile([128, 16, 512], BF16, 'hag')
    hwb = [T.tile([128, 16, 128], BF16, f'hwb{i}') for i in range(3)]
    for ch in range(8):
        for sig in range(3):
            c0 = 2048 + sig * 1024 + ch * 128
            src = win_d[:, c0:c0 + 128].rearrange("(k p) f -> p k f", p=128)
            T.dma('pool', lambda e, src=src, sig=sig: e.dma_start(out=hwb[sig][:], in_=src), writes=[f'hwb{sig}'])
        T.push()
        hp = [T.ptile([128, 512], F32, f'hp{i}') for i in range(4)]
        cnt = 0
        for g in range(16):
            src = aT_d[:, :, g * 512:(g + 1) * 512].rearrange("j p t -> p j t")
            T.dma('sp', lambda e, src=src: e.dma_start(out=hag[:], in_=src), reads=['aT_d'], writes=['hag'])
            for sig in range(3):
                p = hp[cnt % 4]; pk = f'hp{cnt % 4}'; cnt += 1
                for k in range(16):
                    T.op('pe', lambda e, p=p, sig=sig, k=k: e.matmul(p[:, :], lhsT=hwb[sig][:, k, :], rhs=hag[:, k, :], start=(k == 0), stop=(k == 15)),
                         reads=[f'hwb{sig}', 'hag'], writes=[pk])
                T.op('act', lambda e, p=p, sig=sig, g=g: e.activation(out=Bb[sig][:, g * 512:(g + 1) * 512], in_=p[:, :], func=AF.Copy),
                     reads=[pk], writes=[f'B{sig}'])
        T.pop()
        for sig, (si, di) in enumerate(((0, 3), (1, 0), (2, 1))):
            t = sig * 8 + ch
            src = Bb[si]; dst = Bb[di]; sk = f'B{si}'; dk = f'B{di}'
            T.op('dve', lambda e, src=src, dst=dst, t=t: e.tensor_scalar(out=dst[:, :], in0=src[:, :], scalar1=hcw[:, t, 1:2], scalar2=hcb[:, t:t + 1],
                 op0=ALU.mult, op1=ALU.add), reads=[sk, 'hcw', 'hcb'], writes=[dk])
            for (tap, o_lo, i_lo, n) in ((0, 64, 0, S - 64), (2, 0, 64, S - 64), (0, 1, S - 64, 63), (2, S - 64, 1, 63)):
                T.op('dve', lambda e, src=src, dst=dst, t=t, tap=tap, o_lo=o_lo, i_lo=i_lo, n=n: e.scalar_tensor_tensor(
                    out=dst[:, o_lo:o_lo + n], in0=src[:, i_lo:i_lo + n], scalar=hcw[:, t, tap:tap + 1], in1=dst[:, o_lo:o_lo + n],
                    op0=ALU.mult, op1=ALU.add), reads=[sk, dk, 'hcw'], writes=[dk])
        T.dma('sp', lambda e, ch=ch: e.dma_start(out=KF[:], in_=kf_d[0, ch]), reads=['kf_d'], writes=['KF'])
        fft_forward(Bb[3], 'B3', 'mul')
        fft_inverse(Bb[0], 'B0', Bb[2], 'B2')
        T.dma('sp', lambda e, ch=ch: e.dma_start(out=KF[:], in_=kf_d[1, ch]), reads=['kf_d'], writes=['KF'])
        fft_forward(Bb[2], 'B2', 'mul')
        fft_inverse(Bb[1], 'B1', Bb[3], 'B3')
        T.dma('sp', lambda e, ch=ch: e.dma_start(out=mix_d[8 + ch], in_=Bb[3][:, :]), reads=['B3'], writes=['mix_d'])
    T.pop()
    T.pop()


    T.push()
    wob = T.tile([128, 16, D], BF16, 'wob')
    for dc in range(4):
        src = wout_d[:, dc * 512:(dc + 1) * 512].rearrange("(k p) f -> p k f", p=128)
        T.dma('pool', lambda e, src=src, dc=dc: e.dma_start(out=wob[:, :, dc * 512:(dc + 1) * 512], in_=src), writes=['wob'])
    gnT = T.tile([128, 16], F32, 'gnT'); G1bc = T.tile([128, D], F32, 'G1bc'); onesb = T.tile([128, 128], BF16, 'onesb')
    T.dma('sp', lambda e: e.dma_start(out=gnT[:], in_=gnT_d), writes=['gnT'])
    T.dma('sp', lambda e: e.dma_start(out=G1bc[:], in_=grow_d[0:1, :].partition_broadcast(128)), reads=['grow_d'], writes=['G1bc'])
    T.op('pool', lambda e: e.memset(onesb[:], 1.0), writes=['onesb'])
    mixg = T.tile([128, 16, 512], BF16, 'mixg'); mixn = T.tile([128, 16, 512], BF16, 'mixn')
    sqb = [T.tile([128, 512], BF16, f'sqb{i}') for i in range(2)]
    rs = T.tile([128, 2, 512], F32, 'rs')
    prs = [T.ptile([128, 512], F32, f'prs{i}') for i in range(2)]
    po = [T.ptile([128, 512], F32, f'po{i}') for i in range(2)]
    ptr3 = [T.ptile([128, 512], F32, f'ptr3{i}') for i in range(2)]
    X3 = [T.tile([128, D], F32, f'X3{i}') for i in range(2)]
    H3 = [T.tile([128, D], F32, f'H3{i}') for i in range(2)]
    scr3 = ([T.tile([128, D], BF16, f'junk{i}') for i in range(2)],
            [T.tile([128, 1], F32, f'ss{i}') for i in range(2)],
            [T.tile([128, D], F32, f'XN{i}') for i in range(2)])
    mTg = [T.tile([128, 16, 512], BF16, f'mTg{i}') for i in range(2)]
    it = 0
    offreg = T.es.enter_context(nc.sync.register("offreg"))
    for g in range(SL // 512):
        def ld_mix(e, g=g):
            e.reg_load(offreg, offs_d[0:1, g:g + 1])
            v = e.snap(offreg)
            return e.dma_start(out=mixg[:], in_=mix_d[:, :, bass.ds(v, 512)].rearrange("j p t -> p j t"))
        T.dma('sp', ld_mix, reads=['mix_d'], writes=['mixg'])
        for grp in range(2):
            for jj in range(8):
                j = grp * 8 + jj
                sq = sqb[j % 2]; sk = f'sqb{j % 2}'
                T.op('act', lambda e, sq=sq, j=j: e.activation(out=sq[:], in_=mixg[:, j, :], func=AF.Square), reads=['mixg'], writes=[sk])
                T.op('pe', lambda e, sq=sq, grp=grp, jj=jj: e.matmul(prs[grp][:, :], lhsT=onesb[:, :], rhs=sq[:, :], start=(jj == 0), stop=(jj == 7)),
                     reads=[sk, 'onesb'], writes=[f'prs{grp}'])
            T.op('act', lambda e, grp=grp: e.activation(out=rs[:, grp, :], in_=prs[grp][:, :], func=AF.Sqrt, scale=1.0 / 1024, bias=epsT[:, 0:1]),
                 reads=[f'prs{grp}', 'epsT'], writes=['rs'])
            T.op('dve', lambda e, grp=grp: e.reciprocal(out=rs[:, grp, :], in_=rs[:, grp, :]), reads=['rs'], writes=['rs'])
        for j in range(16):
            T.op('dve', lambda e, j=j: e.scalar_tensor_tensor(out=mixn[:, j, :], in0=mixg[:, j, :], scalar=gnT[:, j:j + 1], in1=rs[:, j // 8, :],
                 op0=ALU.mult, op1=ALU.mult), reads=['mixg', 'gnT', 'rs'], writes=['mixn'])
        mg = mTg[g % 2]; mgk = f'mTg{g % 2}'
        for sub in range(4):
            X = X3[it % 2]; xk = f'X3{it % 2}'; H = H3[it % 2]; hk = f'H3{it % 2}'
            r0 = g * 512 + sub * 128
            T.dma('sp', lambda e, X=X, r0=r0: e.dma_start(out=X[:], in_=xh_d[r0:r0 + 128, :]), writes=[xk])
            for dc in range(4):
                p = po[dc % 2]; pk = f'po{dc % 2}'
                for k in range(16):
                    T.op('pe', lambda e, p=p, k=k, sub=sub, dc=dc: e.matmul(p[:, :], lhsT=mixn[:, k, sub * 128:(sub + 1) * 128],
                         rhs=wob[:, k, dc * 512:(dc + 1) * 512], start=(k == 0), stop=(k == 15)), reads=['mixn', 'wob'], writes=[pk])
                T.op('dve', lambda e, p=p, H=H, dc=dc: e.tensor_tensor(out=H[:, dc * 512:(dc + 1) * 512], in0=p[:, :], in1=G1bc[:, dc * 512:(dc + 1) * 512],
                     op=ALU.mult), reads=[pk, 'G1bc'], writes=[hk])
            T.op('pool', lambda e, H=H, X=X: e.tensor_tensor(out=H[:], in0=H[:], in1=X[:], op=ALU.add), reads=[hk, xk], writes=[hk])
            T.dma('sp', lambda e, H=H, r0=r0: e.dma_start(out=hx1_d[r0:r0 + 128, :], in_=H[:]), reads=[hk], writes=['hx1_d'])
            norm_to_T(H, hk, A2, modT[:, 48:64, :], 0, mg, mgk, sub * 128, scr3, ptr3, ['ptr30', 'ptr31'], it, abk=('A2', 'modT'))
            it += 1
        dst = mT_d[:, :, g * 512:(g + 1) * 512].rearrange("j p t -> p j t")
        T.dma('sp', lambda e, mg=mg, dst=dst: e.dma_start(out=dst, in_=mg[:]), reads=[mgk], writes=['mT_d'])
    T.pop()

    TC = 1024
    T.push()
    PS = [T.ptile([128, 512], F32, f'PS{i}') for i in range(8)]
    mTc = T.tile([128, 16, TC], BF16, 'mTc'); acc = T.tile([128, 16, TC], F32, 'acc'); actb = T.tile([128, 16, TC], BF16, 'actb')
    wst = [T.tile([128, 8, 256], F32, f'wst{i}') for i in range(2)]
    wr = [T.tile([128, 16, 256], BF16, f'wr{i}') for i in range(4)]
    gT = T.tile([32, TC], F32, 'gT'); gbc = T.tile([128, TC], BF16, 'gbc')
    mt = [T.tile([128, 512], F32, f'mt{i}') for i in range(3)]
    bguT = T.tile([128, 32, 32], F32, 'bguT'); BD = T.tile([32, D], F32, 'BD')
    rwb = T.tile([128, 16, 32], BF16, 'rwb'); RBbc = T.tile([128, 32], F32, 'RBbc')
    T.dma('sp', lambda e: e.dma_start(out=bguT[:], in_=bguT_d), writes=['bguT'])
    T.dma('sp', lambda e: e.dma_start(out=BD[:], in_=bd_d), writes=['BD'])
    T.dma('pool', lambda e: e.dma_start(out=rwb[:], in_=rw_d.rearrange("(k p) f -> p k f", p=128)), writes=['rwb'])
    T.dma('sp', lambda e: e.dma_start(out=RBbc[:], in_=rb_d.partition_broadcast(128)), writes=['RBbc'])
    Lg = T.tile([128, 32], F32, 'Lg'); Eg = T.tile([128, 32], F32, 'Eg'); v8 = T.tile([128, 8], F32, 'v8'); sm = T.tile([128, 2], F32, 'sm')
    wcount = [0]
    def f32view(ap3):
        return ap3.bitcast(F32).rearrange("p a b -> p (a b)")
    hx_t = [f32view(actb[:, 0:4, :]), f32view(actb[:, 4:8, :])]
    mo_t = f32view(actb[:, 8:12, :])
    G2bc = f32view(actb[:, 12:16, :])
    finbc = wst[0][:, :, :].rearrange("p a b -> p (a b)")

    def load_piece(src_fn, ncols=256):
        r = wcount[0] % 4
        wt = wr[r]; wk = f'wr{r}'
        for half in range(2):
            stg = wst[(2 * wcount[0] + half) % 2]; sk = f'wst{(2 * wcount[0] + half) % 2}'
            T.dma('sp', lambda e, stg=stg, half=half: e.dma_start(out=stg[:], in_=src_fn(half)), writes=[sk])
            eng = ('act', 'dve', 'pool')[(2 * wcount[0] + half) % 3]
            if eng == 'act':
                T.op('act', lambda e, wt=wt, stg=stg, half=half: e.activation(out=wt[:, half * 8:(half + 1) * 8, :], in_=stg[:], func=AF.Copy), reads=[sk], writes=[wk])
            else:
                T.op(eng, lambda e, wt=wt, stg=stg, half=half: e.tensor_copy(out=wt[:, half * 8:(half + 1) * 8, :], in_=stg[:]), reads=[sk], writes=[wk])
        wcount[0] += 1
        return wt, wk

    def bc_free(ap_col, n):
        a = ap_col
        return bass.AP(tensor=a.tensor, offset=a.offset, ap=[list(a.ap[0]), [0, n]])

    for chk in range(SL // TC):
        t0 = chk * TC
        T.dma('sp', lambda e, t0=t0: e.dma_start(out=mTc[:], in_=mT_d[:, :, t0:t0 + TC].rearrange("j p t -> p j t")), reads=['mT_d'], writes=['mTc'])
        for sub in range(TC // 128):
            for k in range(16):
                T.op('pe', lambda e, k=k, sub=sub: e.matmul(PS[0][:, 0:32], lhsT=mTc[:, k, sub * 128:(sub + 1) * 128], rhs=rwb[:, k, :],
                     start=(k == 0), stop=(k == 15)), reads=['mTc', 'rwb'], writes=['PS0'])
            T.op('dve', lambda e: e.tensor_tensor(out=Lg[:], in0=PS[0][:, 0:32], in1=RBbc[:], op=ALU.add), reads=['PS0', 'RBbc'], writes=['Lg'])
            T.op('dve', lambda e: e.max(out=v8[:], in_=Lg[:]), reads=['Lg'], writes=['v8'])
            T.op('dve', lambda e: e.tensor_scalar(out=sm[:, 0:1], in0=v8[:, 0:1], scalar1=-1.0, scalar2=None, op0=ALU.mult), reads=['v8'], writes=['sm'])
            T.op('act', lambda e: e.activation(out=Eg[:], in_=Lg[:], func=AF.Exp, bias=sm[:, 0:1]), reads=['Lg', 'sm'], writes=['Eg'])
            T.op('dve', lambda e: e.tensor_scalar(out=Lg[:], in0=Lg[:], scalar1=v8[:, 3:4], scalar2=None, op0=ALU.is_ge), reads=['Lg', 'v8'], writes=['Lg'])
            T.op('dve', lambda e: e.tensor_tensor(out=Eg[:], in0=Eg[:], in1=Lg[:], op=ALU.mult), reads=['Eg', 'Lg'], writes=['Eg'])
            T.op('dve', lambda e: e.tensor_reduce(out=sm[:, 1:2], in_=Eg[:], axis=mybir.AxisListType.X, op=ALU.add), reads=['Eg'], writes=['sm'])
            T.op('dve', lambda e: e.reciprocal(out=sm[:, 1:2], in_=sm[:, 1:2]), reads=['sm'], writes=['sm'])
            T.op('dve', lambda e: e.tensor_scalar(out=Eg[:], in0=Eg[:], scalar1=sm[:, 1:2], scalar2=None, op0=ALU.mult), reads=['Eg', 'sm'], writes=['Eg'])
            T.op('pe', lambda e: e.transpose(out=PS[1][0:32, 0:128], in_=Eg[:, :], identity=identf[:]), reads=['Eg', 'identf'], writes=['PS1'])
            T.op('act', lambda e, sub=sub: e.activation(out=gT[:, sub * 128:(sub + 1) * 128], in_=PS[1][0:32, 0:128], func=AF.Copy), reads=['PS1'], writes=['gT'])
        if dbg and chk == 0:
            T.dma('sp', lambda e: e.dma_start(out=dbg_d['gT'], in_=gT[:]), reads=['gT'])
        for m in range(16):
            for h in range(2):
                p = PS[2 + h]; pk = f'PS{2 + h}'
                T.op('pe', lambda e, p=p, m=m, h=h: e.matmul(p[:, :], lhsT=BD[0:32, m * 128:(m + 1) * 128], rhs=gT[0:32, h * 512:(h + 1) * 512],
                     start=True, stop=True), reads=['BD', 'gT'], writes=[pk])
                T.op('act', lambda e, p=p, m=m, h=h: e.activation(out=acc[:, m, h * 512:(h + 1) * 512], in_=p[:, :], func=AF.Copy), reads=[pk], writes=['acc'])
        for ex in range(32):
            for h in range(2):
                p = PS[2 + h]; pk = f'PS{2 + h}'
                T.op('pe', lambda e, p=p, h=h, ex=ex: e.matmul(p[:, :], lhsT=bc_free(identf[0:32, ex:ex + 1], 128), rhs=gT[0:32, h * 512:(h + 1) * 512],
                     start=True, stop=True), reads=['identf', 'gT'], writes=[pk])
                T.op('act', lambda e, p=p, h=h: e.activation(out=gbc[:, h * 512:(h + 1) * 512], in_=p[:, :], func=AF.Copy), reads=[pk], writes=['gbc'])
            for step in range(8):
                j0 = 2 * step
                wg, wgk = load_piece(lambda half, ex=ex, j0=j0: wgu_d[ex, half * 1024:(half + 1) * 1024, j0 * 128:j0 * 128 + 256].rearrange("(k p) f -> p k f", p=128))
                wl, wlk = load_piece(lambda half, ex=ex, j0=j0: wgu_d[ex, half * 1024:(half + 1) * 1024, 2048 + j0 * 128:2048 + j0 * 128 + 256].rearrange("(k p) f -> p k f", p=128))
                for jj in range(2):
                    j = j0 + jj
                    for h in range(2):
                        pg = PS[4 + h]; pgk = f'PS{4 + h}'; pl_ = PS[6 + h]; plk = f'PS{6 + h}'
                        for k in range(16):
                            T.op('pe', lambda e, pg=pg, wg=wg, k=k, jj=jj, h=h: e.matmul(pg[:, :], lhsT=wg[:, k, jj * 128:(jj + 1) * 128],
                                 rhs=mTc[:, k, h * 512:(h + 1) * 512], start=(k == 0), stop=(k == 15)), reads=[wgk, 'mTc'], writes=[pgk])
                        for k in range(16):
                            T.op('pe', lambda e, pl_=pl_, wl=wl, k=k, jj=jj, h=h: e.matmul(pl_[:, :], lhsT=wl[:, k, jj * 128:(jj + 1) * 128],
                                 rhs=mTc[:, k, h * 512:(h + 1) * 512], start=(k == 0), stop=(k == 15)), reads=[wlk, 'mTc'], writes=[plk])
                        t1, t2, t3 = mt
                        T.op('dve', lambda e, pg=pg, j=j, ex=ex: e.tensor_scalar(out=t1[:], in0=pg[:, :], scalar1=bguT[:, ex, j:j + 1], scalar2=7.0, op0=ALU.add, op1=ALU.min),
                             reads=[pgk, 'bguT'], writes=['mt0'])
                        T.op('act', lambda e: e.activation(out=t2[:], in_=t1[:], func=AF.Sigmoid, scale=1.702), reads=['mt0'], writes=['mt1'])
                        T.op('dve', lambda e, pl_=pl_, j=j, ex=ex: e.tensor_scalar(out=t3[:], in0=pl_[:, :], scalar1=bguT[:, ex, 16 + j:16 + j + 1], scalar2=7.0, op0=ALU.add, op1=ALU.min),
                             reads=[plk, 'bguT'], writes=['mt2'])
                        T.op('pool', lambda e: e.tensor_scalar(out=t3[:], in0=t3[:], scalar1=-7.0, scalar2=1.0, op0=ALU.max, op1=ALU.add), reads=['mt2'], writes=['mt2'])
                        T.op('pool', lambda e: e.tensor_tensor(out=t1[:], in0=t1[:], in1=t2[:], op=ALU.mult), reads=['mt0', 'mt1'], writes=['mt0'])
                        T.op('dve', lambda e: e.tensor_tensor(out=t1[:], in0=t1[:], in1=t3[:], op=ALU.mult), reads=['mt0', 'mt2'], writes=['mt0'])
                        T.op('pool', lambda e, j=j, h=h: e.tensor_tensor(out=actb[:, j, h * 512:(h + 1) * 512], in0=t1[:], in1=gbc[:, h * 512:(h + 1) * 512], op=ALU.mult),
                             reads=['mt0', 'gbc'], writes=['actb'])
            for step in range(8):
                m0 = 2 * step
                wd, wdk = load_piece(lambda half, ex=ex, m0=m0: wdn_d[ex, half * 1024:(half + 1) * 1024, m0 * 128:m0 * 128 + 256].rearrange("(k p) f -> p k f", p=128))
                for mm in range(2):
                    m = m0 + mm
                    for h in range(2):
                        p = PS[2 + h]; pk = f'PS{2 + h}'
                        for k in range(16):
                            T.op('pe', lambda e, p=p, wd=wd, k=k, mm=mm, h=h: e.matmul(p[:, :], lhsT=wd[:, k, mm * 128:(mm + 1) * 128],
                                 rhs=actb[:, k, h * 512:(h + 1) * 512], start=(k == 0), stop=(k == 15)), reads=[wdk, 'actb'], writes=[pk])
                        T.op('dve', lambda e, p=p, m=m, h=h: e.tensor_tensor(out=acc[:, m, h * 512:(h + 1) * 512], in0=acc[:, m, h * 512:(h + 1) * 512], in1=p[:, :], op=ALU.add),
                             reads=[pk, 'acc'], writes=['acc'])
        T.dma('sp', lambda e: e.dma_start(out=G2bc, in_=grow_d[1:2, :].partition_broadcast(128)), reads=['grow_d'], writes=['actb'])
        T.dma('sp', lambda e: e.dma_start(out=finbc, in_=fing_d.partition_broadcast(128)), writes=['wst0'])
        for sub in range(TC // 128):
            r0 = t0 + sub * 128
            hxt = hx_t[sub % 2]; hk = 'actb'
            T.dma('sp', lambda e, hxt=hxt, r0=r0: e.dma_start(out=hxt, in_=hx1_d[r0:r0 + 128, :]), reads=['hx1_d'], writes=[hk])
            for q in range(4):
                p = PS[4 + q]; pk = f'PS{4 + q}'
                for jj in range(4):
                    m = q * 4 + jj
                    T.op('pe', lambda e, p=p, jj=jj, m=m, sub=sub: e.transpose(out=p[:, jj * 128:(jj + 1) * 128], in_=acc[:, m, sub * 128:(sub + 1) * 128], identity=identf[:]),
                         reads=['acc', 'identf'], writes=[pk])
                T.op('dve', lambda e, p=p, q=q: e.tensor_tensor(out=mo_t[:, q * 512:(q + 1) * 512], in0=p[:, :], in1=G2bc[:, q * 512:(q + 1) * 512], op=ALU.mult),
                     reads=[pk, 'actb'], writes=['actb'])
            T.op('pool', lambda e, hxt=hxt: e.tensor_tensor(out=hxt, in0=hxt, in1=mo_t, op=ALU.add), reads=['actb'], writes=['actb'])
            T.op('act', lambda e, hxt=hxt: e.activation(out=mo_t, in_=hxt, func=AF.Square, accum_out=sm[:, 0:1]), reads=[hk], writes=['actb', 'sm'])
            T.op('act', lambda e: e.activation(out=sm[:, 0:1], in_=sm[:, 0:1], func=AF.Sqrt, scale=1.0 / D, bias=epsT[:, 0:1]), reads=['sm', 'epsT'], writes=['sm'])
            T.op('dve', lambda e: e.reciprocal(out=sm[:, 0:1], in_=sm[:, 0:1]), reads=['sm'], writes=['sm'])
            T.op('dve', lambda e, hxt=hxt: e.scalar_tensor_tensor(out=hxt, in0=hxt, scalar=sm[:, 0:1], in1=finbc, op0=ALU.mult, op1=ALU.mult),
                 reads=[hk, 'sm', 'wst0'], writes=[hk])
            T.dma('sp', lambda e, hxt=hxt, r0=r0: e.dma_start(out=out_d[r0:r0 + 128, :], in_=hxt), reads=[hk], writes=['out_d'])
    T.pop()
    T.finish()
    return nc


def hy_consts():
    N = 16384
    bf = ml_dtypes.bfloat16
    n1 = np.arange(64)[:, None]; k1 = np.arange(65)[None, :]
    f1 = np.concatenate([np.cos(2 * np.pi * n1 * k1 / 128), -np.sin(2 * np.pi * n1 * k1 / 128)], 1)
    n2 = np.arange(128)[:, None]
    twf = np.concatenate([np.cos(2 * np.pi * n2 * k1 / N), -np.sin(2 * np.pi * n2 * k1 / N)], 1)
    a = np.arange(128)[:, None]; b = np.arange(128)[None, :]
    C = np.cos(2 * np.pi * a * b / 128); Sn = np.sin(2 * np.pi * a * b / 128)
    csn = np.stack([C, Sn, -Sn], 1)
    csh = np.zeros((128, 2, 2, 128))
    for h in range(2):
        csh[:, 0, h, 0:64] = C[:, h * 64:(h + 1) * 64]; csh[:, 0, h, 64:128] = Sn[:, h * 64:(h + 1) * 64]
        csh[:, 1, h, 0:64] = -Sn[:, h * 64:(h + 1) * 64]; csh[:, 1, h, 64:128] = C[:, h * 64:(h + 1) * 64]
    kk = np.arange(65)[:, None]; nn = np.arange(128)[None, :]
    twi = np.stack([np.cos(2 * np.pi * nn * kk / N), np.sin(2 * np.pi * nn * kk / N)], 1)
    w = np.full((65, 1), 2.0); w[0] = 1.0; w[64] = 1.0
    m1 = np.arange(64)[None, :]
    eri = np.stack([w * np.cos(2 * np.pi * m1 * kk / 128) / N, -w * np.sin(2 * np.pi * m1 * kk / 128) / N], 1)
    L = S
    t = np.linspace(0.0, 1.0, L, dtype=np.float32)[:, None]
    wv = (2.0 * np.pi * np.arange(L, dtype=np.float32)[:, None] / L).astype(np.float32)
    f = np.linspace(1e-4, 15, 16, dtype=np.float32)[None, :]
    z = np.concatenate([t, np.cos(f * wv), -np.sin(f * wv)], -1).astype(np.float32)
    import math
    max_decay = math.log(1e-2) / 0.3; min_decay = math.log(1e-2) / 1.5
    deltas = np.abs(np.linspace(min_decay, max_decay, 1024, dtype=np.float32))
    sidx = np.arange(L)
    perm = 128 * (sidx % 64) + sidx // 64
    dec = np.exp(-t * deltas[None, :])[perm]
    z = z[perm]
    return {
        "c_zT": np.ascontiguousarray(z.T), "c_dec": np.ascontiguousarray(dec.T).reshape(8, 128, L).astype(bf),
        "c_identb": np.eye(128).astype(bf), "c_f1": f1.astype(bf), "c_twf": twf.astype(np.float32),
        "c_csn": csn.astype(bf), "c_csh": csh.astype(bf), "c_twi": twi.astype(np.float32), "c_eri": eri.astype(bf),
    }


def make_in_map(b, inp, h=0):
    g = lambda k: np.asarray(inp[k], np.float32)
    cvec = np.stack([g('c')[b], g('c_ctx')], 0)
    m = {
        "x": np.ascontiguousarray(g('x')[b]),
        "ctx": np.ascontiguousarray(g('ctx')[b]),
        "cT": np.ascontiguousarray(cvec.reshape(2, 16, 128).transpose(2, 1, 0)),
        "w_mod": np.ascontiguousarray(g('w_mod')[0]),
        "bmodT": fm(g('b_mod')[0], 96),
        "n1gT": fm(g('norm1_g')[0], 16),
        "n2gT": fm(g('norm2_g')[0], 16),
        "final_g": np.ascontiguousarray(g('final_g').reshape(1, D)),
        "w_in": np.ascontiguousarray(g('w_in')[0]),
        "lcwT": np.ascontiguousarray(g('lru_conv_w')[0].reshape(4, 8, 128).transpose(2, 1, 0)),
        "lcbT": fm(g('lru_conv_b')[0], 8),
        "lru_wa": np.ascontiguousarray(g('lru_wa')[0]),
        "lru_wi": np.ascontiguousarray(g('lru_wi')[0]),
        "lbaT": np.ascontiguousarray(g('lru_ba')[0].reshape(2, 8, 128).transpose(2, 0, 1)),
        "lbiT": np.ascontiguousarray(g('lru_bi')[0].reshape(2, 8, 128).transpose(2, 0, 1)),
        "llamT": np.ascontiguousarray(g('lru_lambda')[0].reshape(2, 8, 128).transpose(2, 0, 1)),
        "identf": np.eye(128, dtype=np.float32),
        "xh": np.ascontiguousarray(g('x')[b][h * SL:(h + 1) * SL]),
        "offs": (h * SL + 512 * np.arange(8, dtype=np.int32)).reshape(1, 8).astype(np.int32),
        "hcwT": np.ascontiguousarray(g('hy_conv_w')[0].reshape(3, 24, 128).transpose(2, 1, 0)),
        "hcbT": fm(g('hy_conv_b')[0], 24),
        "hy_w1": np.ascontiguousarray(g('hy_w1')[0]), "hy_w2": np.ascontiguousarray(g('hy_w2')[0]), "hy_w3": np.ascontiguousarray(g('hy_w3')[0]),
        "hy_bT": np.ascontiguousarray(np.stack([g('hy_b1')[0], g('hy_b2')[0], g('hy_b3')[0], g('hy_freq')[0]], 1)),
        "hy_w4": np.ascontiguousarray(g('hy_w4')[0]), "hy_d": np.ascontiguousarray(g('hy_d')[0]),
        "w_out": np.ascontiguousarray(g('w_out')[0]),
        "w_gu": np.ascontiguousarray(g('exp_w_gu')[0]), "w_dn": np.ascontiguousarray(g('exp_w_down')[0]),
        "bguT": np.ascontiguousarray(g('exp_b_gu')[0].reshape(32, 32, 128).transpose(2, 0, 1)),
        "b_dn": np.ascontiguousarray(g('exp_b_down')[0]),
        "router_w": np.ascontiguousarray(g('router_w')[0]), "router_b": np.ascontiguousarray(g('router_b')[0].reshape(1, 32)),
        "gnT": np.ascontiguousarray(np.concatenate([fm(g('gn_lru')[0], 8), fm(g('gn_hy')[0], 8)], 1)),
    }
    m.update(hy_consts())
    return m


def kernel(**inputs):
    nc = build(dbg=False)
    shared = {}
    in_maps = []
    for c in range(NCORES):
        b, h = c // 2, c % 2
        if h == 0:
            shared = make_in_map(b, inputs, 0)
            in_maps.append(shared)
        else:
            m = dict(shared)
            m["xh"] = np.ascontiguousarray(np.asarray(inputs['x'], np.float32)[b][SL:2 * SL])
            m["offs"] = (SL + 512 * np.arange(8, dtype=np.int32)).reshape(1, 8).astype(np.int32)
            in_maps.append(m)
    res = run_bass_kernel_spmd(nc, in_maps, core_ids=list(range(NCORES)))
    out = np.empty((4, S, D), np.float32)
    for c in range(NCORES):
        out[c // 2, (c % 2) * SL:(c % 2 + 1) * SL] = np.asarray(res.results[c]["out"], np.float32)
    return out
```

```python
import numpy as np
from contextlib import ExitStack
import ml_dtypes
import concourse.bass as bass
import concourse.mybir as mybir
from concourse.bass_utils import run_bass_kernel_spmd

F32 = mybir.dt.float32
BF16 = mybir.dt.bfloat16
ALU = mybir.AluOpType
AF = mybir.ActivationFunctionType

S = 8192
CTX = 256
D = 2048
ST = S + CTX
NCORES = 8
SL = S // 2
EPS = 1e-6


class Trk:
    NDS = 8

    def __init__(self, nc):
        self.nc = nc
        self.es = ExitStack()
        self.engs = {'pe': nc.tensor, 'act': nc.scalar, 'dve': nc.vector, 'pool': nc.gpsimd, 'sp': nc.sync}
        self.sem = {e: self.es.enter_context(nc.semaphore('c_' + e)) for e in self.engs}
        self.cnt = {e: 0 for e in self.engs}
        self.dsem = {q: [self.es.enter_context(nc.semaphore(f'd_{q}{i}')) for i in range(self.NDS)]
                     for q in ('sp', 'pool')}
        self.duse = {q: [0] * self.NDS for q in self.dsem}
        self.drr = {q: 0 for q in self.dsem}
        self.waited = {e: {} for e in self.engs}
        self.lastw = {}
        self.readers = {}
        self.ntile = 0
        self.scopes = [self.es]

    def tile(self, shape, dt, name=None):
        self.ntile += 1
        return self.scopes[-1].enter_context(self.nc.sbuf_tensor(f'{name or "t"}_{self.ntile}', list(shape), dt))

    def ptile(self, shape, dt, name=None):
        self.ntile += 1
        return self.scopes[-1].enter_context(self.nc.psum_tensor(f'{name or "p"}_{self.ntile}', list(shape), dt))

    def push(self):
        self.scopes.append(ExitStack())

    def pop(self):
        self.barrier()
        self.scopes.pop().close()

    def _need(self, E, tok, waits):
        if tok is None:
            return
        kind, a, v = tok
        if kind == 'e':
            if a == E and E == 'pe':
                return
            key = ('e', a)
        else:
            key = ('d', a[0], a[1])
        if self.waited[E].get(key, -1) >= v:
            return
        self.waited[E][key] = v
        waits.append((key, v))

    def _deps(self, E, reads, writes):
        waits = []
        for b in reads:
            self._need(E, self.lastw.get(b), waits)
        for b in writes:
            self._need(E, self.lastw.get(b), waits)
            for t in self.readers.get(b, ()):
                self._need(E, t, waits)
        return waits

    def _commit(self, tok, reads, writes):
        for b in reads:
            self.readers.setdefault(b, []).append(tok)
        for b in writes:
            self.lastw[b] = tok
            self.readers[b] = []

    def _semof(self, key):
        return self.sem[key[1]] if key[0] == 'e' else self.dsem[key[1]][key[2]]

    def _emit(self, E, waits, fn, inc):
        eng = self.engs[E]
        for key, v in waits:
            eng.wait_ge(self._semof(key), v)
        if fn is None:
            return
        ins = fn(eng)
        if inc[0] == 'e':
            ins.then_inc(self.sem[E], 1)
        else:
            ins.then_inc(self.dsem[inc[1]][inc[2]], 16)

    def op(self, E, fn, reads=(), writes=()):
        waits = self._deps(E, reads, writes)
        self.cnt[E] += 1
        tok = ('e', E, self.cnt[E])
        self._emit(E, waits, fn, ('e', E))
        self._commit(tok, reads, writes)

    def dma(self, Q, fn, reads=(), writes=()):
        r = self.drr[Q]
        self.drr[Q] = (r + 1) % self.NDS
        waits = self._deps(Q, reads, writes)
        prev = self.duse[Q][r]
        if prev > 0:
            self._need(Q, ('d', (Q, r), 16 * prev), waits)
        self.duse[Q][r] += 1
        tok = ('d', (Q, r), 16 * self.duse[Q][r])
        self._emit(Q, waits, fn, ('d', Q, r))
        self._commit(tok, reads, writes)

    def barrier(self):
        for E in self.engs:
            waits = []
            for E2 in self.engs:
                if E2 != E and self.cnt[E2] > 0:
                    self._need(E, ('e', E2, self.cnt[E2]), waits)
            for q in self.dsem:
                for r in range(self.NDS):
                    if self.duse[q][r] > 0:
                        self._need(E, ('d', (q, r), 16 * self.duse[q][r]), waits)
            self._emit(E, waits, None, None)
        self.lastw = {}
        self.readers = {}

    def finish(self):
        self.barrier()
        while self.scopes:
            self.scopes.pop().close()


def fm(v, ntiles):
    return np.ascontiguousarray(np.asarray(v, np.float32).reshape(ntiles, 128).T)


def build(dbg=False):
    nc = bass.Bass("TRN2", target_bir_lowering=False)
    T = Trk(nc)

    def din(name, shape, dt=F32):
        return nc.dram_tensor(name, list(shape), dt, kind="ExternalInput").ap()

    def dout(name, shape, dt=F32):
        return nc.dram_tensor(name, list(shape), dt, kind="ExternalOutput").ap()

    def dscr(name, shape, dt):
        return nc.dram_tensor(name, list(shape), dt).ap()

    x_d = din("x", [S, D])
    ctx_d = din("ctx", [CTX, D])
    cT_d = din("cT", [128, 16, 2])
    wmod_d = din("w_mod", [D, 6 * D])
    bmodT_d = din("bmodT", [128, 96])
    n1gT_d = din("n1gT", [128, 16])
    n2gT_d = din("n2gT", [128, 16])
    fing_d = din("final_g", [1, D])
    win_d = din("w_in", [D, 5120])
    lcw_d = din("lcwT", [128, 8, 4])
    lcb_d = din("lcbT", [128, 8])
    lwa_d = din("lru_wa", [2, 8, 128, 128])
    lwi_d = din("lru_wi", [2, 8, 128, 128])
    lba_d = din("lbaT", [128, 2, 8])
    lbi_d = din("lbiT", [128, 2, 8])
    llam_d = din("llamT", [128, 2, 8])
    identf_d = din("identf", [128, 128])
    out_d = dout("out", [SL, D])
    xh_d = din("xh", [SL, D])
    offs_d = din("offs", [1, 8], mybir.dt.int32)
    dbg_d = {}
    if dbg:
        dbg_d['modT'] = dout("dbg_modT", [128, 96, 2])
        dbg_d['aT'] = dout("dbg_aT", [16, 128, ST], BF16)
        dbg_d['mix'] = dout("dbg_mix", [16, 128, S], BF16)
        dbg_d['kf'] = dout("dbg_kf", [2, 8, 128, 2, 65, 128], BF16)
    aT_d = dbg_d['aT'] if dbg else dscr("aT_s", [16, 128, ST], BF16)
    mix_d = dbg_d['mix'] if dbg else dscr("mix_s", [16, 128, S], BF16)
    kf_d = dbg_d['kf'] if dbg else dscr("kf_s", [2, 8, 128, 2, 65, 128], BF16)
    grow_d = dscr("grow_s", [2, D], F32)
    if dbg:
        dbg_d['hx1'] = dout("dbg_hx1", [SL, D]); dbg_d['mT'] = dout("dbg_mT", [16, 128, SL], BF16)
    hx1_d = dbg_d['hx1'] if dbg else dscr("hx1_s", [SL, D], F32)
    mT_d = dbg_d['mT'] if dbg else dscr("mT_s", [16, 128, SL], BF16)
    wout_d = din("w_out", [D, D]); gnT_d = din("gnT", [128, 16])
    wgu_d = din("w_gu", [32, D, 2 * D]); wdn_d = din("w_dn", [32, D, D])
    wbf_l = [dscr(f"wbf_s{i}", [8 * 24, 128, 16 * 256], BF16) for i in range(4)]
    bguT_d = din("bguT", [128, 32, 32]); bd_d = din("b_dn", [32, D]); rw_d = din("router_w", [D, 32]); rb_d = din("router_b", [1, 32])
    if dbg:
        dbg_d['gT'] = dout("dbg_gT", [32, 1024])
    hcw_d = din("hcwT", [128, 24, 3]); hcb_d = din("hcbT", [128, 24])
    hw1_d = din("hy_w1", [33, 64]); hw2_d = din("hy_w2", [64, 64]); hw3_d = din("hy_w3", [64, 64])
    hb_d = din("hy_bT", [64, 4])
    hw4_d = din("hy_w4", [64, 4096]); hyd_d = din("hy_d", [2, 1024])
    zT_d = din("c_zT", [33, S]); dec_d = din("c_dec", [8, 128, S], BF16)
    identb_d = din("c_identb", [128, 128], BF16); f1_d = din("c_f1", [64, 130], BF16)
    twf_d = din("c_twf", [128, 130]); csn_d = din("c_csn", [128, 3, 128], BF16)
    csh_d = din("c_csh", [128, 2, 2, 128], BF16); twi_d = din("c_twi", [65, 2, 128]); eri_d = din("c_eri", [65, 2, 64], BF16)

    identf = T.tile([128, 128], F32, 'identf')
    T.dma('sp', lambda e: e.dma_start(out=identf[:], in_=identf_d), writes=['identf'])
    modT = T.tile([128, 96, 2], F32, 'modT')
    epsT = T.tile([128, 1], F32, 'epsT')
    oneT = T.tile([128, 1], F32, 'oneT')
    T.op('pool', lambda e: e.memset(epsT[:], EPS), writes=['epsT'])
    T.op('pool', lambda e: e.memset(oneT[:], 1.0), writes=['oneT'])
    A1 = T.tile([128, 16, 2], F32, 'A1')
    A2 = T.tile([128, 16, 2], F32, 'A2')

    T.push()
    cT = T.tile([128, 16, 2], F32, 'cT')
    sT = T.tile([128, 16, 2], F32, 'sT')
    bmodT = T.tile([128, 96], F32, 'bmodT')
    n1gT = T.tile([128, 16], F32, 'n1gT')
    T.dma('sp', lambda e: e.dma_start(out=cT[:], in_=cT_d), writes=['cT'])
    T.dma('sp', lambda e: e.dma_start(out=bmodT[:], in_=bmodT_d), writes=['bmodT'])
    T.dma('sp', lambda e: e.dma_start(out=n1gT[:], in_=n1gT_d), writes=['n1gT'])
    n2gT = T.tile([128, 16], F32, 'n2gT')
    T.dma('sp', lambda e: e.dma_start(out=n2gT[:], in_=n2gT_d), writes=['n2gT'])
    T.op('act', lambda e: e.activation(out=sT[:], in_=cT[:], func=AF.Silu), reads=['cT'], writes=['sT'])
    wm = [T.tile([128, 16, 512], F32, f'wm{i}') for i in range(2)]
    pm = [T.ptile([128, 4, 2], F32, f'pm{i}') for i in range(2)]
    for ch in range(24):
        w = wm[ch % 2]
        wk = f'wm{ch % 2}'
        pk = f'pm{ch % 2}'
        p = pm[ch % 2]
        src = wmod_d[:, ch * 512:(ch + 1) * 512].rearrange("(k p) f -> p k f", p=128)
        T.dma('sp', lambda e, w=w, src=src: e.dma_start(out=w[:], in_=src), writes=[wk])
        for j in range(4):
            for k in range(16):
                T.op('pe', lambda e, p=p, w=w, j=j, k=k: e.matmul(
                    p[:, j, :], lhsT=w[:, k, j * 128:(j + 1) * 128], rhs=sT[:, k, :],
                    start=(k == 0), stop=(k == 15)), reads=[wk, 'sT'], writes=[pk])
        for j in range(4):
            t = ch * 4 + j
            T.op('dve', lambda e, p=p, j=j, t=t: e.tensor_scalar(
                out=modT[:, t, :], in0=p[:, j, :], scalar1=bmodT[:, t:t + 1], scalar2=None, op0=ALU.add),
                reads=[pk, 'bmodT'], writes=['modT'])
    T.op('dve', lambda e: e.tensor_scalar(out=A1[:], in0=modT[:, 16:32, :], scalar1=1.0, scalar2=None, op0=ALU.add),
         reads=['modT'], writes=['A1'])
    for r in range(2):
        T.op('dve', lambda e, r=r: e.tensor_tensor(out=A1[:, :, r], in0=A1[:, :, r], in1=n1gT[:], op=ALU.mult),
             reads=['A1', 'n1gT'], writes=['A1'])
    T.op('dve', lambda e: e.tensor_scalar(out=A2[:], in0=modT[:, 64:80, :], scalar1=1.0, scalar2=None, op0=ALU.add),
         reads=['modT'], writes=['A2'])
    for r in range(2):
        T.op('dve', lambda e, r=r: e.tensor_tensor(out=A2[:, :, r], in0=A2[:, :, r], in1=n2gT[:], op=ALU.mult),
             reads=['A2', 'n2gT'], writes=['A2'])
    g12 = T.tile([128, 2, 16], F32, 'g12')
    T.op('dve', lambda e: e.tensor_copy(out=g12[:, 0, :], in_=modT[:, 32:48, 0]), reads=['modT'], writes=['g12'])
    T.op('dve', lambda e: e.tensor_copy(out=g12[:, 1, :], in_=modT[:, 80:96, 0]), reads=['modT'], writes=['g12'])
    T.dma('sp', lambda e: e.dma_start(out=grow_d.rearrange("r (t p) -> p r t", p=128), in_=g12[:], allow_slow_non_contiguous=True),
          reads=['g12'], writes=['grow_d'])
    if dbg:
        T.dma('sp', lambda e: e.dma_start(out=dbg_d['modT'], in_=modT[:]), reads=['modT'])
    T.pop()

    def norm_to_T(X, xk, A, B, row, dst, dk, col0, scr, ptr, pk_list, it, abk=('A1', 'modT')):
        junk, ss, XN = scr
        i2 = it % 2
        T.op('act', lambda e: e.activation(out=junk[i2][:], in_=X[:], func=AF.Square, accum_out=ss[i2][:]),
             reads=[xk], writes=[f'junk{i2}', f'ss{i2}'])
        T.op('act', lambda e: e.activation(out=ss[i2][:], in_=ss[i2][:], func=AF.Sqrt, scale=1.0 / D, bias=epsT[:, 0:1]),
             reads=[f'ss{i2}', 'epsT'], writes=[f'ss{i2}'])
        T.op('dve', lambda e: e.reciprocal(out=ss[i2][:], in_=ss[i2][:]), reads=[f'ss{i2}'], writes=[f'ss{i2}'])
        T.op('pool', lambda e: e.tensor_scalar(out=XN[i2][:], in0=X[:], scalar1=ss[i2][:, 0:1], scalar2=None, op0=ALU.mult),
             reads=[xk, f'ss{i2}'], writes=[f'XN{i2}'])
        for q in range(4):
            p = ptr[q % 2]
            pk = pk_list[q % 2]
            for jj in range(4):
                j = q * 4 + jj
                T.op('pe', lambda e, p=p, jj=jj, j=j: e.transpose(
                    out=p[:, jj * 128:(jj + 1) * 128], in_=XN[i2][:, j * 128:(j + 1) * 128], identity=identf[:]),
                    reads=[f'XN{i2}', 'identf'], writes=[pk])
            for jj in range(4):
                j = q * 4 + jj
                if jj % 2 == 0:
                    T.op('dve', lambda e, p=p, jj=jj, j=j: e.tensor_scalar(
                        out=dst[:, j, col0:col0 + 128], in0=p[:, jj * 128:(jj + 1) * 128],
                        scalar1=A[:, j, row:row + 1], scalar2=B[:, j, row:row + 1], op0=ALU.mult, op1=ALU.add),
                        reads=[pk] + list(abk), writes=[dk])
                else:
                    T.op('act', lambda e, p=p, jj=jj, j=j: e.activation(
                        out=dst[:, j, col0:col0 + 128], in_=p[:, jj * 128:(jj + 1) * 128], func=AF.Identity,
                        scale=A[:, j, row:row + 1], bias=B[:, j, row:row + 1]),
                        reads=[pk] + list(abk), writes=[dk])

    T.push()
    Xt = [T.tile([128, D], F32, f'X{i}') for i in range(2)]
    scr = ([T.tile([128, D], BF16, f'junk{i}') for i in range(2)],
           [T.tile([128, 1], F32, f'ss{i}') for i in range(2)],
           [T.tile([128, D], F32, f'XN{i}') for i in range(2)])
    ptr = [T.ptile([128, 512], F32, f'ptr{i}') for i in range(2)]
    aTg = [T.tile([128, 16, 512], BF16, f'aTg{i}') for i in range(2)]
    it = 0
    for g in range(17):
        nsub = 4 if g < 16 else 2
        ag = aTg[g % 2]
        agk = f'aTg{g % 2}'
        for sub in range(nsub):
            X = Xt[it % 2]
            xk = f'X{it % 2}'
            if g < 16:
                src = x_d[g * 512 + sub * 128: g * 512 + (sub + 1) * 128, :]
                row = 0
            else:
                src = ctx_d[sub * 128:(sub + 1) * 128, :]
                row = 1
            T.dma('sp', lambda e, X=X, src=src: e.dma_start(out=X[:], in_=src), writes=[xk])
            norm_to_T(X, xk, A1, modT[:, 0:16, :], row, ag, agk, sub * 128, scr, ptr, ['ptr0', 'ptr1'], it)
            it += 1
        ncol = nsub * 128
        dst = aT_d[:, :, g * 512: g * 512 + ncol].rearrange("j p t -> p j t")
        T.dma('sp', lambda e, ag=ag, dst=dst, ncol=ncol: e.dma_start(out=dst, in_=ag[:, :, 0:ncol]), reads=[agk], writes=['aT_d'])
    T.pop()

    def rev(ap2d, n):
        a = ap2d
        return bass.AP(tensor=a.tensor, offset=a.offset + (n - 1), ap=[list(a.ap[0]), [-1, n]])

    T.push()
    lcw = T.tile([128, 8, 4], F32, 'lcw'); lcb = T.tile([128, 8], F32, 'lcb')
    lba = T.tile([128, 2, 8], F32, 'lba'); lbi = T.tile([128, 2, 8], F32, 'lbi'); sca = T.tile([128, 2, 8], F32, 'sca')
    for t_, d_, k_ in ((lcw, lcw_d, 'lcw'), (lcb, lcb_d, 'lcb'), (lba, lba_d, 'lba'), (lbi, lbi_d, 'lbi'), (sca, llam_d, 'sca')):
        T.dma('sp', lambda e, t_=t_, d_=d_: e.dma_start(out=t_[:], in_=d_), writes=[k_])
    T.op('act', lambda e: e.activation(out=sca[:], in_=sca[:], func=AF.Exp, scale=-1.0), reads=['sca'], writes=['sca'])
    T.op('act', lambda e: e.activation(out=sca[:], in_=sca[:], func=AF.Ln, bias=oneT[:, 0:1]), reads=['sca', 'oneT'], writes=['sca'])
    T.op('dve', lambda e: e.tensor_scalar(out=sca[:], in0=sca[:], scalar1=-8.0, scalar2=None, op0=ALU.mult), reads=['sca'], writes=['sca'])

    RA = T.tile([128, ST], F32, 'RA'); UB = T.tile([128, ST], F32, 'UB')
    ub = T.tile([128, ST], BF16, 'ub'); gg = T.tile([128, S], BF16, 'gg')
    hs = T.tile([128, ST], F32, 'hs'); h2 = T.tile([128, ST], F32, 'h2')
    wrb = T.tile([128, 16, 128], BF16, 'wrb'); wgb = T.tile([128, 16, 128], BF16, 'wgb')
    gst = T.tile([128, 128], F32, 'gst')
    wab = [T.tile([128, 128], BF16, f'wab{d}') for d in range(2)]
    wib = [T.tile([128, 128], BF16, f'wib{d}') for d in range(2)]
    ag2 = [T.tile([128, 16, 512], BF16, 'ag0')]
    pp = [T.ptile([128, 512], F32, f'pp{i}') for i in range(4)]
    tmp = [T.tile([128, 512], F32, f'tmp{i}') for i in range(4)]

    for blk in range(8):
        for (wt, wk, c0) in ((wrb, 'wrb', blk * 128), (wgb, 'wgb', 1024 + blk * 128)):
            src = win_d[:, c0:c0 + 128].rearrange("(k p) f -> p k f", p=128)
            T.dma('pool', lambda e, src=src, wt=wt: e.dma_start(out=wt[:], in_=src), writes=[wk])
        for d in range(2):
            for (wt, wk, srcw) in ((wab[d], f'wab{d}', lwa_d), (wib[d], f'wib{d}', lwi_d)):
                T.dma('sp', lambda e, srcw=srcw, d=d: e.dma_start(out=gst[:], in_=srcw[d, blk]), writes=['gst'])
                T.op('dve', lambda e, wt=wt: e.tensor_copy(out=wt[:], in_=gst[:]), reads=['gst'], writes=[wk])
        for g in range(17):
            ncol = 512 if g < 16 else 256
            a = ag2[0]; ak = 'ag0'
            src = aT_d[:, :, g * 512: g * 512 + ncol].rearrange("j p t -> p j t")
            T.dma('sp', lambda e, a=a, src=src, ncol=ncol: e.dma_start(out=a[:, :, 0:ncol], in_=src), reads=['aT_d'], writes=[ak])
            p0 = pp[(2 * g) % 4]; p0k = f'pp{(2 * g) % 4}'
            for k in range(16):
                T.op('pe', lambda e, p0=p0, a=a, k=k, ncol=ncol: e.matmul(p0[:, 0:ncol], lhsT=wrb[:, k, :], rhs=a[:, k, 0:ncol],
                     start=(k == 0), stop=(k == 15)), reads=['wrb', ak], writes=[p0k])
            T.op('act', lambda e, p0=p0, g=g, ncol=ncol: e.activation(out=RA[:, g * 512: g * 512 + ncol], in_=p0[:, 0:ncol], func=AF.Copy),
                 reads=[p0k], writes=['RA'])
            if g < 16:
                p1 = pp[(2 * g + 1) % 4]; p1k = f'pp{(2 * g + 1) % 4}'
                for k in range(16):
                    T.op('pe', lambda e, p1=p1, a=a, k=k: e.matmul(p1[:, :], lhsT=wgb[:, k, :], rhs=a[:, k, :],
                         start=(k == 0), stop=(k == 15)), reads=['wgb', ak], writes=[p1k])
                T.op('act', lambda e, p1=p1, g=g: e.activation(out=gg[:, g * 512:(g + 1) * 512], in_=p1[:, :], func=AF.Gelu),
                     reads=[p1k], writes=['gg'])
        for (lo, n) in ((0, S), (S, CTX)):
            T.op('dve', lambda e, lo=lo, n=n: e.tensor_scalar(out=UB[:, lo:lo + n], in0=RA[:, lo:lo + n],
                 scalar1=lcw[:, blk, 2:3], scalar2=lcb[:, blk:blk + 1], op0=ALU.mult, op1=ALU.add),
                 reads=['RA', 'lcw', 'lcb'], writes=['UB'])
            for (tap, sh) in ((0, -2), (1, -1), (3, 1)):
                if sh < 0:
                    o_lo, o_n, i_lo = lo - sh, n + sh, lo
                else:
                    o_lo, o_n, i_lo = lo, n - sh, lo + sh
                T.op('dve', lambda e, tap=tap, o_lo=o_lo, o_n=o_n, i_lo=i_lo: e.scalar_tensor_tensor(
                    out=UB[:, o_lo:o_lo + o_n], in0=RA[:, i_lo:i_lo + o_n], scalar=lcw[:, blk, tap:tap + 1],
                    in1=UB[:, o_lo:o_lo + o_n], op0=ALU.mult, op1=ALU.add), reads=['RA', 'UB', 'lcw'], writes=['UB'])
        T.op('pool', lambda e: e.tensor_copy(out=ub[:], in_=UB[:]), reads=['UB'], writes=['ub'])
        for d in range(2):
            for g in range(17):
                ncol = 512 if g < 16 else 256
                c0 = g * 512
                pa = pp[(2 * g) % 4]; pak = f'pp{(2 * g) % 4}'
                pi_ = pp[(2 * g + 1) % 4]; pik = f'pp{(2 * g + 1) % 4}'
                T.op('pe', lambda e, pa=pa, c0=c0, ncol=ncol, d=d: e.matmul(pa[:, 0:ncol], lhsT=wab[d][:], rhs=ub[:, c0:c0 + ncol],
                     start=True, stop=True), reads=[f'wab{d}', 'ub'], writes=[pak])
                T.op('pe', lambda e, pi_=pi_, c0=c0, ncol=ncol, d=d: e.matmul(pi_[:, 0:ncol], lhsT=wib[d][:], rhs=ub[:, c0:c0 + ncol],
                     start=True, stop=True), reads=[f'wib{d}', 'ub'], writes=[pik])
                t0, t1, t2, t3 = tmp
                T.op('act', lambda e, pa=pa, ncol=ncol, d=d: e.activation(out=t0[:, 0:ncol], in_=pa[:, 0:ncol], func=AF.Sigmoid,
                     bias=lba[:, d, blk:blk + 1]), reads=[pak, 'lba'], writes=['tmp0'])
                T.op('act', lambda e, pi_=pi_, ncol=ncol, d=d: e.activation(out=t1[:, 0:ncol], in_=pi_[:, 0:ncol], func=AF.Sigmoid,
                     bias=lbi[:, d, blk:blk + 1]), reads=[pik, 'lbi'], writes=['tmp1'])
                T.op('act', lambda e, c0=c0, ncol=ncol, d=d: e.activation(out=RA[:, c0:c0 + ncol], in_=t0[:, 0:ncol], func=AF.Exp,
                     scale=sca[:, d, blk:blk + 1]), reads=['tmp0', 'sca'], writes=['RA'])
                T.op('act', lambda e, c0=c0, ncol=ncol: e.activation(out=t2[:, 0:ncol], in_=RA[:, c0:c0 + ncol], func=AF.Square),
                     reads=['RA'], writes=['tmp2'])
                T.op('act', lambda e, ncol=ncol: e.activation(out=t2[:, 0:ncol], in_=t2[:, 0:ncol], func=AF.Sqrt, scale=-1.0,
                     bias=oneT[:, 0:1]), reads=['tmp2', 'oneT'], writes=['tmp2'])
                T.op('dve', lambda e, c0=c0, ncol=ncol: e.tensor_tensor(out=t3[:, 0:ncol], in0=t1[:, 0:ncol], in1=ub[:, c0:c0 + ncol], op=ALU.mult),
                     reads=['tmp1', 'ub'], writes=['tmp3'])
                T.op('pool', lambda e, c0=c0, ncol=ncol: e.tensor_tensor(out=UB[:, c0:c0 + ncol], in0=t3[:, 0:ncol], in1=t2[:, 0:ncol], op=ALU.mult),
                     reads=['tmp3', 'tmp2'], writes=['UB'])
            H = hs if d == 0 else h2
            hk = 'hs' if d == 0 else 'h2'
            if d == 0:
                T.op('dve', lambda e: e.tensor_tensor_scan(out=H[:, S:ST], data0=RA[:, S:ST], data1=UB[:, S:ST], initial=0.0,
                     op0=ALU.mult, op1=ALU.add), reads=['RA', 'UB'], writes=[hk])
                T.op('dve', lambda e: e.tensor_tensor_scan(out=H[:, 0:S], data0=RA[:, 0:S], data1=UB[:, 0:S], initial=H[:, ST - 1:ST],
                     op0=ALU.mult, op1=ALU.add), reads=['RA', 'UB', hk], writes=[hk])
            else:
                T.op('dve', lambda e: e.tensor_tensor_scan(out=rev(H[:, S:ST], CTX), data0=rev(RA[:, S:ST], CTX), data1=rev(UB[:, S:ST], CTX),
                     initial=0.0, op0=ALU.mult, op1=ALU.add), reads=['RA', 'UB'], writes=[hk])
                T.op('dve', lambda e: e.tensor_tensor_scan(out=rev(H[:, 0:S], S), data0=rev(RA[:, 0:S], S), data1=rev(UB[:, 0:S], S),
                     initial=H[:, S:S + 1], op0=ALU.mult, op1=ALU.add), reads=['RA', 'UB', hk], writes=[hk])
        T.op('pool', lambda e: e.tensor_tensor(out=hs[:, 0:S], in0=hs[:, 0:S], in1=h2[:, 0:S], op=ALU.add), reads=['hs', 'h2'], writes=['hs'])
        T.op('dve', lambda e: e.tensor_tensor(out=hs[:, 0:S], in0=hs[:, 0:S], in1=gg[:, :], op=ALU.mult), reads=['hs', 'gg'], writes=['hs'])
        T.op('pool', lambda e: e.tensor_copy(out=gg[:, :], in_=hs[:, 0:S]), reads=['hs'], writes=['gg'])
        T.dma('sp', lambda e, blk=blk: e.dma_start(out=mix_d[blk], in_=gg[:, :]), reads=['gg'], writes=['mix_d'])
    T.pop()


    def bc_mid(ap2d, n):
        a = ap2d
        return bass.AP(tensor=a.tensor, offset=a.offset, ap=[list(a.ap[0]), [0, n], list(a.ap[1])])

    T.push()
    identb = T.tile([128, 128], BF16, 'identb'); f1m = T.tile([64, 130], BF16, 'f1m'); twf = T.tile([128, 130], F32, 'twf')
    csn = T.tile([128, 3, 128], BF16, 'csn'); csh = T.tile([128, 2, 2, 128], BF16, 'csh')
    twi = T.tile([65, 2, 128], F32, 'twi'); eri = T.tile([65, 2, 64], BF16, 'eri')
    for t_, d_, k_ in ((identb, identb_d, 'identb'), (f1m, f1_d, 'f1m'), (twf, twf_d, 'twf'), (csn, csn_d, 'csn'),
                       (csh, csh_d, 'csh'), (twi, twi_d, 'twi'), (eri, eri_d, 'eri')):
        T.dma('sp', lambda e, t_=t_, d_=d_: e.dma_start(out=t_[:], in_=d_), writes=[k_])
    UC = T.tile([128, 16384], BF16, 'UC')
    AY = T.tile([128, 2, 65, 128], BF16, 'AY')
    KF = T.tile([128, 2, 65, 128], BF16, 'KF')
    U = UC[0:64, :].rearrange("p (c n) -> p c n", c=128)
    CPP = UC[0:65, :].rearrange("p (r n c) -> p r n c", r=2, n=64)
    AYK = [f'AY{i}' for i in range(17)]
    KGS = [(4 * i, 4) for i in range(16)] + [(64, 1)]
    ftmp = [T.tile([128, 512], F32, f'ft{i}') for i in range(4)]

    def fft_forward(u, uk, mode, dbc=None):
        T.push()
        pt = [T.ptile([64, 8, 128], BF16, f'pt{i}') for i in range(2)]
        pa = [T.ptile([128, 3, 130], F32, f'pa{i}') for i in range(2)]
        px = [T.ptile([128, 512], F32, f'px{i}') for i in range(2)]
        tw = [T.tile([128, 3, 65], F32, f'tw{i}') for i in range(4)]
        for q in range(16):
            p = pt[q % 2]; pk = f'pt{q % 2}'
            for i in range(8):
                n2 = q * 8 + i
                T.op('pe', lambda e, p=p, i=i, n2=n2: e.transpose(out=p[:, i, :], in_=u[:, n2 * 64:(n2 + 1) * 64], identity=identb[:]),
                     reads=[uk, 'identb'], writes=[pk])
            dst = U[:, :, q * 8:(q + 1) * 8].rearrange("p c n -> p n c")
            if q % 2 == 0:
                T.op('act', lambda e, p=p, dst=dst: e.activation(out=dst, in_=p[:, :, :], func=AF.Copy), reads=[pk], writes=['UC'])
            else:
                T.op('dve', lambda e, p=p, dst=dst: e.tensor_copy(out=dst, in_=p[:, :, :]), reads=[pk], writes=['UC'])
        ngr = 43
        for gi in range(ngr):
            c0 = gi * 3; nch = min(3, 128 - c0)
            p = pa[gi % 2]; pk = f'pa{gi % 2}'
            for i in range(nch):
                T.op('pe', lambda e, p=p, i=i, c=c0 + i: e.matmul(p[:, i, :], lhsT=U[:, c, :], rhs=f1m[:, :], start=True, stop=True),
                     reads=['UC', 'f1m'], writes=[pk])
            Pr = p[:, 0:nch, 0:65]; Pi = p[:, 0:nch, 65:130]
            twr = bc_mid(twf[:, 0:65], nch); twim = bc_mid(twf[:, 65:130], nch)
            t1, t2, t3, t4 = [t[:, 0:nch, :] for t in tw]
            T.op('dve', lambda e, t1=t1, Pr=Pr, twr=twr: e.tensor_tensor(out=t1, in0=Pr, in1=twr, op=ALU.mult), reads=[pk, 'twf'], writes=['tw0'])
            T.op('dve', lambda e, t2=t2, Pi=Pi, twim=twim: e.tensor_tensor(out=t2, in0=Pi, in1=twim, op=ALU.mult), reads=[pk, 'twf'], writes=['tw1'])
            T.op('dve', lambda e, t3=t3, Pr=Pr, twim=twim: e.tensor_tensor(out=t3, in0=Pr, in1=twim, op=ALU.mult), reads=[pk, 'twf'], writes=['tw2'])
            T.op('dve', lambda e, t4=t4, Pi=Pi, twr=twr: e.tensor_tensor(out=t4, in0=Pi, in1=twr, op=ALU.mult), reads=[pk, 'twf'], writes=['tw3'])
            dr = AY[:, 0, :, c0:c0 + nch].rearrange("p k c -> p c k"); di = AY[:, 1, :, c0:c0 + nch].rearrange("p k c -> p c k")
            T.op('pool', lambda e, dr=dr, t1=t1, t2=t2: e.tensor_tensor(out=dr, in0=t1, in1=t2, op=ALU.subtract), reads=['tw0', 'tw1'], writes=AYK)
            T.op('pool', lambda e, di=di, t3=t3, t4=t4: e.tensor_tensor(out=di, in0=t3, in1=t4, op=ALU.add), reads=['tw2', 'tw3'], writes=AYK)
        for gi, (k0, nk) in enumerate(KGS):
            ncol = nk * 128
            pr = px[0]; prk = 'px0'
            pi_ = px[1]; pik = 'px1'
            Ar = AY[:, 0, k0:k0 + nk, :]; Ai = AY[:, 1, k0:k0 + nk, :]
            ak = AYK[gi]
            T.op('pe', lambda e, pr=pr, Ar=Ar, ncol=ncol: e.matmul(pr[:, 0:ncol], lhsT=csn[:, 0, :], rhs=Ar, start=True, stop=False), reads=[ak, 'csn'], writes=[prk])
            T.op('pe', lambda e, pr=pr, Ai=Ai, ncol=ncol: e.matmul(pr[:, 0:ncol], lhsT=csn[:, 1, :], rhs=Ai, start=False, stop=True), reads=[ak, 'csn'], writes=[prk])
            T.op('pe', lambda e, pi_=pi_, Ai=Ai, ncol=ncol: e.matmul(pi_[:, 0:ncol], lhsT=csn[:, 0, :], rhs=Ai, start=True, stop=False), reads=[ak, 'csn'], writes=[pik])
            T.op('pe', lambda e, pi_=pi_, Ar=Ar, ncol=ncol: e.matmul(pi_[:, 0:ncol], lhsT=csn[:, 2, :], rhs=Ar, start=False, stop=True), reads=[ak, 'csn'], writes=[pik])
            Xr = pr[:, 0:ncol].rearrange("p (k c) -> p k c", c=128); Xi = pi_[:, 0:ncol].rearrange("p (k c) -> p k c", c=128)
            Kr = KF[:, 0, k0:k0 + nk, :]; Ki = KF[:, 1, k0:k0 + nk, :]
            if mode == 'kf0':
                T.op('dve', lambda e, Kr=Kr, Xr=Xr, nk=nk: e.tensor_tensor(out=Kr, in0=Xr, in1=bc_mid(dbc[:, :], nk), op=ALU.add), reads=[prk, 'dbc'], writes=['KF'])
                T.op('act', lambda e, Ki=Ki, Xi=Xi: e.activation(out=Ki, in_=Xi, func=AF.Copy), reads=[pik], writes=['KF'])
            elif mode == 'kf1':
                T.op('dve', lambda e, Kr=Kr, Xr=Xr: e.tensor_tensor(out=Kr, in0=Kr, in1=Xr, op=ALU.add), reads=[prk, 'KF'], writes=['KF'])
                T.op('dve', lambda e, Ki=Ki, Xi=Xi: e.tensor_tensor(out=Ki, in0=Ki, in1=Xi, op=ALU.subtract), reads=[pik, 'KF'], writes=['KF'])
            else:
                f1_, f2_, f3_, f4_ = [t[:, 0:ncol].rearrange("p (k c) -> p k c", c=128) for t in ftmp]
                T.op('dve', lambda e, f1_=f1_, Xr=Xr, Kr=Kr: e.tensor_tensor(out=f1_, in0=Xr, in1=Kr, op=ALU.mult), reads=[prk, 'KF'], writes=['ft0'])
                T.op('dve', lambda e, f2_=f2_, Xi=Xi, Ki=Ki: e.tensor_tensor(out=f2_, in0=Xi, in1=Ki, op=ALU.mult), reads=[pik, 'KF'], writes=['ft1'])
                T.op('dve', lambda e, f3_=f3_, Xr=Xr, Ki=Ki: e.tensor_tensor(out=f3_, in0=Xr, in1=Ki, op=ALU.mult), reads=[prk, 'KF'], writes=['ft2'])
                T.op('dve', lambda e, f4_=f4_, Xi=Xi, Kr=Kr: e.tensor_tensor(out=f4_, in0=Xi, in1=Kr, op=ALU.mult), reads=[pik, 'KF'], writes=['ft3'])
                T.op('pool', lambda e, Ar=Ar, f1_=f1_, f2_=f2_: e.tensor_tensor(out=Ar, in0=f1_, in1=f2_, op=ALU.subtract), reads=['ft0', 'ft1'], writes=[ak])
                T.op('pool', lambda e, Ai=Ai, f3_=f3_, f4_=f4_: e.tensor_tensor(out=Ai, in0=f3_, in1=f4_, op=ALU.add), reads=['ft2', 'ft3'], writes=[ak])
        T.pop()

    def fft_inverse(gate, gk, dst, dk):
        T.push()
        pc = [T.ptile([65, 4, 128], F32, f'pc{i}') for i in range(2)]
        py = [T.ptile([128, 8, 64], F32, f'py{i}') for i in range(2)]
        tw = [T.tile([65, 4, 64], F32, f'iw{i}') for i in range(4)]
        for h in range(2):
            for gi in range(32):
                c0 = gi * 4
                p = pc[gi % 2]; pk = f'pc{gi % 2}'
                for i in range(4):
                    c = c0 + i
                    T.op('pe', lambda e, p=p, i=i, c=c: e.matmul(p[:, i, :], lhsT=AY[:, 0, :, c], rhs=csh[:, 0, h, :], start=True, stop=False),
                         reads=AYK + ['csh'], writes=[pk])
                    T.op('pe', lambda e, p=p, i=i, c=c: e.matmul(p[:, i, :], lhsT=AY[:, 1, :, c], rhs=csh[:, 1, h, :], start=False, stop=True),
                         reads=AYK + ['csh'], writes=[pk])
                Pr = p[:, :, 0:64]; Pi = p[:, :, 64:128]
                ct = bc_mid(twi[:, 0, h * 64:(h + 1) * 64], 4); st = bc_mid(twi[:, 1, h * 64:(h + 1) * 64], 4)
                t1, t2, t3, t4 = [t[:, :, :] for t in tw]
                T.op('dve', lambda e, t1=t1, Pr=Pr, ct=ct: e.tensor_tensor(out=t1, in0=Pr, in1=ct, op=ALU.mult), reads=[pk, 'twi'], writes=['iw0'])
                T.op('dve', lambda e, t2=t2, Pi=Pi, st=st: e.tensor_tensor(out=t2, in0=Pi, in1=st, op=ALU.mult), reads=[pk, 'twi'], writes=['iw1'])
                T.op('dve', lambda e, t3=t3, Pr=Pr, st=st: e.tensor_tensor(out=t3, in0=Pr, in1=st, op=ALU.mult), reads=[pk, 'twi'], writes=['iw2'])
                T.op('dve', lambda e, t4=t4, Pi=Pi, ct=ct: e.tensor_tensor(out=t4, in0=Pi, in1=ct, op=ALU.mult), reads=[pk, 'twi'], writes=['iw3'])
                dr = CPP[:, 0, :, c0:c0 + 4].rearrange("p n c -> p c n"); di = CPP[:, 1, :, c0:c0 + 4].rearrange("p n c -> p c n")
                T.op('pool', lambda e, dr=dr, t1=t1, t2=t2: e.tensor_tensor(out=dr, in0=t1, in1=t2, op=ALU.subtract), reads=['iw0', 'iw1'], writes=['UC'])
                T.op('pool', lambda e, di=di, t3=t3, t4=t4: e.tensor_tensor(out=di, in0=t3, in1=t4, op=ALU.add), reads=['iw2', 'iw3'], writes=['UC'])
            for q in range(8):
                p = py[q % 2]; pk = f'py{q % 2}'
                for i in range(8):
                    nl = q * 8 + i
                    T.op('pe', lambda e, p=p, i=i, nl=nl: e.matmul(p[:, i, :], lhsT=CPP[:, 0, nl, :], rhs=eri[:, 0, :], start=True, stop=False),
                         reads=['UC', 'eri'], writes=[pk])
                    T.op('pe', lambda e, p=p, i=i, nl=nl: e.matmul(p[:, i, :], lhsT=CPP[:, 1, nl, :], rhs=eri[:, 1, :], start=False, stop=True),
                         reads=['UC', 'eri'], writes=[pk])
                tok0 = (h * 64 + q * 8) * 64
                T.op('dve', lambda e, p=p, tok0=tok0: e.tensor_tensor(out=dst[:, tok0:tok0 + 512], in0=p[:, :, :].rearrange("p a b -> p (a b)"),
                     in1=gate[:, tok0:tok0 + 512], op=ALU.mult), reads=[pk, gk], writes=[dk])
        T.pop()

    T.push()
    h3T = T.tile([64, S], F32, 'h3T')
    hbT = T.tile([64, 4], F32, 'hbT'); frb = T.tile([64, 3], F32, 'frb')
    T.dma('sp', lambda e: e.dma_start(out=hbT[:], in_=hb_d), writes=['hbT'])
    for l in range(3):
        T.op('dve', lambda e, l=l: e.tensor_tensor(out=frb[:, l:l + 1], in0=hbT[:, l:l + 1], in1=hbT[:, 3:4], op=ALU.mult), reads=['hbT'], writes=['frb'])
    T.push()
    zT = T.tile([33, S], F32, 'zT')
    w1 = T.tile([33, 64], F32, 'w1'); w2 = T.tile([64, 64], F32, 'w2'); w3 = T.tile([64, 64], F32, 'w3')
    for t_, d_, k_ in ((zT, zT_d, 'zT'), (w1, hw1_d, 'w1'), (w2, hw2_d, 'w2'), (w3, hw3_d, 'w3')):
        T.dma('sp', lambda e, t_=t_, d_=d_: e.dma_start(out=t_[:], in_=d_), writes=[k_])
    pm_ = [T.ptile([64, 512], F32, f'pml{i}') for i in range(2)]
    ha = [T.tile([64, 512], F32, f'ha{i}') for i in range(2)]
    PI_ = 3.14159
    for cc in range(16):
        cs = slice(cc * 512, (cc + 1) * 512)
        cur = zT[:, cs]; curk = 'zT'
        for l, (wl, wk) in enumerate(((w1, 'w1'), (w2, 'w2'), (w3, 'w3'))):
            p = pm_[l % 2]; pk = f'pml{l % 2}'
            T.op('pe', lambda e, p=p, wl=wl, cur=cur: e.matmul(p[:, :], lhsT=wl[:, :], rhs=cur, start=True, stop=True), reads=[wk, curk], writes=[pk])
            dstt = ha[l % 2][:, :] if l < 2 else h3T[:, cs]
            dk = f'ha{l % 2}' if l < 2 else 'h3T'
            T.op('dve', lambda e, p=p, dstt=dstt, l=l: e.tensor_scalar(out=dstt, in0=p[:, :], scalar1=hbT[:, 3:4], scalar2=frb[:, l:l + 1],
                 op0=ALU.mult, op1=ALU.add), reads=[pk, 'hbT', 'frb'], writes=[dk])
            T.op('dve', lambda e, dstt=dstt: e.tensor_scalar(out=dstt, in0=dstt, scalar1=PI_, scalar2=-PI_, op0=ALU.min, op1=ALU.max), reads=[dk], writes=[dk])
            T.op('act', lambda e, dstt=dstt: e.activation(out=dstt, in_=dstt, func=AF.Sin), reads=[dk], writes=[dk])
            cur = dstt; curk = dk
    T.pop()
    dec = T.tile([128, S], BF16, 'dec'); hch = T.tile([128, S], BF16, 'hch')
    w4t = T.tile([64, 128], F32, 'w4t'); dbc = T.tile([128, 128], F32, 'dbc')
    pf = [T.ptile([128, 512], F32, f'pf{i}') for i in range(2)]
    for ch in range(8):
        T.dma('sp', lambda e, ch=ch: e.dma_start(out=dec[:], in_=dec_d[ch]), writes=['dec'])
        for o in range(2):
            T.dma('sp', lambda e, o=o: e.dma_start(out=dbc[:], in_=hyd_d[o:o + 1, ch * 128:(ch + 1) * 128].partition_broadcast(128)), writes=['dbc'])
            for d in range(2):
                col = o * 2048 + d * 1024 + ch * 128
                T.dma('sp', lambda e, col=col: e.dma_start(out=w4t[:], in_=hw4_d[:, col:col + 128]), writes=['w4t'])
                for cc in range(16):
                    p = pf[cc % 2]; pk = f'pf{cc % 2}'
                    T.op('pe', lambda e, p=p, cc=cc: e.matmul(p[:, :], lhsT=w4t[:, :], rhs=h3T[:, cc * 512:(cc + 1) * 512], start=True, stop=True),
                         reads=['w4t', 'h3T'], writes=[pk])
                    T.op('dve', lambda e, p=p, cc=cc: e.tensor_tensor(out=hch[:, cc * 512:(cc + 1) * 512], in0=p[:, :], in1=dec[:, cc * 512:(cc + 1) * 512],
                         op=ALU.mult), reads=[pk, 'dec'], writes=['hch'])
                if d == 1:
                    T.op('pool', lambda e: e.memset(hch[:, 0:1], 0.0), reads=['hch'], writes=['hch'])
                fft_forward(hch, 'hch', 'kf0' if d == 0 else 'kf1', dbc)
            T.dma('sp', lambda e, o=o: e.dma_start(out=kf_d[o, ch], in_=KF[:]), reads=['KF'], writes=['kf_d'])
    T.pop()


    T.push()
    Bb = [T.tile([128, S], BF16, f'B{i}') for i in range(4)]
    hcw = T.tile([128, 24, 3], F32, 'hcw'); hcb = T.tile([128, 24], F32, 'hcb')
    T.dma('sp', lambda e: e.dma_start(out=hcw[:], in_=hcw_d), writes=['hcw'])
    T.dma('sp', lambda e: e.dma_start(out=hcb[:], in_=hcb_d), writes=['hcb'])
    hag = T.tile([128, 16, 512], BF16, 'hag')
    hwb = [T.tile([128, 16, 128], BF16, f'hwb{i}') for i in range(3)]
    for ch in range(8):
        for sig in range(3):
            c0 = 2048 + sig * 1024 + ch * 128
            src = win_d[:, c0:c0 + 128].rearrange("(k p) f -> p k f", p=128)
            T.dma('pool', lambda e, src=src, sig=sig: e.dma_start(out=hwb[sig][:], in_=src), writes=[f'hwb{sig}'])
        T.push()
        hp = [T.ptile([128, 512], F32, f'hp{i}') for i in range(4)]
        cnt = 0
        for g in range(16):
            src = aT_d[:, :, g * 512:(g + 1) * 512].rearrange("j p t -> p j t")
            T.dma('sp', lambda e, src=src: e.dma_start(out=hag[:], in_=src), reads=['aT_d'], writes=['hag'])
            for sig in range(3):
                p = hp[cnt % 4]; pk = f'hp{cnt % 4}'; cnt += 1
                for k in range(16):
                    T.op('pe', lambda e, p=p, sig=sig, k=k: e.matmul(p[:, :], lhsT=hwb[sig][:, k, :], rhs=hag[:, k, :], start=(k == 0), stop=(k == 15)),
                         reads=[f'hwb{sig}', 'hag'], writes=[pk])
                T.op('act', lambda e, p=p, sig=sig, g=g: e.activation(out=Bb[sig][:, g * 512:(g + 1) * 512], in_=p[:, :], func=AF.Copy),
                     reads=[pk], writes=[f'B{sig}'])
        T.pop()
        for sig, (si, di) in enumerate(((0, 3), (1, 0), (2, 1))):
            t = sig * 8 + ch
            src = Bb[si]; dst = Bb[di]; sk = f'B{si}'; dk = f'B{di}'
            T.op('dve', lambda e, src=src, dst=dst, t=t: e.tensor_scalar(out=dst[:, :], in0=src[:, :], scalar1=hcw[:, t, 1:2], scalar2=hcb[:, t:t + 1],
                 op0=ALU.mult, op1=ALU.add), reads=[sk, 'hcw', 'hcb'], writes=[dk])
            for (tap, o_lo, i_lo, n) in ((0, 64, 0, S - 64), (2, 0, 64, S - 64), (0, 1, S - 64, 63), (2, S - 64, 1, 63)):
                T.op('dve', lambda e, src=src, dst=dst, t=t, tap=tap, o_lo=o_lo, i_lo=i_lo, n=n: e.scalar_tensor_tensor(
                    out=dst[:, o_lo:o_lo + n], in0=src[:, i_lo:i_lo + n], scalar=hcw[:, t, tap:tap + 1], in1=dst[:, o_lo:o_lo + n],
                    op0=ALU.mult, op1=ALU.add), reads=[sk, dk, 'hcw'], writes=[dk])
        T.dma('sp', lambda e, ch=ch: e.dma_start(out=KF[:], in_=kf_d[0, ch]), reads=['kf_d'], writes=['KF'])
        fft_forward(Bb[3], 'B3', 'mul')
        fft_inverse(Bb[0], 'B0', Bb[2], 'B2')
        T.dma('sp', lambda e, ch=ch: e.dma_start(out=KF[:], in_=kf_d[1, ch]), reads=['kf_d'], writes=['KF'])
        fft_forward(Bb[2], 'B2', 'mul')
        fft_inverse(Bb[1], 'B1', Bb[3], 'B3')
        T.dma('sp', lambda e, ch=ch: e.dma_start(out=mix_d[8 + ch], in_=Bb[3][:, :]), reads=['B3'], writes=['mix_d'])
    T.pop()
    T.pop()


    T.push()
    wob = T.tile([128, 16, D], BF16, 'wob')
    for dc in range(4):
        src = wout_d[:, dc * 512:(dc + 1) * 512].rearrange("(k p) f -> p k f", p=128)
        T.dma('pool', lambda e, src=src, dc=dc: e.dma_start(out=wob[:, :, dc * 512:(dc + 1) * 512], in_=src), writes=['wob'])
    gnT = T.tile([128, 16], F32, 'gnT'); G1bc = T.tile([128, D], F32, 'G1bc'); onesb = T.tile([128, 128], BF16, 'onesb')
    T.dma('sp', lambda e: e.dma_start(out=gnT[:], in_=gnT_d), writes=['gnT'])
    T.dma('sp', lambda e: e.dma_start(out=G1bc[:], in_=grow_d[0:1, :].partition_broadcast(128)), reads=['grow_d'], writes=['G1bc'])
    T.op('pool', lambda e: e.memset(onesb[:], 1.0), writes=['onesb'])
    mixg = T.tile([128, 16, 512], BF16, 'mixg'); mixn = T.tile([128, 16, 512], BF16, 'mixn')
    sqb = [T.tile([128, 512], BF16, f'sqb{i}') for i in range(2)]
    rs = T.tile([128, 2, 512], F32, 'rs')
    prs = [T.ptile([128, 512], F32, f'prs{i}') for i in range(2)]
    po = [T.ptile([128, 512], F32, f'po{i}') for i in range(2)]
    ptr3 = [T.ptile([128, 512], F32, f'ptr3{i}') for i in range(2)]
    X3 = [T.tile([128, D], F32, f'X3{i}') for i in range(2)]
    H3 = [T.tile([128, D], F32, f'H3{i}') for i in range(2)]
    scr3 = ([T.tile([128, D], BF16, f'junk{i}') for i in range(2)],
            [T.tile([128, 1], F32, f'ss{i}') for i in range(2)],
            [T.tile([128, D], F32, f'XN{i}') for i in range(2)])
    mTg = [T.tile([128, 16, 512], BF16, f'mTg{i}') for i in range(2)]
    it = 0
    offreg = T.es.enter_context(nc.sync.register("offreg"))
    for g in range(SL // 512):
        def ld_mix(e, g=g):
            e.reg_load(offreg, offs_d[0:1, g:g + 1])
            v = e.snap(offreg)
            return e.dma_start(out=mixg[:], in_=mix_d[:, :, bass.ds(v, 512)].rearrange("j p t -> p j t"))
        T.dma('sp', ld_mix, reads=['mix_d'], writes=['mixg'])
        for grp in range(2):
            for jj in range(8):
                j = grp * 8 + jj
                sq = sqb[j % 2]; sk = f'sqb{j % 2}'
                T.op('act', lambda e, sq=sq, j=j: e.activation(out=sq[:], in_=mixg[:, j, :], func=AF.Square), reads=['mixg'], writes=[sk])
                T.op('pe', lambda e, sq=sq, grp=grp, jj=jj: e.matmul(prs[grp][:, :], lhsT=onesb[:, :], rhs=sq[:, :], start=(jj == 0), stop=(jj == 7)),
                     reads=[sk, 'onesb'], writes=[f'prs{grp}'])
            T.op('act', lambda e, grp=grp: e.activation(out=rs[:, grp, :], in_=prs[grp][:, :], func=AF.Sqrt, scale=1.0 / 1024, bias=epsT[:, 0:1]),
                 reads=[f'prs{grp}', 'epsT'], writes=['rs'])
            T.op('dve', lambda e, grp=grp: e.reciprocal(out=rs[:, grp, :], in_=rs[:, grp, :]), reads=['rs'], writes=['rs'])
        for j in range(16):
            T.op('dve', lambda e, j=j: e.scalar_tensor_tensor(out=mixn[:, j, :], in0=mixg[:, j, :], scalar=gnT[:, j:j + 1], in1=rs[:, j // 8, :],
                 op0=ALU.mult, op1=ALU.mult), reads=['mixg', 'gnT', 'rs'], writes=['mixn'])
        mg = mTg[g % 2]; mgk = f'mTg{g % 2}'
        for sub in range(4):
            X = X3[it % 2]; xk = f'X3{it % 2}'; H = H3[it % 2]; hk = f'H3{it % 2}'
            r0 = g * 512 + sub * 128
            T.dma('sp', lambda e, X=X, r0=r0: e.dma_start(out=X[:], in_=xh_d[r0:r0 + 128, :]), writes=[xk])
            for dc in range(4):
                p = po[dc % 2]; pk = f'po{dc % 2}'
                for k in range(16):
                    T.op('pe', lambda e, p=p, k=k, sub=sub, dc=dc: e.matmul(p[:, :], lhsT=mixn[:, k, sub * 128:(sub + 1) * 128],
                         rhs=wob[:, k, dc * 512:(dc + 1) * 512], start=(k == 0), stop=(k == 15)), reads=['mixn', 'wob'], writes=[pk])
                T.op('dve', lambda e, p=p, H=H, dc=dc: e.tensor_tensor(out=H[:, dc * 512:(dc + 1) * 512], in0=p[:, :], in1=G1bc[:, dc * 512:(dc + 1) * 512],
                     op=ALU.mult), reads=[pk, 'G1bc'], writes=[hk])
            T.op('pool', lambda e, H=H, X=X: e.tensor_tensor(out=H[:], in0=H[:], in1=X[:], op=ALU.add), reads=[hk, xk], writes=[hk])
            T.dma('sp', lambda e, H=H, r0=r0: e.dma_start(out=hx1_d[r0:r0 + 128, :], in_=H[:]), reads=[hk], writes=['hx1_d'])
            norm_to_T(H, hk, A2, modT[:, 48:64, :], 0, mg, mgk, sub * 128, scr3, ptr3, ['ptr30', 'ptr31'], it, abk=('A2', 'modT'))
            it += 1
        dst = mT_d[:, :, g * 512:(g + 1) * 512].rearrange("j p t -> p j t")
        T.dma('sp', lambda e, mg=mg, dst=dst: e.dma_start(out=dst, in_=mg[:]), reads=[mgk], writes=['mT_d'])
    T.pop()

    TC = 1024
    T.push()
    PS = [T.ptile([128, 512], F32, f'PS{i}') for i in range(8)]
    mTc = T.tile([128, 16, TC], BF16, 'mTc'); acc = T.tile([128, 16, TC], F32, 'acc'); actb = T.tile([128, 16, TC], BF16, 'actb')
    wst = [T.tile([128, 8, 256], F32, f'wst{i}') for i in range(2)]
    wr = [T.tile([128, 16, 256], BF16, f'wr{i}') for i in range(4)]
    gT = T.tile([32, TC], F32, 'gT'); gbc = T.tile([128, TC], BF16, 'gbc')
    mt = [T.tile([128, 512], F32, f'mt{i}') for i in range(3)]
    bguT = T.tile([128, 32, 32], F32, 'bguT'); BD = T.tile([32, D], F32, 'BD')
    rwb = T.tile([128, 16, 32], BF16, 'rwb'); RBbc = T.tile([128, 32], F32, 'RBbc')
    T.dma('sp', lambda e: e.dma_start(out=bguT[:], in_=bguT_d), writes=['bguT'])
    T.dma('sp', lambda e: e.dma_start(out=BD[:], in_=bd_d), writes=['BD'])
    T.dma('pool', lambda e: e.dma_start(out=rwb[:], in_=rw_d.rearrange("(k p) f -> p k f", p=128)), writes=['rwb'])
    T.dma('sp', lambda e: e.dma_start(out=RBbc[:], in_=rb_d.partition_broadcast(128)), writes=['RBbc'])
    Lg = T.tile([128, 32], F32, 'Lg'); Eg = T.tile([128, 32], F32, 'Eg'); v8 = T.tile([128, 8], F32, 'v8'); sm = T.tile([128, 2], F32, 'sm')
    wcount = [0]
    def f32view(ap3):
        return ap3.bitcast(F32).rearrange("p a b -> p (a b)")
    hx_t = [f32view(actb[:, 0:4, :]), f32view(actb[:, 4:8, :])]
    mo_t = f32view(actb[:, 8:12, :])
    G2bc = f32view(actb[:, 12:16, :])
    finbc = wst[0][:, :, :].rearrange("p a b -> p (a b)")

    def load_piece(src_fn, pid, first):
        r = wcount[0] % 4
        wt = wr[r]; wk = f'wr{r}'
        if not first:
            T.dma('sp', lambda e, wt=wt, pid=pid: e.dma_start(out=wt[:].rearrange("p k f -> p (k f)"), in_=wbf_l[pid // 192][pid % 192]), reads=[f'wbf{pid}'], writes=[wk])
            wcount[0] += 1
            return wt, wk
        for half in range(2):
            stg = wst[(2 * wcount[0] + half) % 2]; sk = f'wst{(2 * wcount[0] + half) % 2}'
            T.dma('sp', lambda e, stg=stg, half=half: e.dma_start(out=stg[:], in_=src_fn(half)), writes=[sk])
            eng = ('act', 'dve', 'pool')[(2 * wcount[0] + half) % 3]
            if eng == 'act':
                T.op('act', lambda e, wt=wt, stg=stg, half=half: e.activation(out=wt[:, half * 8:(half + 1) * 8, :], in_=stg[:], func=AF.Copy), reads=[sk], writes=[wk])
            else:
                T.op(eng, lambda e, wt=wt, stg=stg, half=half: e.tensor_copy(out=wt[:, half * 8:(half + 1) * 8, :], in_=stg[:]), reads=[sk], writes=[wk])
        T.dma('sp', lambda e, wt=wt, pid=pid: e.dma_start(out=wbf_l[pid // 192][pid % 192], in_=wt[:].rearrange("p k f -> p (k f)")), reads=[wk], writes=[f'wbf{pid}'])
        wcount[0] += 1
        return wt, wk

    def bc_free(ap_col, n):
        a = ap_col
        return bass.AP(tensor=a.tensor, offset=a.offset, ap=[list(a.ap[0]), [0, n]])

    for chk in range(SL // TC):
        t0 = chk * TC
        T.dma('sp', lambda e, t0=t0: e.dma_start(out=mTc[:], in_=mT_d[:, :, t0:t0 + TC].rearrange("j p t -> p j t")), reads=['mT_d'], writes=['mTc'])
        for sub in range(TC // 128):
            for k in range(16):
                T.op('pe', lambda e, k=k, sub=sub: e.matmul(PS[0][:, 0:32], lhsT=mTc[:, k, sub * 128:(sub + 1) * 128], rhs=rwb[:, k, :],
                     start=(k == 0), stop=(k == 15)), reads=['mTc', 'rwb'], writes=['PS0'])
            T.op('dve', lambda e: e.tensor_tensor(out=Lg[:], in0=PS[0][:, 0:32], in1=RBbc[:], op=ALU.add), reads=['PS0', 'RBbc'], writes=['Lg'])
            T.op('dve', lambda e: e.max(out=v8[:], in_=Lg[:]), reads=['Lg'], writes=['v8'])
            T.op('dve', lambda e: e.tensor_scalar(out=sm[:, 0:1], in0=v8[:, 0:1], scalar1=-1.0, scalar2=None, op0=ALU.mult), reads=['v8'], writes=['sm'])
            T.op('act', lambda e: e.activation(out=Eg[:], in_=Lg[:], func=AF.Exp, bias=sm[:, 0:1]), reads=['Lg', 'sm'], writes=['Eg'])
            T.op('dve', lambda e: e.tensor_scalar(out=Lg[:], in0=Lg[:], scalar1=v8[:, 3:4], scalar2=None, op0=ALU.is_ge), reads=['Lg', 'v8'], writes=['Lg'])
            T.op('dve', lambda e: e.tensor_tensor(out=Eg[:], in0=Eg[:], in1=Lg[:], op=ALU.mult), reads=['Eg', 'Lg'], writes=['Eg'])
            T.op('dve', lambda e: e.tensor_reduce(out=sm[:, 1:2], in_=Eg[:], axis=mybir.AxisListType.X, op=ALU.add), reads=['Eg'], writes=['sm'])
            T.op('dve', lambda e: e.reciprocal(out=sm[:, 1:2], in_=sm[:, 1:2]), reads=['sm'], writes=['sm'])
            T.op('dve', lambda e: e.tensor_scalar(out=Eg[:], in0=Eg[:], scalar1=sm[:, 1:2], scalar2=None, op0=ALU.mult), reads=['Eg', 'sm'], writes=['Eg'])
            T.op('pe', lambda e: e.transpose(out=PS[1][0:32, 0:128], in_=Eg[:, :], identity=identf[:]), reads=['Eg', 'identf'], writes=['PS1'])
            T.op('act', lambda e, sub=sub: e.activation(out=gT[:, sub * 128:(sub + 1) * 128], in_=PS[1][0:32, 0:128], func=AF.Copy), reads=['PS1'], writes=['gT'])
        if dbg and chk == 0:
            T.dma('sp', lambda e: e.dma_start(out=dbg_d['gT'], in_=gT[:]), reads=['gT'])
        for m in range(16):
            for h in range(2):
                p = PS[2 + h]; pk = f'PS{2 + h}'
                T.op('pe', lambda e, p=p, m=m, h=h: e.matmul(p[:, :], lhsT=BD[0:32, m * 128:(m + 1) * 128], rhs=gT[0:32, h * 512:(h + 1) * 512],
                     start=True, stop=True), reads=['BD', 'gT'], writes=[pk])
                T.op('act', lambda e, p=p, m=m, h=h: e.activation(out=acc[:, m, h * 512:(h + 1) * 512], in_=p[:, :], func=AF.Copy), reads=[pk], writes=['acc'])
        for ex in range(32):
            for h in range(2):
                p = PS[2 + h]; pk = f'PS{2 + h}'
                T.op('pe', lambda e, p=p, h=h, ex=ex: e.matmul(p[:, :], lhsT=bc_free(identf[0:32, ex:ex + 1], 128), rhs=gT[0:32, h * 512:(h + 1) * 512],
                     start=True, stop=True), reads=['identf', 'gT'], writes=[pk])
                T.op('act', lambda e, p=p, h=h: e.activation(out=gbc[:, h * 512:(h + 1) * 512], in_=p[:, :], func=AF.Copy), reads=[pk], writes=['gbc'])
            for step in range(8):
                j0 = 2 * step
                wg, wgk = load_piece(lambda half, ex=ex, j0=j0: wgu_d[ex, half * 1024:(half + 1) * 1024, j0 * 128:j0 * 128 + 256].rearrange("(k p) f -> p k f", p=128), ex * 24 + 2 * step, chk == 0)
                wl, wlk = load_piece(lambda half, ex=ex, j0=j0: wgu_d[ex, half * 1024:(half + 1) * 1024, 2048 + j0 * 128:2048 + j0 * 128 + 256].rearrange("(k p) f -> p k f", p=128), ex * 24 + 2 * step + 1, chk == 0)
                for jj in range(2):
                    j = j0 + jj
                    for h in range(2):
                        pg = PS[4 + h]; pgk = f'PS{4 + h}'; pl_ = PS[6 + h]; plk = f'PS{6 + h}'
                        for k in range(16):
                            T.op('pe', lambda e, pg=pg, wg=wg, k=k, jj=jj, h=h: e.matmul(pg[:, :], lhsT=wg[:, k, jj * 128:(jj + 1) * 128],
                                 rhs=mTc[:, k, h * 512:(h + 1) * 512], start=(k == 0), stop=(k == 15)), reads=[wgk, 'mTc'], writes=[pgk])
                        for k in range(16):
                            T.op('pe', lambda e, pl_=pl_, wl=wl, k=k, jj=jj, h=h: e.matmul(pl_[:, :], lhsT=wl[:, k, jj * 128:(jj + 1) * 128],
                                 rhs=mTc[:, k, h * 512:(h + 1) * 512], start=(k == 0), stop=(k == 15)), reads=[wlk, 'mTc'], writes=[plk])
                        t1, t2, t3 = mt
                        T.op('dve', lambda e, pg=pg, j=j, ex=ex: e.tensor_scalar(out=t1[:], in0=pg[:, :], scalar1=bguT[:, ex, j:j + 1], scalar2=7.0, op0=ALU.add, op1=ALU.min),
                             reads=[pgk, 'bguT'], writes=['mt0'])
                        T.op('act', lambda e: e.activation(out=t2[:], in_=t1[:], func=AF.Sigmoid, scale=1.702), reads=['mt0'], writes=['mt1'])
                        T.op('dve', lambda e, pl_=pl_, j=j, ex=ex: e.tensor_scalar(out=t3[:], in0=pl_[:, :], scalar1=bguT[:, ex, 16 + j:16 + j + 1], scalar2=7.0, op0=ALU.add, op1=ALU.min),
                             reads=[plk, 'bguT'], writes=['mt2'])
                        T.op('pool', lambda e: e.tensor_scalar(out=t3[:], in0=t3[:], scalar1=-7.0, scalar2=1.0, op0=ALU.max, op1=ALU.add), reads=['mt2'], writes=['mt2'])
                        T.op('pool', lambda e: e.tensor_tensor(out=t1[:], in0=t1[:], in1=t2[:], op=ALU.mult), reads=['mt0', 'mt1'], writes=['mt0'])
                        T.op('dve', lambda e: e.tensor_tensor(out=t1[:], in0=t1[:], in1=t3[:], op=ALU.mult), reads=['mt0', 'mt2'], writes=['mt0'])
                        T.op('pool', lambda e, j=j, h=h: e.tensor_tensor(out=actb[:, j, h * 512:(h + 1) * 512], in0=t1[:], in1=gbc[:, h * 512:(h + 1) * 512], op=ALU.mult),
                             reads=['mt0', 'gbc'], writes=['actb'])
            for step in range(8):
                m0 = 2 * step
                wd, wdk = load_piece(lambda half, ex=ex, m0=m0: wdn_d[ex, half * 1024:(half + 1) * 1024, m0 * 128:m0 * 128 + 256].rearrange("(k p) f -> p k f", p=128), ex * 24 + 16 + step, chk == 0)
                for mm in range(2):
                    m = m0 + mm
                    for h in range(2):
                        p = PS[2 + h]; pk = f'PS{2 + h}'
                        for k in range(16):
                            T.op('pe', lambda e, p=p, wd=wd, k=k, mm=mm, h=h: e.matmul(p[:, :], lhsT=wd[:, k, mm * 128:(mm + 1) * 128],
                                 rhs=actb[:, k, h * 512:(h + 1) * 512], start=(k == 0), stop=(k == 15)), reads=[wdk, 'actb'], writes=[pk])
                        T.op('dve', lambda e, p=p, m=m, h=h: e.tensor_tensor(out=acc[:, m, h * 512:(h + 1) * 512], in0=acc[:, m, h * 512:(h + 1) * 512], in1=p[:, :], op=ALU.add),
                             reads=[pk, 'acc'], writes=['acc'])
        T.dma('sp', lambda e: e.dma_start(out=G2bc, in_=grow_d[1:2, :].partition_broadcast(128)), reads=['grow_d'], writes=['actb'])
        T.dma('sp', lambda e: e.dma_start(out=finbc, in_=fing_d.partition_broadcast(128)), writes=['wst0'])
        for sub in range(TC // 128):
            r0 = t0 + sub * 128
            hxt = hx_t[sub % 2]; hk = 'actb'
            T.dma('sp', lambda e, hxt=hxt, r0=r0: e.dma_start(out=hxt, in_=hx1_d[r0:r0 + 128, :]), reads=['hx1_d'], writes=[hk])
            for q in range(4):
                p = PS[4 + q]; pk = f'PS{4 + q}'
                for jj in range(4):
                    m = q * 4 + jj
                    T.op('pe', lambda e, p=p, jj=jj, m=m, sub=sub: e.transpose(out=p[:, jj * 128:(jj + 1) * 128], in_=acc[:, m, sub * 128:(sub + 1) * 128], identity=identf[:]),
                         reads=['acc', 'identf'], writes=[pk])
                T.op('dve', lambda e, p=p, q=q: e.tensor_tensor(out=mo_t[:, q * 512:(q + 1) * 512], in0=p[:, :], in1=G2bc[:, q * 512:(q + 1) * 512], op=ALU.mult),
                     reads=[pk, 'actb'], writes=['actb'])
            T.op('pool', lambda e, hxt=hxt: e.tensor_tensor(out=hxt, in0=hxt, in1=mo_t, op=ALU.add), reads=['actb'], writes=['actb'])
            T.op('act', lambda e, hxt=hxt: e.activation(out=mo_t, in_=hxt, func=AF.Square, accum_out=sm[:, 0:1]), reads=[hk], writes=['actb', 'sm'])
            T.op('act', lambda e: e.activation(out=sm[:, 0:1], in_=sm[:, 0:1], func=AF.Sqrt, scale=1.0 / D, bias=epsT[:, 0:1]), reads=['sm', 'epsT'], writes=['sm'])
            T.op('dve', lambda e: e.reciprocal(out=sm[:, 0:1], in_=sm[:, 0:1]), reads=['sm'], writes=['sm'])
            T.op('dve', lambda e, hxt=hxt: e.scalar_tensor_tensor(out=hxt, in0=hxt, scalar=sm[:, 0:1], in1=finbc, op0=ALU.mult, op1=ALU.mult),
                 reads=[hk, 'sm', 'wst0'], writes=[hk])
            T.dma('sp', lambda e, hxt=hxt, r0=r0: e.dma_start(out=out_d[r0:r0 + 128, :], in_=hxt), reads=[hk], writes=['out_d'])
    T.pop()
    T.finish()
    return nc


def hy_consts():
    N = 16384
    bf = ml_dtypes.bfloat16
    n1 = np.arange(64)[:, None]; k1 = np.arange(65)[None, :]
    f1 = np.concatenate([np.cos(2 * np.pi * n1 * k1 / 128), -np.sin(2 * np.pi * n1 * k1 / 128)], 1)
    n2 = np.arange(128)[:, None]
    twf = np.concatenate([np.cos(2 * np.pi * n2 * k1 / N), -np.sin(2 * np.pi * n2 * k1 / N)], 1)
    a = np.arange(128)[:, None]; b = np.arange(128)[None, :]
    C = np.cos(2 * np.pi * a * b / 128); Sn = np.sin(2 * np.pi * a * b / 128)
    csn = np.stack([C, Sn, -Sn], 1)
    csh = np.zeros((128, 2, 2, 128))
    for h in range(2):
        csh[:, 0, h, 0:64] = C[:, h * 64:(h + 1) * 64]; csh[:, 0, h, 64:128] = Sn[:, h * 64:(h + 1) * 64]
        csh[:, 1, h, 0:64] = -Sn[:, h * 64:(h + 1) * 64]; csh[:, 1, h, 64:128] = C[:, h * 64:(h + 1) * 64]
    kk = np.arange(65)[:, None]; nn = np.arange(128)[None, :]
    twi = np.stack([np.cos(2 * np.pi * nn * kk / N), np.sin(2 * np.pi * nn * kk / N)], 1)
    w = np.full((65, 1), 2.0); w[0] = 1.0; w[64] = 1.0
    m1 = np.arange(64)[None, :]
    eri = np.stack([w * np.cos(2 * np.pi * m1 * kk / 128) / N, -w * np.sin(2 * np.pi * m1 * kk / 128) / N], 1)
    L = S
    t = np.linspace(0.0, 1.0, L, dtype=np.float32)[:, None]
    wv = (2.0 * np.pi * np.arange(L, dtype=np.float32)[:, None] / L).astype(np.float32)
    f = np.linspace(1e-4, 15, 16, dtype=np.float32)[None, :]
    z = np.concatenate([t, np.cos(f * wv), -np.sin(f * wv)], -1).astype(np.float32)
    import math
    max_decay = math.log(1e-2) / 0.3; min_decay = math.log(1e-2) / 1.5
    deltas = np.abs(np.linspace(min_decay, max_decay, 1024, dtype=np.float32))
    sidx = np.arange(L)
    perm = 128 * (sidx % 64) + sidx // 64
    dec = np.exp(-t * deltas[None, :])[perm]
    z = z[perm]
    return {
        "c_zT": np.ascontiguousarray(z.T), "c_dec": np.ascontiguousarray(dec.T).reshape(8, 128, L).astype(bf),
        "c_identb": np.eye(128).astype(bf), "c_f1": f1.astype(bf), "c_twf": twf.astype(np.float32),
        "c_csn": csn.astype(bf), "c_csh": csh.astype(bf), "c_twi": twi.astype(np.float32), "c_eri": eri.astype(bf),
    }


def make_in_map(b, inp, h=0):
    g = lambda k: np.asarray(inp[k], np.float32)
    cvec = np.stack([g('c')[b], g('c_ctx')], 0)
    m = {
        "x": np.ascontiguousarray(g('x')[b]),
        "ctx": np.ascontiguousarray(g('ctx')[b]),
        "cT": np.ascontiguousarray(cvec.reshape(2, 16, 128).transpose(2, 1, 0)),
        "w_mod": np.ascontiguousarray(g('w_mod')[0]),
        "bmodT": fm(g('b_mod')[0], 96),
        "n1gT": fm(g('norm1_g')[0], 16),
        "n2gT": fm(g('norm2_g')[0], 16),
        "final_g": np.ascontiguousarray(g('final_g').reshape(1, D)),
        "w_in": np.ascontiguousarray(g('w_in')[0]),
        "lcwT": np.ascontiguousarray(g('lru_conv_w')[0].reshape(4, 8, 128).transpose(2, 1, 0)),
        "lcbT": fm(g('lru_conv_b')[0], 8),
        "lru_wa": np.ascontiguousarray(g('lru_wa')[0]),
        "lru_wi": np.ascontiguousarray(g('lru_wi')[0]),
        "lbaT": np.ascontiguousarray(g('lru_ba')[0].reshape(2, 8, 128).transpose(2, 0, 1)),
        "lbiT": np.ascontiguousarray(g('lru_bi')[0].reshape(2, 8, 128).transpose(2, 0, 1)),
        "llamT": np.ascontiguousarray(g('lru_lambda')[0].reshape(2, 8, 128).transpose(2, 0, 1)),
        "identf": np.eye(128, dtype=np.float32),
        "xh": np.ascontiguousarray(g('x')[b][h * SL:(h + 1) * SL]),
        "offs": (h * SL + 512 * np.arange(8, dtype=np.int32)).reshape(1, 8).astype(np.int32),
        "hcwT": np.ascontiguousarray(g('hy_conv_w')[0].reshape(3, 24, 128).transpose(2, 1, 0)),
        "hcbT": fm(g('hy_conv_b')[0], 24),
        "hy_w1": np.ascontiguousarray(g('hy_w1')[0]), "hy_w2": np.ascontiguousarray(g('hy_w2')[0]), "hy_w3": np.ascontiguousarray(g('hy_w3')[0]),
        "hy_bT": np.ascontiguousarray(np.stack([g('hy_b1')[0], g('hy_b2')[0], g('hy_b3')[0], g('hy_freq')[0]], 1)),
        "hy_w4": np.ascontiguousarray(g('hy_w4')[0]), "hy_d": np.ascontiguousarray(g('hy_d')[0]),
        "w_out": np.ascontiguousarray(g('w_out')[0]),
        "w_gu": np.ascontiguousarray(g('exp_w_gu')[0]), "w_dn": np.ascontiguousarray(g('exp_w_down')[0]),
        "bguT": np.ascontiguousarray(g('exp_b_gu')[0].reshape(32, 32, 128).transpose(2, 0, 1)),
        "b_dn": np.ascontiguousarray(g('exp_b_down')[0]),
        "router_w": np.ascontiguousarray(g('router_w')[0]), "router_b": np.ascontiguousarray(g('router_b')[0].reshape(1, 32)),
        "gnT": np.ascontiguousarray(np.concatenate([fm(g('gn_lru')[0], 8), fm(g('gn_hy')[0], 8)], 1)),
    }
    m.update(hy_consts())
    return m


def kernel(**inputs):
    nc = build(dbg=False)
    shared = {}
    in_maps = []
    for c in range(NCORES):
        b, h = c // 2, c % 2
        if h == 0:
            shared = make_in_map(b, inputs, 0)
            in_maps.append(shared)
        else:
            m = dict(shared)
            m["xh"] = np.ascontiguousarray(np.asarray(inputs['x'], np.float32)[b][SL:2 * SL])
            m["offs"] = (SL + 512 * np.arange(8, dtype=np.int32)).reshape(1, 8).astype(np.int32)
            in_maps.append(m)
    res = run_bass_kernel_spmd(nc, in_maps, core_ids=list(range(NCORES)))
    out = np.empty((4, S, D), np.float32)
    for c in range(NCORES):
        out[c // 2, (c % 2) * SL:(c % 2 + 1) * SL] = np.asarray(res.results[c]["out"], np.float32)
    return out
```

```python
import numpy as np
from contextlib import ExitStack
import ml_dtypes
import concourse.bass as bass
import concourse.mybir as mybir
from concourse.bass_utils import run_bass_kernel_spmd

F32 = mybir.dt.float32
BF16 = mybir.dt.bfloat16
ALU = mybir.AluOpType
AF = mybir.ActivationFunctionType

S = 8192
CTX = 256
D = 2048
ST = S + CTX
NCORES = 8
SL = S // 2
EPS = 1e-6


class Trk:
    NDS = 8

    def __init__(self, nc):
        self.nc = nc
        self.es = ExitStack()
        self.engs = {'pe': nc.tensor, 'act': nc.scalar, 'dve': nc.vector, 'pool': nc.gpsimd, 'sp': nc.sync}
        self.sem = {e: self.es.enter_context(nc.semaphore('c_' + e)) for e in self.engs}
        self.cnt = {e: 0 for e in self.engs}
        self.dsem = {q: [self.es.enter_context(nc.semaphore(f'd_{q}{i}')) for i in range(self.NDS)]
                     for q in ('sp', 'pool')}
        self.duse = {q: [0] * self.NDS for q in self.dsem}
        self.drr = {q: 0 for q in self.dsem}
        self.waited = {e: {} for e in self.engs}
        self.lastw = {}
        self.readers = {}
        self.ntile = 0
        self.scopes = [self.es]

    def tile(self, shape, dt, name=None):
        self.ntile += 1
        return self.scopes[-1].enter_context(self.nc.sbuf_tensor(f'{name or "t"}_{self.ntile}', list(shape), dt))

    def ptile(self, shape, dt, name=None):
        self.ntile += 1
        return self.scopes[-1].enter_context(self.nc.psum_tensor(f'{name or "p"}_{self.ntile}', list(shape), dt))

    def push(self):
        self.scopes.append(ExitStack())

    def pop(self):
        self.barrier()
        self.scopes.pop().close()

    def _need(self, E, tok, waits):
        if tok is None:
            return
        kind, a, v = tok
        if kind == 'e':
            if a == E and E == 'pe':
                return
            key = ('e', a)
        else:
            key = ('d', a[0], a[1])
        if self.waited[E].get(key, -1) >= v:
            return
        self.waited[E][key] = v
        waits.append((key, v))

    def _deps(self, E, reads, writes):
        waits = []
        for b in reads:
            self._need(E, self.lastw.get(b), waits)
        for b in writes:
            self._need(E, self.lastw.get(b), waits)
            for t in self.readers.get(b, ()):
                self._need(E, t, waits)
        return waits

    def _commit(self, tok, reads, writes):
        for b in reads:
            self.readers.setdefault(b, []).append(tok)
        for b in writes:
            self.lastw[b] = tok
            self.readers[b] = []

    def _semof(self, key):
        return self.sem[key[1]] if key[0] == 'e' else self.dsem[key[1]][key[2]]

    def _emit(self, E, waits, fn, inc):
        eng = self.engs[E]
        for key, v in waits:
            eng.wait_ge(self._semof(key), v)
        if fn is None:
            return
        ins = fn(eng)
        if inc[0] == 'e':
            ins.then_inc(self.sem[E], 1)
        else:
            ins.then_inc(self.dsem[inc[1]][inc[2]], 16)

    def op(self, E, fn, reads=(), writes=()):
        waits = self._deps(E, reads, writes)
        self.cnt[E] += 1
        tok = ('e', E, self.cnt[E])
        self._emit(E, waits, fn, ('e', E))
        self._commit(tok, reads, writes)

    def dma(self, Q, fn, reads=(), writes=()):
        r = self.drr[Q]
        self.drr[Q] = (r + 1) % self.NDS
        waits = self._deps(Q, reads, writes)
        prev = self.duse[Q][r]
        if prev > 0:
            self._need(Q, ('d', (Q, r), 16 * prev), waits)
        self.duse[Q][r] += 1
        tok = ('d', (Q, r), 16 * self.duse[Q][r])
        self._emit(Q, waits, fn, ('d', Q, r))
        self._commit(tok, reads, writes)

    def barrier(self):
        for E in self.engs:
            waits = []
            for E2 in self.engs:
                if E2 != E and self.cnt[E2] > 0:
                    self._need(E, ('e', E2, self.cnt[E2]), waits)
            for q in self.dsem:
                for r in range(self.NDS):
                    if self.duse[q][r] > 0:
                        self._need(E, ('d', (q, r), 16 * self.duse[q][r]), waits)
            self._emit(E, waits, None, None)
        self.lastw = {}
        self.readers = {}

    def finish(self):
        self.barrier()
        while self.scopes:
            self.scopes.pop().close()


def fm(v, ntiles):
    return np.ascontiguousarray(np.asarray(v, np.float32).reshape(ntiles, 128).T)


def build(dbg=False):
    nc = bass.Bass("TRN2", target_bir_lowering=False)
    T = Trk(nc)

    def din(name, shape, dt=F32):
        return nc.dram_tensor(name, list(shape), dt, kind="ExternalInput").ap()

    def dout(name, shape, dt=F32):
        return nc.dram_tensor(name, list(shape), dt, kind="ExternalOutput").ap()

    def dscr(name, shape, dt):
        return nc.dram_tensor(name, list(shape), dt).ap()

    x_d = din("x", [S, D])
    ctx_d = din("ctx", [CTX, D])
    cT_d = din("cT", [128, 16, 2])
    wmod_d = din("w_mod", [D, 6 * D])
    bmodT_d = din("bmodT", [128, 96])
    n1gT_d = din("n1gT", [128, 16])
    n2gT_d = din("n2gT", [128, 16])
    fing_d = din("final_g", [1, D])
    win_d = din("w_in", [D, 5120])
    lcw_d = din("lcwT", [128, 8, 4])
    lcb_d = din("lcbT", [128, 8])
    lwa_d = din("lru_wa", [2, 8, 128, 128])
    lwi_d = din("lru_wi", [2, 8, 128, 128])
    lba_d = din("lbaT", [128, 2, 8])
    lbi_d = din("lbiT", [128, 2, 8])
    llam_d = din("llamT", [128, 2, 8])
    identf_d = din("identf", [128, 128])
    out_d = dout("out", [SL, D])
    xh_d = din("xh", [SL, D])
    offs_d = din("offs", [1, 8], mybir.dt.int32)
    dbg_d = {}
    if dbg:
        dbg_d['modT'] = dout("dbg_modT", [128, 96, 2])
        dbg_d['aT'] = dout("dbg_aT", [16, 128, ST], BF16)
        dbg_d['mix'] = dout("dbg_mix", [16, 128, S], BF16)
        dbg_d['kf'] = dout("dbg_kf", [2, 8, 128, 2, 65, 128], BF16)
    aT_d = dbg_d['aT'] if dbg else dscr("aT_s", [16, 128, ST], BF16)
    mix_d = dbg_d['mix'] if dbg else dscr("mix_s", [16, 128, S], BF16)
    kf_d = dbg_d['kf'] if dbg else dscr("kf_s", [2, 8, 128, 2, 65, 128], BF16)
    grow_d = dscr("grow_s", [2, D], F32)
    if dbg:
        dbg_d['hx1'] = dout("dbg_hx1", [SL, D]); dbg_d['mT'] = dout("dbg_mT", [16, 128, SL], BF16)
    hx1_d = dbg_d['hx1'] if dbg else dscr("hx1_s", [SL, D], F32)
    mT_d = dbg_d['mT'] if dbg else dscr("mT_s", [16, 128, SL], BF16)
    wout_d = din("w_out", [D, D]); gnT_d = din("gnT", [128, 16])
    wgu_d = din("w_gu", [32, D, 2 * D]); wdn_d = din("w_dn", [32, D, D])
    wbf_l = [dscr(f"wbf_s{i}", [8 * 24, 128, 16 * 256], BF16) for i in range(4)]
    bguT_d = din("bguT", [128, 32, 32]); bd_d = din("b_dn", [32, D]); rw_d = din("router_w", [D, 32]); rb_d = din("router_b", [1, 32])
    if dbg:
        dbg_d['gT'] = dout("dbg_gT", [32, 1024])
    hcw_d = din("hcwT", [128, 24, 3]); hcb_d = din("hcbT", [128, 24])
    hw1_d = din("hy_w1", [33, 64]); hw2_d = din("hy_w2", [64, 64]); hw3_d = din("hy_w3", [64, 64])
    hb_d = din("hy_bT", [64, 4])
    hw4_d = din("hy_w4", [64, 4096]); hyd_d = din("hy_d", [2, 1024])
    zT_d = din("c_zT", [33, S]); dec_d = din("c_dec", [8, 128, S], BF16)
    identb_d = din("c_identb", [128, 128], BF16); f1_d = din("c_f1", [64, 130], BF16)
    twf_d = din("c_twf", [128, 130]); csn_d = din("c_csn", [128, 3, 128], BF16)
    csh_d = din("c_csh", [128, 2, 2, 128], BF16); twi_d = din("c_twi", [65, 2, 128]); eri_d = din("c_eri", [65, 2, 64], BF16)

    identf = T.tile([128, 128], F32, 'identf')
    T.dma('sp', lambda e: e.dma_start(out=identf[:], in_=identf_d), writes=['identf'])
    modT = T.tile([128, 96, 2], F32, 'modT')
    epsT = T.tile([128, 1], F32, 'epsT')
    oneT = T.tile([128, 1], F32, 'oneT')
    T.op('pool', lambda e: e.memset(epsT[:], EPS), writes=['epsT'])
    T.op('pool', lambda e: e.memset(oneT[:], 1.0), writes=['oneT'])
    A1 = T.tile([128, 16, 2], F32, 'A1')
    A2 = T.tile([128, 16, 2], F32, 'A2')

    T.push()
    cT = T.tile([128, 16, 2], F32, 'cT')
    sT = T.tile([128, 16, 2], F32, 'sT')
    bmodT = T.tile([128, 96], F32, 'bmodT')
    n1gT = T.tile([128, 16], F32, 'n1gT')
    T.dma('sp', lambda e: e.dma_start(out=cT[:], in_=cT_d), writes=['cT'])
    T.dma('sp', lambda e: e.dma_start(out=bmodT[:], in_=bmodT_d), writes=['bmodT'])
    T.dma('sp', lambda e: e.dma_start(out=n1gT[:], in_=n1gT_d), writes=['n1gT'])
    n2gT = T.tile([128, 16], F32, 'n2gT')
    T.dma('sp', lambda e: e.dma_start(out=n2gT[:], in_=n2gT_d), writes=['n2gT'])
    T.op('act', lambda e: e.activation(out=sT[:], in_=cT[:], func=AF.Silu), reads=['cT'], writes=['sT'])
    wm = [T.tile([128, 16, 512], F32, f'wm{i}') for i in range(2)]
    pm = [T.ptile([128, 4, 2], F32, f'pm{i}') for i in range(2)]
    for ch in range(24):
        w = wm[ch % 2]
        wk = f'wm{ch % 2}'
        pk = f'pm{ch % 2}'
        p = pm[ch % 2]
        src = wmod_d[:, ch * 512:(ch + 1) * 512].rearrange("(k p) f -> p k f", p=128)
        T.dma('sp', lambda e, w=w, src=src: e.dma_start(out=w[:], in_=src), writes=[wk])
        for j in range(4):
            for k in range(16):
                T.op('pe', lambda e, p=p, w=w, j=j, k=k: e.matmul(
                    p[:, j, :], lhsT=w[:, k, j * 128:(j + 1) * 128], rhs=sT[:, k, :],
                    start=(k == 0), stop=(k == 15)), reads=[wk, 'sT'], writes=[pk])
        for j in range(4):
            t = ch * 4 + j
            T.op('dve', lambda e, p=p, j=j, t=t: e.tensor_scalar(
                out=modT[:, t, :], in0=p[:, j, :], scalar1=bmodT[:, t:t + 1], scalar2=None, op0=ALU.add),
                reads=[pk, 'bmodT'], writes=['modT'])
    T.op('dve', lambda e: e.tensor_scalar(out=A1[:], in0=modT[:, 16:32, :], scalar1=1.0, scalar2=None, op0=ALU.add),
         reads=['modT'], writes=['A1'])
    for r in range(2):
        T.op('dve', lambda e, r=r: e.tensor_tensor(out=A1[:, :, r], in0=A1[:, :, r], in1=n1gT[:], op=ALU.mult),
             reads=['A1', 'n1gT'], writes=['A1'])
    T.op('dve', lambda e: e.tensor_scalar(out=A2[:], in0=modT[:, 64:80, :], scalar1=1.0, scalar2=None, op0=ALU.add),
         reads=['modT'], writes=['A2'])
    for r in range(2):
        T.op('dve', lambda e, r=r: e.tensor_tensor(out=A2[:, :, r], in0=A2[:, :, r], in1=n2gT[:], op=ALU.mult),
             reads=['A2', 'n2gT'], writes=['A2'])
    g12 = T.tile([128, 2, 16], F32, 'g12')
    T.op('dve', lambda e: e.tensor_copy(out=g12[:, 0, :], in_=modT[:, 32:48, 0]), reads=['modT'], writes=['g12'])
    T.op('dve', lambda e: e.tensor_copy(out=g12[:, 1, :], in_=modT[:, 80:96, 0]), reads=['modT'], writes=['g12'])
    T.dma('sp', lambda e: e.dma_start(out=grow_d.rearrange("r (t p) -> p r t", p=128), in_=g12[:], allow_slow_non_contiguous=True),
          reads=['g12'], writes=['grow_d'])
    if dbg:
        T.dma('sp', lambda e: e.dma_start(out=dbg_d['modT'], in_=modT[:]), reads=['modT'])
    T.pop()

    def norm_to_T(X, xk, A, B, row, dst, dk, col0, scr, ptr, pk_list, it, abk=('A1', 'modT')):
        junk, ss, XN = scr
        i2 = it % 2
        T.op('act', lambda e: e.activation(out=junk[i2][:], in_=X[:], func=AF.Square, accum_out=ss[i2][:]),
             reads=[xk], writes=[f'junk{i2}', f'ss{i2}'])
        T.op('act', lambda e: e.activation(out=ss[i2][:], in_=ss[i2][:], func=AF.Sqrt, scale=1.0 / D, bias=epsT[:, 0:1]),
             reads=[f'ss{i2}', 'epsT'], writes=[f'ss{i2}'])
        T.op('dve', lambda e: e.reciprocal(out=ss[i2][:], in_=ss[i2][:]), reads=[f'ss{i2}'], writes=[f'ss{i2}'])
        T.op('pool', lambda e: e.tensor_scalar(out=XN[i2][:], in0=X[:], scalar1=ss[i2][:, 0:1], scalar2=None, op0=ALU.mult),
             reads=[xk, f'ss{i2}'], writes=[f'XN{i2}'])
        for q in range(4):
            p = ptr[q % 2]
            pk = pk_list[q % 2]
            for jj in range(4):
                j = q * 4 + jj
                T.op('pe', lambda e, p=p, jj=jj, j=j: e.transpose(
                    out=p[:, jj * 128:(jj + 1) * 128], in_=XN[i2][:, j * 128:(j + 1) * 128], identity=identf[:]),
                    reads=[f'XN{i2}', 'identf'], writes=[pk])
            for jj in range(4):
                j = q * 4 + jj
                if jj % 2 == 0:
                    T.op('dve', lambda e, p=p, jj=jj, j=j: e.tensor_scalar(
                        out=dst[:, j, col0:col0 + 128], in0=p[:, jj * 128:(jj + 1) * 128],
                        scalar1=A[:, j, row:row + 1], scalar2=B[:, j, row:row + 1], op0=ALU.mult, op1=ALU.add),
                        reads=[pk] + list(abk), writes=[dk])
                else:
                    T.op('act', lambda e, p=p, jj=jj, j=j: e.activation(
                        out=dst[:, j, col0:col0 + 128], in_=p[:, jj * 128:(jj + 1) * 128], func=AF.Identity,
                        scale=A[:, j, row:row + 1], bias=B[:, j, row:row + 1]),
                        reads=[pk] + list(abk), writes=[dk])

    T.push()
    Xt = [T.tile([128, D], F32, f'X{i}') for i in range(2)]
    scr = ([T.tile([128, D], BF16, f'junk{i}') for i in range(2)],
           [T.tile([128, 1], F32, f'ss{i}') for i in range(2)],
           [T.tile([128, D], F32, f'XN{i}') for i in range(2)])
    ptr = [T.ptile([128, 512], F32, f'ptr{i}') for i in range(2)]
    aTg = [T.tile([128, 16, 512], BF16, f'aTg{i}') for i in range(2)]
    it = 0
    for g in range(17):
        nsub = 4 if g < 16 else 2
        ag = aTg[g % 2]
        agk = f'aTg{g % 2}'
        for sub in range(nsub):
            X = Xt[it % 2]
            xk = f'X{it % 2}'
            if g < 16:
                src = x_d[g * 512 + sub * 128: g * 512 + (sub + 1) * 128, :]
                row = 0
            else:
                src = ctx_d[sub * 128:(sub + 1) * 128, :]
                row = 1
            T.dma('sp', lambda e, X=X, src=src: e.dma_start(out=X[:], in_=src), writes=[xk])
            norm_to_T(X, xk, A1, modT[:, 0:16, :], row, ag, agk, sub * 128, scr, ptr, ['ptr0', 'ptr1'], it)
            it += 1
        ncol = nsub * 128
        dst = aT_d[:, :, g * 512: g * 512 + ncol].rearrange("j p t -> p j t")
        T.dma('sp', lambda e, ag=ag, dst=dst, ncol=ncol: e.dma_start(out=dst, in_=ag[:, :, 0:ncol]), reads=[agk], writes=['aT_d'])
    T.pop()

    def rev(ap2d, n):
        a = ap2d
        return bass.AP(tensor=a.tensor, offset=a.offset + (n - 1), ap=[list(a.ap[0]), [-1, n]])

    T.push()
    lcw = T.tile([128, 8, 4], F32, 'lcw'); lcb = T.tile([128, 8], F32, 'lcb')
    lba = T.tile([128, 2, 8], F32, 'lba'); lbi = T.tile([128, 2, 8], F32, 'lbi'); sca = T.tile([128, 2, 8], F32, 'sca')
    for t_, d_, k_ in ((lcw, lcw_d, 'lcw'), (lcb, lcb_d, 'lcb'), (lba, lba_d, 'lba'), (lbi, lbi_d, 'lbi'), (sca, llam_d, 'sca')):
        T.dma('sp', lambda e, t_=t_, d_=d_: e.dma_start(out=t_[:], in_=d_), writes=[k_])
    T.op('act', lambda e: e.activation(out=sca[:], in_=sca[:], func=AF.Exp, scale=-1.0), reads=['sca'], writes=['sca'])
    T.op('act', lambda e: e.activation(out=sca[:], in_=sca[:], func=AF.Ln, bias=oneT[:, 0:1]), reads=['sca', 'oneT'], writes=['sca'])
    T.op('dve', lambda e: e.tensor_scalar(out=sca[:], in0=sca[:], scalar1=-8.0, scalar2=None, op0=ALU.mult), reads=['sca'], writes=['sca'])

    RA = T.tile([128, ST], F32, 'RA'); UB = T.tile([128, ST], F32, 'UB')
    ub = T.tile([128, ST], BF16, 'ub'); gg = T.tile([128, S], BF16, 'gg')
    hs = T.tile([128, ST], F32, 'hs'); h2 = T.tile([128, ST], F32, 'h2')
    wrb = T.tile([128, 16, 128], BF16, 'wrb'); wgb = T.tile([128, 16, 128], BF16, 'wgb')
    gst = T.tile([128, 128], F32, 'gst')
    wab = [T.tile([128, 128], BF16, f'wab{d}') for d in range(2)]
    wib = [T.tile([128, 128], BF16, f'wib{d}') for d in range(2)]
    ag2 = [T.tile([128, 16, 512], BF16, 'ag0')]
    pp = [T.ptile([128, 512], F32, f'pp{i}') for i in range(4)]
    tmp = [T.tile([128, 512], F32, f'tmp{i}') for i in range(4)]

    for blk in range(8):
        for (wt, wk, c0) in ((wrb, 'wrb', blk * 128), (wgb, 'wgb', 1024 + blk * 128)):
            src = win_d[:, c0:c0 + 128].rearrange("(k p) f -> p k f", p=128)
            T.dma('pool', lambda e, src=src, wt=wt: e.dma_start(out=wt[:], in_=src), writes=[wk])
        for d in range(2):
            for (wt, wk, srcw) in ((wab[d], f'wab{d}', lwa_d), (wib[d], f'wib{d}', lwi_d)):
                T.dma('sp', lambda e, srcw=srcw, d=d: e.dma_start(out=gst[:], in_=srcw[d, blk]), writes=['gst'])
                T.op('dve', lambda e, wt=wt: e.tensor_copy(out=wt[:], in_=gst[:]), reads=['gst'], writes=[wk])
        for g in range(17):
            ncol = 512 if g < 16 else 256
            a = ag2[0]; ak = 'ag0'
            src = aT_d[:, :, g * 512: g * 512 + ncol].rearrange("j p t -> p j t")
            T.dma('sp', lambda e, a=a, src=src, ncol=ncol: e.dma_start(out=a[:, :, 0:ncol], in_=src), reads=['aT_d'], writes=[ak])
            p0 = pp[(2 * g) % 4]; p0k = f'pp{(2 * g) % 4}'
            for k in range(16):
                T.op('pe', lambda e, p0=p0, a=a, k=k, ncol=ncol: e.matmul(p0[:, 0:ncol], lhsT=wrb[:, k, :], rhs=a[:, k, 0:ncol],
                     start=(k == 0), stop=(k == 15)), reads=['wrb', ak], writes=[p0k])
            T.op('act', lambda e, p0=p0, g=g, ncol=ncol: e.activation(out=RA[:, g * 512: g * 512 + ncol], in_=p0[:, 0:ncol], func=AF.Copy),
                 reads=[p0k], writes=['RA'])
            if g < 16:
                p1 = pp[(2 * g + 1) % 4]; p1k = f'pp{(2 * g + 1) % 4}'
                for k in range(16):
                    T.op('pe', lambda e, p1=p1, a=a, k=k: e.matmul(p1[:, :], lhsT=wgb[:, k, :], rhs=a[:, k, :],
                         start=(k == 0), stop=(k == 15)), reads=['wgb', ak], writes=[p1k])
                T.op('act', lambda e, p1=p1, g=g: e.activation(out=gg[:, g * 512:(g + 1) * 512], in_=p1[:, :], func=AF.Gelu),
                     reads=[p1k], writes=['gg'])
        for (lo, n) in ((0, S), (S, CTX)):
            T.op('dve', lambda e, lo=lo, n=n: e.tensor_scalar(out=UB[:, lo:lo + n], in0=RA[:, lo:lo + n],
                 scalar1=lcw[:, blk, 2:3], scalar2=lcb[:, blk:blk + 1], op0=ALU.mult, op1=ALU.add),
                 reads=['RA', 'lcw', 'lcb'], writes=['UB'])
            for (tap, sh) in ((0, -2), (1, -1), (3, 1)):
                if sh < 0:
                    o_lo, o_n, i_lo = lo - sh, n + sh, lo
                else:
                    o_lo, o_n, i_lo = lo, n - sh, lo + sh
                T.op('dve', lambda e, tap=tap, o_lo=o_lo, o_n=o_n, i_lo=i_lo: e.scalar_tensor_tensor(
                    out=UB[:, o_lo:o_lo + o_n], in0=RA[:, i_lo:i_lo + o_n], scalar=lcw[:, blk, tap:tap + 1],
                    in1=UB[:, o_lo:o_lo + o_n], op0=ALU.mult, op1=ALU.add), reads=['RA', 'UB', 'lcw'], writes=['UB'])
        T.op('pool', lambda e: e.tensor_copy(out=ub[:], in_=UB[:]), reads=['UB'], writes=['ub'])
        for d in range(2):
            for g in range(17):
                ncol = 512 if g < 16 else 256
                c0 = g * 512
                pa = pp[(2 * g) % 4]; pak = f'pp{(2 * g) % 4}'
                pi_ = pp[(2 * g + 1) % 4]; pik = f'pp{(2 * g + 1) % 4}'
                T.op('pe', lambda e, pa=pa, c0=c0, ncol=ncol, d=d: e.matmul(pa[:, 0:ncol], lhsT=wab[d][:], rhs=ub[:, c0:c0 + ncol],
                     start=True, stop=True), reads=[f'wab{d}', 'ub'], writes=[pak])
                T.op('pe', lambda e, pi_=pi_, c0=c0, ncol=ncol, d=d: e.matmul(pi_[:, 0:ncol], lhsT=wib[d][:], rhs=ub[:, c0:c0 + ncol],
                     start=True, stop=True), reads=[f'wib{d}', 'ub'], writes=[pik])
                t0, t1, t2, t3 = tmp
                T.op('act', lambda e, pa=pa, ncol=ncol, d=d: e.activation(out=t0[:, 0:ncol], in_=pa[:, 0:ncol], func=AF.Sigmoid,
                     bias=lba[:, d, blk:blk + 1]), reads=[pak, 'lba'], writes=['tmp0'])
                T.op('act', lambda e, pi_=pi_, ncol=ncol, d=d: e.activation(out=t1[:, 0:ncol], in_=pi_[:, 0:ncol], func=AF.Sigmoid,
                     bias=lbi[:, d, blk:blk + 1]), reads=[pik, 'lbi'], writes=['tmp1'])
                T.op('act', lambda e, c0=c0, ncol=ncol, d=d: e.activation(out=RA[:, c0:c0 + ncol], in_=t0[:, 0:ncol], func=AF.Exp,
                     scale=sca[:, d, blk:blk + 1]), reads=['tmp0', 'sca'], writes=['RA'])
                T.op('act', lambda e, c0=c0, ncol=ncol: e.activation(out=t2[:, 0:ncol], in_=RA[:, c0:c0 + ncol], func=AF.Square),
                     reads=['RA'], writes=['tmp2'])
                T.op('act', lambda e, ncol=ncol: e.activation(out=t2[:, 0:ncol], in_=t2[:, 0:ncol], func=AF.Sqrt, scale=-1.0,
                     bias=oneT[:, 0:1]), reads=['tmp2', 'oneT'], writes=['tmp2'])
                T.op('dve', lambda e, c0=c0, ncol=ncol: e.tensor_tensor(out=t3[:, 0:ncol], in0=t1[:, 0:ncol], in1=ub[:, c0:c0 + ncol], op=ALU.mult),
                     reads=['tmp1', 'ub'], writes=['tmp3'])
                T.op('pool', lambda e, c0=c0, ncol=ncol: e.tensor_tensor(out=UB[:, c0:c0 + ncol], in0=t3[:, 0:ncol], in1=t2[:, 0:ncol], op=ALU.mult),
                     reads=['tmp3', 'tmp2'], writes=['UB'])
            H = hs if d == 0 else h2
            hk = 'hs' if d == 0 else 'h2'
            if d == 0:
                T.op('dve', lambda e: e.tensor_tensor_scan(out=H[:, S:ST], data0=RA[:, S:ST], data1=UB[:, S:ST], initial=0.0,
                     op0=ALU.mult, op1=ALU.add), reads=['RA', 'UB'], writes=[hk])
                T.op('dve', lambda e: e.tensor_tensor_scan(out=H[:, 0:S], data0=RA[:, 0:S], data1=UB[:, 0:S], initial=H[:, ST - 1:ST],
                     op0=ALU.mult, op1=ALU.add), reads=['RA', 'UB', hk], writes=[hk])
            else:
                T.op('dve', lambda e: e.tensor_tensor_scan(out=rev(H[:, S:ST], CTX), data0=rev(RA[:, S:ST], CTX), data1=rev(UB[:, S:ST], CTX),
                     initial=0.0, op0=ALU.mult, op1=ALU.add), reads=['RA', 'UB'], writes=[hk])
                T.op('dve', lambda e: e.tensor_tensor_scan(out=rev(H[:, 0:S], S), data0=rev(RA[:, 0:S], S), data1=rev(UB[:, 0:S], S),
                     initial=H[:, S:S + 1], op0=ALU.mult, op1=ALU.add), reads=['RA', 'UB', hk], writes=[hk])
        T.op('pool', lambda e: e.tensor_tensor(out=hs[:, 0:S], in0=hs[:, 0:S], in1=h2[:, 0:S], op=ALU.add), reads=['hs', 'h2'], writes=['hs'])
        T.op('dve', lambda e: e.tensor_tensor(out=hs[:, 0:S], in0=hs[:, 0:S], in1=gg[:, :], op=ALU.mult), reads=['hs', 'gg'], writes=['hs'])
        T.op('pool', lambda e: e.tensor_copy(out=gg[:, :], in_=hs[:, 0:S]), reads=['hs'], writes=['gg'])
        T.dma('sp', lambda e, blk=blk: e.dma_start(out=mix_d[blk], in_=gg[:, :]), reads=['gg'], writes=['mix_d'])
    T.pop()


    def bc_mid(ap2d, n):
        a = ap2d
        return bass.AP(tensor=a.tensor, offset=a.offset, ap=[list(a.ap[0]), [0, n], list(a.ap[1])])

    T.push()
    identb = T.tile([128, 128], BF16, 'identb'); f1m = T.tile([64, 130], BF16, 'f1m'); twf = T.tile([128, 130], F32, 'twf')
    csn = T.tile([128, 3, 128], BF16, 'csn'); csh = T.tile([128, 2, 2, 128], BF16, 'csh')
    twi = T.tile([65, 2, 128], F32, 'twi'); eri = T.tile([65, 2, 64], BF16, 'eri')
    for t_, d_, k_ in ((identb, identb_d, 'identb'), (f1m, f1_d, 'f1m'), (twf, twf_d, 'twf'), (csn, csn_d, 'csn'),
                       (csh, csh_d, 'csh'), (twi, twi_d, 'twi'), (eri, eri_d, 'eri')):
        T.dma('sp', lambda e, t_=t_, d_=d_: e.dma_start(out=t_[:], in_=d_), writes=[k_])
    UC = T.tile([128, 16384], BF16, 'UC')
    AY = T.tile([128, 2, 65, 128], BF16, 'AY')
    KF = T.tile([128, 2, 65, 128], BF16, 'KF')
    U = UC[0:64, :].rearrange("p (c n) -> p c n", c=128)
    CPP = UC[0:65, :].rearrange("p (r n c) -> p r n c", r=2, n=64)
    AYK = [f'AY{i}' for i in range(17)]
    KGS = [(4 * i, 4) for i in range(16)] + [(64, 1)]
    ftmp = [T.tile([128, 512], F32, f'ft{i}') for i in range(4)]

    def fft_forward(u, uk, mode, dbc=None):
        T.push()
        pt = [T.ptile([64, 8, 128], BF16, f'pt{i}') for i in range(2)]
        pa = [T.ptile([128, 3, 130], F32, f'pa{i}') for i in range(2)]
        px = [T.ptile([128, 512], F32, f'px{i}') for i in range(2)]
        tw = [T.tile([128, 3, 65], F32, f'tw{i}') for i in range(4)]
        for q in range(16):
            p = pt[q % 2]; pk = f'pt{q % 2}'
            for i in range(8):
                n2 = q * 8 + i
                T.op('pe', lambda e, p=p, i=i, n2=n2: e.transpose(out=p[:, i, :], in_=u[:, n2 * 64:(n2 + 1) * 64], identity=identb[:]),
                     reads=[uk, 'identb'], writes=[pk])
            dst = U[:, :, q * 8:(q + 1) * 8].rearrange("p c n -> p n c")
            if q % 2 == 0:
                T.op('act', lambda e, p=p, dst=dst: e.activation(out=dst, in_=p[:, :, :], func=AF.Copy), reads=[pk], writes=['UC'])
            else:
                T.op('dve', lambda e, p=p, dst=dst: e.tensor_copy(out=dst, in_=p[:, :, :]), reads=[pk], writes=['UC'])
        ngr = 43
        for gi in range(ngr):
            c0 = gi * 3; nch = min(3, 128 - c0)
            p = pa[gi % 2]; pk = f'pa{gi % 2}'
            for i in range(nch):
                T.op('pe', lambda e, p=p, i=i, c=c0 + i: e.matmul(p[:, i, :], lhsT=U[:, c, :], rhs=f1m[:, :], start=True, stop=True),
                     reads=['UC', 'f1m'], writes=[pk])
            Pr = p[:, 0:nch, 0:65]; Pi = p[:, 0:nch, 65:130]
            twr = bc_mid(twf[:, 0:65], nch); twim = bc_mid(twf[:, 65:130], nch)
            t1, t2, t3, t4 = [t[:, 0:nch, :] for t in tw]
            T.op('dve', lambda e, t1=t1, Pr=Pr, twr=twr: e.tensor_tensor(out=t1, in0=Pr, in1=twr, op=ALU.mult), reads=[pk, 'twf'], writes=['tw0'])
            T.op('dve', lambda e, t2=t2, Pi=Pi, twim=twim: e.tensor_tensor(out=t2, in0=Pi, in1=twim, op=ALU.mult), reads=[pk, 'twf'], writes=['tw1'])
            T.op('dve', lambda e, t3=t3, Pr=Pr, twim=twim: e.tensor_tensor(out=t3, in0=Pr, in1=twim, op=ALU.mult), reads=[pk, 'twf'], writes=['tw2'])
            T.op('dve', lambda e, t4=t4, Pi=Pi, twr=twr: e.tensor_tensor(out=t4, in0=Pi, in1=twr, op=ALU.mult), reads=[pk, 'twf'], writes=['tw3'])
            dr = AY[:, 0, :, c0:c0 + nch].rearrange("p k c -> p c k"); di = AY[:, 1, :, c0:c0 + nch].rearrange("p k c -> p c k")
            T.op('pool', lambda e, dr=dr, t1=t1, t2=t2: e.tensor_tensor(out=dr, in0=t1, in1=t2, op=ALU.subtract), reads=['tw0', 'tw1'], writes=AYK)
            T.op('pool', lambda e, di=di, t3=t3, t4=t4: e.tensor_tensor(out=di, in0=t3, in1=t4, op=ALU.add), reads=['tw2', 'tw3'], writes=AYK)
        for gi, (k0, nk) in enumerate(KGS):
            ncol = nk * 128
            pr = px[0]; prk = 'px0'
            pi_ = px[1]; pik = 'px1'
            Ar = AY[:, 0, k0:k0 + nk, :]; Ai = AY[:, 1, k0:k0 + nk, :]
            ak = AYK[gi]
            T.op('pe', lambda e, pr=pr, Ar=Ar, ncol=ncol: e.matmul(pr[:, 0:ncol], lhsT=csn[:, 0, :], rhs=Ar, start=True, stop=False), reads=[ak, 'csn'], writes=[prk])
            T.op('pe', lambda e, pr=pr, Ai=Ai, ncol=ncol: e.matmul(pr[:, 0:ncol], lhsT=csn[:, 1, :], rhs=Ai, start=False, stop=True), reads=[ak, 'csn'], writes=[prk])
            T.op('pe', lambda e, pi_=pi_, Ai=Ai, ncol=ncol: e.matmul(pi_[:, 0:ncol], lhsT=csn[:, 0, :], rhs=Ai, start=True, stop=False), reads=[ak, 'csn'], writes=[pik])
            T.op('pe', lambda e, pi_=pi_, Ar=Ar, ncol=ncol: e.matmul(pi_[:, 0:ncol], lhsT=csn[:, 2, :], rhs=Ar, start=False, stop=True), reads=[ak, 'csn'], writes=[pik])
            Xr = pr[:, 0:ncol].rearrange("p (k c) -> p k c", c=128); Xi = pi_[:, 0:ncol].rearrange("p (k c) -> p k c", c=128)
            Kr = KF[:, 0, k0:k0 + nk, :]; Ki = KF[:, 1, k0:k0 + nk, :]
            if mode == 'kf0':
                T.op('dve', lambda e, Kr=Kr, Xr=Xr, nk=nk: e.tensor_tensor(out=Kr, in0=Xr, in1=bc_mid(dbc[:, :], nk), op=ALU.add), reads=[prk, 'dbc'], writes=['KF'])
                T.op('act', lambda e, Ki=Ki, Xi=Xi: e.activation(out=Ki, in_=Xi, func=AF.Copy), reads=[pik], writes=['KF'])
            elif mode == 'kf1':
                T.op('dve', lambda e, Kr=Kr, Xr=Xr: e.tensor_tensor(out=Kr, in0=Kr, in1=Xr, op=ALU.add), reads=[prk, 'KF'], writes=['KF'])
                T.op('dve', lambda e, Ki=Ki, Xi=Xi: e.tensor_tensor(out=Ki, in0=Ki, in1=Xi, op=ALU.subtract), reads=[pik, 'KF'], writes=['KF'])
            else:
                f1_, f2_, f3_, f4_ = [t[:, 0:ncol].rearrange("p (k c) -> p k c", c=128) for t in ftmp]
                T.op('dve', lambda e, f1_=f1_, Xr=Xr, Kr=Kr: e.tensor_tensor(out=f1_, in0=Xr, in1=Kr, op=ALU.mult), reads=[prk, 'KF'], writes=['ft0'])
                T.op('dve', lambda e, f2_=f2_, Xi=Xi, Ki=Ki: e.tensor_tensor(out=f2_, in0=Xi, in1=Ki, op=ALU.mult), reads=[pik, 'KF'], writes=['ft1'])
                T.op('dve', lambda e, f3_=f3_, Xr=Xr, Ki=Ki: e.tensor_tensor(out=f3_, in0=Xr, in1=Ki, op=ALU.mult), reads=[prk, 'KF'], writes=['ft2'])
                T.op('dve', lambda e, f4_=f4_, Xi=Xi, Kr=Kr: e.tensor_tensor(out=f4_, in0=Xi, in1=Kr, op=ALU.mult), reads=[pik, 'KF'], writes=['ft3'])
                T.op('pool', lambda e, Ar=Ar, f1_=f1_, f2_=f2_: e.tensor_tensor(out=Ar, in0=f1_, in1=f2_, op=ALU.subtract), reads=['ft0', 'ft1'], writes=[ak])
                T.op('pool', lambda e, Ai=Ai, f3_=f3_, f4_=f4_: e.tensor_tensor(out=Ai, in0=f3_, in1=f4_, op=ALU.add), reads=['ft2', 'ft3'], writes=[ak])
        T.pop()

    def fft_inverse(gate, gk, dst, dk):
        T.push()
        pc = [T.ptile([65, 4, 128], F32, f'pc{i}') for i in range(2)]
        py = [T.ptile([128, 8, 64], F32, f'py{i}') for i in range(2)]
        tw = [T.tile([65, 4, 64], F32, f'iw{i}') for i in range(4)]
        for h in range(2):
            for gi in range(32):
                c0 = gi * 4
                p = pc[gi % 2]; pk = f'pc{gi % 2}'
                for i in range(4):
                    c = c0 + i
                    T.op('pe', lambda e, p=p, i=i, c=c: e.matmul(p[:, i, :], lhsT=AY[:, 0, :, c], rhs=csh[:, 0, h, :], start=True, stop=False),
                         reads=AYK + ['csh'], writes=[pk])
                    T.op('pe', lambda e, p=p, i=i, c=c: e.matmul(p[:, i, :], lhsT=AY[:, 1, :, c], rhs=csh[:, 1, h, :], start=False, stop=True),
                         reads=AYK + ['csh'], writes=[pk])
                Pr = p[:, :, 0:64]; Pi = p[:, :, 64:128]
                ct = bc_mid(twi[:, 0, h * 64:(h + 1) * 64], 4); st = bc_mid(twi[:, 1, h * 64:(h + 1) * 64], 4)
                t1, t2, t3, t4 = [t[:, :, :] for t in tw]
                T.op('dve', lambda e, t1=t1, Pr=Pr, ct=ct: e.tensor_tensor(out=t1, in0=Pr, in1=ct, op=ALU.mult), reads=[pk, 'twi'], writes=['iw0'])
                T.op('dve', lambda e, t2=t2, Pi=Pi, st=st: e.tensor_tensor(out=t2, in0=Pi, in1=st, op=ALU.mult), reads=[pk, 'twi'], writes=['iw1'])
                T.op('dve', lambda e, t3=t3, Pr=Pr, st=st: e.tensor_tensor(out=t3, in0=Pr, in1=st, op=ALU.mult), reads=[pk, 'twi'], writes=['iw2'])
                T.op('dve', lambda e, t4=t4, Pi=Pi, ct=ct: e.tensor_tensor(out=t4, in0=Pi, in1=ct, op=ALU.mult), reads=[pk, 'twi'], writes=['iw3'])
                dr = CPP[:, 0, :, c0:c0 + 4].rearrange("p n c -> p c n"); di = CPP[:, 1, :, c0:c0 + 4].rearrange("p n c -> p c n")
                T.op('pool', lambda e, dr=dr, t1=t1, t2=t2: e.tensor_tensor(out=dr, in0=t1, in1=t2, op=ALU.subtract), reads=['iw0', 'iw1'], writes=['UC'])
                T.op('pool', lambda e, di=di, t3=t3, t4=t4: e.tensor_tensor(out=di, in0=t3, in1=t4, op=ALU.add), reads=['iw2', 'iw3'], writes=['UC'])
            for q in range(8):
                p = py[q % 2]; pk = f'py{q % 2}'
                for i in range(8):
                    nl = q * 8 + i
                    T.op('pe', lambda e, p=p, i=i, nl=nl: e.matmul(p[:, i, :], lhsT=CPP[:, 0, nl, :], rhs=eri[:, 0, :], start=True, stop=False),
                         reads=['UC', 'eri'], writes=[pk])
                    T.op('pe', lambda e, p=p, i=i, nl=nl: e.matmul(p[:, i, :], lhsT=CPP[:, 1, nl, :], rhs=eri[:, 1, :], start=False, stop=True),
                         reads=['UC', 'eri'], writes=[pk])
                tok0 = (h * 64 + q * 8) * 64
                T.op('dve', lambda e, p=p, tok0=tok0: e.tensor_tensor(out=dst[:, tok0:tok0 + 512], in0=p[:, :, :].rearrange("p a b -> p (a b)"),
                     in1=gate[:, tok0:tok0 + 512], op=ALU.mult), reads=[pk, gk], writes=[dk])
        T.pop()

    T.push()
    h3T = T.tile([64, S], F32, 'h3T')
    hbT = T.tile([64, 4], F32, 'hbT'); frb = T.tile([64, 3], F32, 'frb')
    T.dma('sp', lambda e: e.dma_start(out=hbT[:], in_=hb_d), writes=['hbT'])
    for l in range(3):
        T.op('dve', lambda e, l=l: e.tensor_tensor(out=frb[:, l:l + 1], in0=hbT[:, l:l + 1], in1=hbT[:, 3:4], op=ALU.mult), reads=['hbT'], writes=['frb'])
    T.push()
    zT = T.tile([33, S], F32, 'zT')
    w1 = T.tile([33, 64], F32, 'w1'); w2 = T.tile([64, 64], F32, 'w2'); w3 = T.tile([64, 64], F32, 'w3')
    for t_, d_, k_ in ((zT, zT_d, 'zT'), (w1, hw1_d, 'w1'), (w2, hw2_d, 'w2'), (w3, hw3_d, 'w3')):
        T.dma('sp', lambda e, t_=t_, d_=d_: e.dma_start(out=t_[:], in_=d_), writes=[k_])
    pm_ = [T.ptile([64, 512], F32, f'pml{i}') for i in range(2)]
    ha = [T.tile([64, 512], F32, f'ha{i}') for i in range(2)]
    PI_ = 3.14159
    for cc in range(16):
        cs = slice(cc * 512, (cc + 1) * 512)
        cur = zT[:, cs]; curk = 'zT'
        for l, (wl, wk) in enumerate(((w1, 'w1'), (w2, 'w2'), (w3, 'w3'))):
            p = pm_[l % 2]; pk = f'pml{l % 2}'
            T.op('pe', lambda e, p=p, wl=wl, cur=cur: e.matmul(p[:, :], lhsT=wl[:, :], rhs=cur, start=True, stop=True), reads=[wk, curk], writes=[pk])
            dstt = ha[l % 2][:, :] if l < 2 else h3T[:, cs]
            dk = f'ha{l % 2}' if l < 2 else 'h3T'
            T.op('dve', lambda e, p=p, dstt=dstt, l=l: e.tensor_scalar(out=dstt, in0=p[:, :], scalar1=hbT[:, 3:4], scalar2=frb[:, l:l + 1],
                 op0=ALU.mult, op1=ALU.add), reads=[pk, 'hbT', 'frb'], writes=[dk])
            T.op('dve', lambda e, dstt=dstt: e.tensor_scalar(out=dstt, in0=dstt, scalar1=PI_, scalar2=-PI_, op0=ALU.min, op1=ALU.max), reads=[dk], writes=[dk])
            T.op('act', lambda e, dstt=dstt: e.activation(out=dstt, in_=dstt, func=AF.Sin), reads=[dk], writes=[dk])
            cur = dstt; curk = dk
    T.pop()
    dec = T.tile([128, S], BF16, 'dec'); hch = T.tile([128, S], BF16, 'hch')
    w4t = T.tile([64, 128], F32, 'w4t'); dbc = T.tile([128, 128], F32, 'dbc')
    pf = [T.ptile([128, 512], F32, f'pf{i}') for i in range(2)]
    for ch in range(8):
        T.dma('sp', lambda e, ch=ch: e.dma_start(out=dec[:], in_=dec_d[ch]), writes=['dec'])
        for o in range(2):
            T.dma('sp', lambda e, o=o: e.dma_start(out=dbc[:], in_=hyd_d[o:o + 1, ch * 128:(ch + 1) * 128].partition_broadcast(128)), writes=['dbc'])
            for d in range(2):
                col = o * 2048 + d * 1024 + ch * 128
                T.dma('sp', lambda e, col=col: e.dma_start(out=w4t[:], in_=hw4_d[:, col:col + 128]), writes=['w4t'])
                for cc in range(16):
                    p = pf[cc % 2]; pk = f'pf{cc % 2}'
                    T.op('pe', lambda e, p=p, cc=cc: e.matmul(p[:, :], lhsT=w4t[:, :], rhs=h3T[:, cc * 512:(cc + 1) * 512], start=True, stop=True),
                         reads=['w4t', 'h3T'], writes=[pk])
                    T.op('dve', lambda e, p=p, cc=cc: e.tensor_tensor(out=hch[:, cc * 512:(cc + 1) * 512], in0=p[:, :], in1=dec[:, cc * 512:(cc + 1) * 512],
                         op=ALU.mult), reads=[pk, 'dec'], writes=['hch'])
                if d == 1:
                    T.op('pool', lambda e: e.memset(hch[:, 0:1], 0.0), reads=['hch'], writes=['hch'])
                fft_forward(hch, 'hch', 'kf0' if d == 0 else 'kf1', dbc)
            T.dma('sp', lambda e, o=o: e.dma_start(out=kf_d[o, ch], in_=KF[:]), reads=['KF'], writes=['kf_d'])
    T.pop()


    T.push()
    Bb = [T.tile([128, S], BF16, f'B{i}') for i in range(4)]
    hcw = T.tile([128, 24, 3], F32, 'hcw'); hcb = T.tile([128, 24], F32, 'hcb')
    T.dma('sp', lambda e: e.dma_start(out=hcw[:], in_=hcw_d), writes=['hcw'])
    T.dma('sp', lambda e: e.dma_start(out=hcb[:], in_=hcb_d), writes=['hcb'])
    hag = T.tile([128, 16, 512], BF16, 'hag')
    hwb = [T.tile([128, 16, 128], BF16, f'hwb{i}') for i in range(3)]
    for ch in range(8):
        for sig in range(3):
            c0 = 2048 + sig * 1024 + ch * 128
            src = win_d[:, c0:c0 + 128].rearrange("(k p) f -> p k f", p=128)
            T.dma('pool', lambda e, src=src, sig=sig: e.dma_start(out=hwb[sig][:], in_=src), writes=[f'hwb{sig}'])
        T.push()
        hp = [T.ptile([128, 512], F32, f'hp{i}') for i in range(4)]
        cnt = 0
        for g in range(16):
            src = aT_d[:, :, g * 512:(g + 1) * 512].rearrange("j p t -> p j t")
            T.dma('sp', lambda e, src=src: e.dma_start(out=hag[:], in_=src), reads=['aT_d'], writes=['hag'])
            for sig in range(3):
                p = hp[cnt % 4]; pk = f'hp{cnt % 4}'; cnt += 1
                for k in range(16):
                    T.op('pe', lambda e, p=p, sig=sig, k=k: e.matmul(p[:, :], lhsT=hwb[sig][:, k, :], rhs=hag[:, k, :], start=(k == 0), stop=(k == 15)),
                         reads=[f'hwb{sig}', 'hag'], writes=[pk])
                T.op('act', lambda e, p=p, sig=sig, g=g: e.activation(out=Bb[sig][:, g * 512:(g + 1) * 512], in_=p[:, :], func=AF.Copy),
                     reads=[pk], writes=[f'B{sig}'])
        T.pop()
        for sig, (si, di) in enumerate(((0, 3), (1, 0), (2, 1))):
            t = sig * 8 + ch
            src = Bb[si]; dst = Bb[di]; sk = f'B{si}'; dk = f'B{di}'
            T.op('dve', lambda e, src=src, dst=dst, t=t: e.tensor_scalar(out=dst[:, :], in0=src[:, :], scalar1=hcw[:, t, 1:2], scalar2=hcb[:, t:t + 1],
                 op0=ALU.mult, op1=ALU.add), reads=[sk, 'hcw', 'hcb'], writes=[dk])
            for (tap, o_lo, i_lo, n) in ((0, 64, 0, S - 64), (2, 0, 64, S - 64), (0, 1, S - 64, 63), (2, S - 64, 1, 63)):
                T.op('dve', lambda e, src=src, dst=dst, t=t, tap=tap, o_lo=o_lo, i_lo=i_lo, n=n: e.scalar_tensor_tensor(
                    out=dst[:, o_lo:o_lo + n], in0=src[:, i_lo:i_lo + n], scalar=hcw[:, t, tap:tap + 1], in1=dst[:, o_lo:o_lo + n],
                    op0=ALU.mult, op1=ALU.add), reads=[sk, dk, 'hcw'], writes=[dk])
        T.dma('sp', lambda e, ch=ch: e.dma_start(out=KF[:], in_=kf_d[0, ch]), reads=['kf_d'], writes=['KF'])
        fft_forward(Bb[3], 'B3', 'mul')
        fft_inverse(Bb[0], 'B0', Bb[2], 'B2')
        T.dma('sp', lambda e, ch=ch: e.dma_start(out=KF[:], in_=kf_d[1, ch]), reads=['kf_d'], writes=['KF'])
        fft_forward(Bb[2], 'B2', 'mul')
        fft_inverse(Bb[1], 'B1', Bb[3], 'B3')
        T.dma('sp', lambda e, ch=ch: e.dma_start(out=mix_d[8 + ch], in_=Bb[3][:, :]), reads=['B3'], writes=['mix_d'])
    T.pop()
    T.pop()


    T.push()
    wob = T.tile([128, 16, D], BF16, 'wob')
    for dc in range(4):
        src = wout_d[:, dc * 512:(dc + 1) * 512].rearrange("(k p) f -> p k f", p=128)
        T.dma('pool', lambda e, src=src, dc=dc: e.dma_start(out=wob[:, :, dc * 512:(dc + 1) * 512], in_=src), writes=['wob'])
    gnT = T.tile([128, 16], F32, 'gnT'); G1bc = T.tile([128, D], F32, 'G1bc'); onesb = T.tile([128, 128], BF16, 'onesb')
    T.dma('sp', lambda e: e.dma_start(out=gnT[:], in_=gnT_d), writes=['gnT'])
    T.dma('sp', lambda e: e.dma_start(out=G1bc[:], in_=grow_d[0:1, :].partition_broadcast(128)), reads=['grow_d'], writes=['G1bc'])
    T.op('pool', lambda e: e.memset(onesb[:], 1.0), writes=['onesb'])
    mixg = T.tile([128, 16, 512], BF16, 'mixg'); mixn = T.tile([128, 16, 512], BF16, 'mixn')
    sqb = [T.tile([128, 512], BF16, f'sqb{i}') for i in range(2)]
    rs = T.tile([128, 2, 512], F32, 'rs')
    prs = [T.ptile([128, 512], F32, f'prs{i}') for i in range(2)]
    po = [T.ptile([128, 512], F32, f'po{i}') for i in range(2)]
    ptr3 = [T.ptile([128, 512], F32, f'ptr3{i}') for i in range(2)]
    X3 = [T.tile([128, D], F32, f'X3{i}') for i in range(2)]
    H3 = [T.tile([128, D], F32, f'H3{i}') for i in range(2)]
    scr3 = ([T.tile([128, D], BF16, f'junk{i}') for i in range(2)],
            [T.tile([128, 1], F32, f'ss{i}') for i in range(2)],
            [T.tile([128, D], F32, f'XN{i}') for i in range(2)])
    mTg = [T.tile([128, 16, 512], BF16, f'mTg{i}') for i in range(2)]
    it = 0
    offreg = T.es.enter_context(nc.sync.register("offreg"))
    for g in range(SL // 512):
        def ld_mix(e, g=g):
            e.reg_load(offreg, offs_d[0:1, g:g + 1])
            v = e.snap(offreg)
            return e.dma_start(out=mixg[:], in_=mix_d[:, :, bass.ds(v, 512)].rearrange("j p t -> p j t"))
        T.dma('sp', ld_mix, reads=['mix_d'], writes=['mixg'])
        for grp in range(2):
            for jj in range(8):
                j = grp * 8 + jj
                sq = sqb[j % 2]; sk = f'sqb{j % 2}'
                T.op('act', lambda e, sq=sq, j=j: e.activation(out=sq[:], in_=mixg[:, j, :], func=AF.Square), reads=['mixg'], writes=[sk])
                T.op('pe', lambda e, sq=sq, grp=grp, jj=jj: e.matmul(prs[grp][:, :], lhsT=onesb[:, :], rhs=sq[:, :], start=(jj == 0), stop=(jj == 7)),
                     reads=[sk, 'onesb'], writes=[f'prs{grp}'])
            T.op('act', lambda e, grp=grp: e.activation(out=rs[:, grp, :], in_=prs[grp][:, :], func=AF.Sqrt, scale=1.0 / 1024, bias=epsT[:, 0:1]),
                 reads=[f'prs{grp}', 'epsT'], writes=['rs'])
            T.op('dve', lambda e, grp=grp: e.reciprocal(out=rs[:, grp, :], in_=rs[:, grp, :]), reads=['rs'], writes=['rs'])
        for j in range(16):
            T.op('dve', lambda e, j=j: e.scalar_tensor_tensor(out=mixn[:, j, :], in0=mixg[:, j, :], scalar=gnT[:, j:j + 1], in1=rs[:, j // 8, :],
                 op0=ALU.mult, op1=ALU.mult), reads=['mixg', 'gnT', 'rs'], writes=['mixn'])
        mg = mTg[g % 2]; mgk = f'mTg{g % 2}'
        for sub in range(4):
            X = X3[it % 2]; xk = f'X3{it % 2}'; H = H3[it % 2]; hk = f'H3{it % 2}'
            r0 = g * 512 + sub * 128
            T.dma('sp', lambda e, X=X, r0=r0: e.dma_start(out=X[:], in_=xh_d[r0:r0 + 128, :]), writes=[xk])
            for dc in range(4):
                p = po[dc % 2]; pk = f'po{dc % 2}'
                for k in range(16):
                    T.op('pe', lambda e, p=p, k=k, sub=sub, dc=dc: e.matmul(p[:, :], lhsT=mixn[:, k, sub * 128:(sub + 1) * 128],
                         rhs=wob[:, k, dc * 512:(dc + 1) * 512], start=(k == 0), stop=(k == 15)), reads=['mixn', 'wob'], writes=[pk])
                T.op('dve', lambda e, p=p, H=H, dc=dc: e.tensor_tensor(out=H[:, dc * 512:(dc + 1) * 512], in0=p[:, :], in1=G1bc[:, dc * 512:(dc + 1) * 512],
                     op=ALU.mult), reads=[pk, 'G1bc'], writes=[hk])
            T.op('pool', lambda e, H=H, X=X: e.tensor_tensor(out=H[:], in0=H[:], in1=X[:], op=ALU.add), reads=[hk, xk], writes=[hk])
            T.dma('sp', lambda e, H=H, r0=r0: e.dma_start(out=hx1_d[r0:r0 + 128, :], in_=H[:]), reads=[hk], writes=['hx1_d'])
            norm_to_T(H, hk, A2, modT[:, 48:64, :], 0, mg, mgk, sub * 128, scr3, ptr3, ['ptr30', 'ptr31'], it, abk=('A2', 'modT'))
            it += 1
        dst = mT_d[:, :, g * 512:(g + 1) * 512].rearrange("j p t -> p j t")
        T.dma('sp', lambda e, mg=mg, dst=dst: e.dma_start(out=dst, in_=mg[:]), reads=[mgk], writes=['mT_d'])
    T.pop()

    TC = 1024
    T.push()
    PS = [T.ptile([128, 512], F32, f'PS{i}') for i in range(8)]
    mTc = T.tile([128, 16, TC], BF16, 'mTc'); acc = T.tile([128, 16, TC], F32, 'acc'); actb = T.tile([128, 16, TC], BF16, 'actb')
    wst = [T.tile([128, 8, 256], F32, f'wst{i}') for i in range(2)]
    wr = [T.tile([128, 16, 256], BF16, f'wr{i}') for i in range(4)]
    gT = T.tile([32, TC], F32, 'gT'); gbc = T.tile([128, TC], BF16, 'gbc')
    mt = [T.tile([128, 512], F32, f'mt{i}') for i in range(3)]
    bguT = T.tile([128, 32, 32], F32, 'bguT'); BD = T.tile([32, D], F32, 'BD')
    rwb = T.tile([128, 16, 32], BF16, 'rwb'); RBbc = T.tile([128, 32], F32, 'RBbc')
    T.dma('sp', lambda e: e.dma_start(out=bguT[:], in_=bguT_d), writes=['bguT'])
    T.dma('sp', lambda e: e.dma_start(out=BD[:], in_=bd_d), writes=['BD'])
    T.dma('pool', lambda e: e.dma_start(out=rwb[:], in_=rw_d.rearrange("(k p) f -> p k f", p=128)), writes=['rwb'])
    T.dma('sp', lambda e: e.dma_start(out=RBbc[:], in_=rb_d.partition_broadcast(128)), writes=['RBbc'])
    Lg = T.tile([128, 32], F32, 'Lg'); Eg = T.tile([128, 32], F32, 'Eg'); v8 = T.tile([128, 8], F32, 'v8'); sm = T.tile([128, 2], F32, 'sm')
    wcount = [0]
    def f32view(ap3):
        return ap3.bitcast(F32).rearrange("p a b -> p (a b)")
    hx_t = [f32view(actb[:, 0:4, :]), f32view(actb[:, 4:8, :])]
    mo_t = f32view(actb[:, 8:12, :])
    G2bc = f32view(actb[:, 12:16, :])
    finbc = wst[0][:, :, :].rearrange("p a b -> p (a b)")

    def load_piece(src_fn, pid, first):
        r = wcount[0] % 4
        wt = wr[r]; wk = f'wr{r}'
        if not first:
            T.dma('sp', lambda e, wt=wt, pid=pid: e.dma_start(out=wt[:].rearrange("p k f -> p (k f)"), in_=wbf_l[pid // 192][pid % 192]), reads=[f'wbf{pid}'], writes=[wk])
            wcount[0] += 1
            return wt, wk
        for half in range(2):
            stg = wst[(2 * wcount[0] + half) % 2]; sk = f'wst{(2 * wcount[0] + half) % 2}'
            T.dma('sp', lambda e, stg=stg, half=half: e.dma_start(out=stg[:], in_=src_fn(half)), writes=[sk])
            eng = ('act', 'dve', 'pool')[(2 * wcount[0] + half) % 3]
            if eng == 'act':
                T.op('act', lambda e, wt=wt, stg=stg, half=half: e.activation(out=wt[:, half * 8:(half + 1) * 8, :], in_=stg[:], func=AF.Copy), reads=[sk], writes=[wk])
            else:
                T.op(eng, lambda e, wt=wt, stg=stg, half=half: e.tensor_copy(out=wt[:, half * 8:(half + 1) * 8, :], in_=stg[:]), reads=[sk], writes=[wk])
        T.dma('pool', lambda e, wt=wt, pid=pid: e.dma_start(out=wbf_l[pid // 192][pid % 192], in_=wt[:].rearrange("p k f -> p (k f)")), reads=[wk], writes=[f'wbf{pid}'])
        wcount[0] += 1
        return wt, wk

    def bc_free(ap_col, n):
        a = ap_col
        return bass.AP(tensor=a.tensor, offset=a.offset, ap=[list(a.ap[0]), [0, n]])

    for chk in range(SL // TC):
        t0 = chk * TC
        T.dma('sp', lambda e, t0=t0: e.dma_start(out=mTc[:], in_=mT_d[:, :, t0:t0 + TC].rearrange("j p t -> p j t")), reads=['mT_d'], writes=['mTc'])
        for sub in range(TC // 128):
            for k in range(16):
                T.op('pe', lambda e, k=k, sub=sub: e.matmul(PS[0][:, 0:32], lhsT=mTc[:, k, sub * 128:(sub + 1) * 128], rhs=rwb[:, k, :],
                     start=(k == 0), stop=(k == 15)), reads=['mTc', 'rwb'], writes=['PS0'])
            T.op('dve', lambda e: e.tensor_tensor(out=Lg[:], in0=PS[0][:, 0:32], in1=RBbc[:], op=ALU.add), reads=['PS0', 'RBbc'], writes=['Lg'])
            T.op('dve', lambda e: e.max(out=v8[:], in_=Lg[:]), reads=['Lg'], writes=['v8'])
            T.op('dve', lambda e: e.tensor_scalar(out=sm[:, 0:1], in0=v8[:, 0:1], scalar1=-1.0, scalar2=None, op0=ALU.mult), reads=['v8'], writes=['sm'])
            T.op('act', lambda e: e.activation(out=Eg[:], in_=Lg[:], func=AF.Exp, bias=sm[:, 0:1]), reads=['Lg', 'sm'], writes=['Eg'])
            T.op('dve', lambda e: e.tensor_scalar(out=Lg[:], in0=Lg[:], scalar1=v8[:, 3:4], scalar2=None, op0=ALU.is_ge), reads=['Lg', 'v8'], writes=['Lg'])
            T.op('dve', lambda e: e.tensor_tensor(out=Eg[:], in0=Eg[:], in1=Lg[:], op=ALU.mult), reads=['Eg', 'Lg'], writes=['Eg'])
            T.op('dve', lambda e: e.tensor_reduce(out=sm[:, 1:2], in_=Eg[:], axis=mybir.AxisListType.X, op=ALU.add), reads=['Eg'], writes=['sm'])
            T.op('dve', lambda e: e.reciprocal(out=sm[:, 1:2], in_=sm[:, 1:2]), reads=['sm'], writes=['sm'])
            T.op('dve', lambda e: e.tensor_scalar(out=Eg[:], in0=Eg[:], scalar1=sm[:, 1:2], scalar2=None, op0=ALU.mult), reads=['Eg', 'sm'], writes=['Eg'])
            T.op('pe', lambda e: e.transpose(out=PS[1][0:32, 0:128], in_=Eg[:, :], identity=identf[:]), reads=['Eg', 'identf'], writes=['PS1'])
            T.op('act', lambda e, sub=sub: e.activation(out=gT[:, sub * 128:(sub + 1) * 128], in_=PS[1][0:32, 0:128], func=AF.Copy), reads=['PS1'], writes=['gT'])
        if dbg and chk == 0:
            T.dma('sp', lambda e: e.dma_start(out=dbg_d['gT'], in_=gT[:]), reads=['gT'])
        for m in range(16):
            for h in range(2):
                p = PS[2 + h]; pk = f'PS{2 + h}'
                T.op('pe', lambda e, p=p, m=m, h=h: e.matmul(p[:, :], lhsT=BD[0:32, m * 128:(m + 1) * 128], rhs=gT[0:32, h * 512:(h + 1) * 512],
                     start=True, stop=True), reads=['BD', 'gT'], writes=[pk])
                T.op('act', lambda e, p=p, m=m, h=h: e.activation(out=acc[:, m, h * 512:(h + 1) * 512], in_=p[:, :], func=AF.Copy), reads=[pk], writes=['acc'])
        for ex in range(32):
            for h in range(2):
                p = PS[2 + h]; pk = f'PS{2 + h}'
                T.op('pe', lambda e, p=p, h=h, ex=ex: e.matmul(p[:, :], lhsT=bc_free(identf[0:32, ex:ex + 1], 128), rhs=gT[0:32, h * 512:(h + 1) * 512],
                     start=True, stop=True), reads=['identf', 'gT'], writes=[pk])
                T.op('act', lambda e, p=p, h=h: e.activation(out=gbc[:, h * 512:(h + 1) * 512], in_=p[:, :], func=AF.Copy), reads=[pk], writes=['gbc'])
            for step in range(8):
                j0 = 2 * step
                wg, wgk = load_piece(lambda half, ex=ex, j0=j0: wgu_d[ex, half * 1024:(half + 1) * 1024, j0 * 128:j0 * 128 + 256].rearrange("(k p) f -> p k f", p=128), ex * 24 + 2 * step, chk == 0)
                wl, wlk = load_piece(lambda half, ex=ex, j0=j0: wgu_d[ex, half * 1024:(half + 1) * 1024, 2048 + j0 * 128:2048 + j0 * 128 + 256].rearrange("(k p) f -> p k f", p=128), ex * 24 + 2 * step + 1, chk == 0)
                for jj in range(2):
                    j = j0 + jj
                    for h in range(2):
                        pg = PS[4 + h]; pgk = f'PS{4 + h}'; pl_ = PS[6 + h]; plk = f'PS{6 + h}'
                        for k in range(16):
                            T.op('pe', lambda e, pg=pg, wg=wg, k=k, jj=jj, h=h: e.matmul(pg[:, :], lhsT=wg[:, k, jj * 128:(jj + 1) * 128],
                                 rhs=mTc[:, k, h * 512:(h + 1) * 512], start=(k == 0), stop=(k == 15)), reads=[wgk, 'mTc'], writes=[pgk])
                        for k in range(16):
                            T.op('pe', lambda e, pl_=pl_, wl=wl, k=k, jj=jj, h=h: e.matmul(pl_[:, :], lhsT=wl[:, k, jj * 128:(jj + 1) * 128],
                                 rhs=mTc[:, k, h * 512:(h + 1) * 512], start=(k == 0), stop=(k == 15)), reads=[wlk, 'mTc'], writes=[plk])
                        t1, t2, t3 = mt
                        T.op('dve', lambda e, pg=pg, j=j, ex=ex: e.tensor_scalar(out=t1[:], in0=pg[:, :], scalar1=bguT[:, ex, j:j + 1], scalar2=7.0, op0=ALU.add, op1=ALU.min),
                             reads=[pgk, 'bguT'], writes=['mt0'])
                        T.op('act', lambda e: e.activation(out=t2[:], in_=t1[:], func=AF.Sigmoid, scale=1.702), reads=['mt0'], writes=['mt1'])
                        T.op('dve', lambda e, pl_=pl_, j=j, ex=ex: e.tensor_scalar(out=t3[:], in0=pl_[:, :], scalar1=bguT[:, ex, 16 + j:16 + j + 1], scalar2=7.0, op0=ALU.add, op1=ALU.min),
                             reads=[plk, 'bguT'], writes=['mt2'])
                        T.op('pool', lambda e: e.tensor_scalar(out=t3[:], in0=t3[:], scalar1=-7.0, scalar2=1.0, op0=ALU.max, op1=ALU.add), reads=['mt2'], writes=['mt2'])
                        T.op('pool', lambda e: e.tensor_tensor(out=t1[:], in0=t1[:], in1=t2[:], op=ALU.mult), reads=['mt0', 'mt1'], writes=['mt0'])
                        T.op('dve', lambda e: e.tensor_tensor(out=t1[:], in0=t1[:], in1=t3[:], op=ALU.mult), reads=['mt0', 'mt2'], writes=['mt0'])
                        T.op('pool', lambda e, j=j, h=h: e.tensor_tensor(out=actb[:, j, h * 512:(h + 1) * 512], in0=t1[:], in1=gbc[:, h * 512:(h + 1) * 512], op=ALU.mult),
                             reads=['mt0', 'gbc'], writes=['actb'])
            for step in range(8):
                m0 = 2 * step
                wd, wdk = load_piece(lambda half, ex=ex, m0=m0: wdn_d[ex, half * 1024:(half + 1) * 1024, m0 * 128:m0 * 128 + 256].rearrange("(k p) f -> p k f", p=128), ex * 24 + 16 + step, chk == 0)
                for mm in range(2):
                    m = m0 + mm
                    for h in range(2):
                        p = PS[2 + h]; pk = f'PS{2 + h}'
                        for k in range(16):
                            T.op('pe', lambda e, p=p, wd=wd, k=k, mm=mm, h=h: e.matmul(p[:, :], lhsT=wd[:, k, mm * 128:(mm + 1) * 128],
                                 rhs=actb[:, k, h * 512:(h + 1) * 512], start=(k == 0), stop=(k == 15)), reads=[wdk, 'actb'], writes=[pk])
                        T.op('dve', lambda e, p=p, m=m, h=h: e.tensor_tensor(out=acc[:, m, h * 512:(h + 1) * 512], in0=acc[:, m, h * 512:(h + 1) * 512], in1=p[:, :], op=ALU.add),
                             reads=[pk, 'acc'], writes=['acc'])
        T.dma('sp', lambda e: e.dma_start(out=G2bc, in_=grow_d[1:2, :].partition_broadcast(128)), reads=['grow_d'], writes=['actb'])
        T.dma('sp', lambda e: e.dma_start(out=finbc, in_=fing_d.partition_broadcast(128)), writes=['wst0'])
        for sub in range(TC // 128):
            r0 = t0 + sub * 128
            hxt = hx_t[sub % 2]; hk = 'actb'
            T.dma('sp', lambda e, hxt=hxt, r0=r0: e.dma_start(out=hxt, in_=hx1_d[r0:r0 + 128, :]), reads=['hx1_d'], writes=[hk])
            for q in range(4):
                p = PS[4 + q]; pk = f'PS{4 + q}'
                for jj in range(4):
                    m = q * 4 + jj
                    T.op('pe', lambda e, p=p, jj=jj, m=m, sub=sub: e.transpose(out=p[:, jj * 128:(jj + 1) * 128], in_=acc[:, m, sub * 128:(sub + 1) * 128], identity=identf[:]),
                         reads=['acc', 'identf'], writes=[pk])
                T.op('dve', lambda e, p=p, q=q: e.tensor_tensor(out=mo_t[:, q * 512:(q + 1) * 512], in0=p[:, :], in1=G2bc[:, q * 512:(q + 1) * 512], op=ALU.mult),
                     reads=[pk, 'actb'], writes=['actb'])
            T.op('pool', lambda e, hxt=hxt: e.tensor_tensor(out=hxt, in0=hxt, in1=mo_t, op=ALU.add), reads=['actb'], writes=['actb'])
            T.op('act', lambda e, hxt=hxt: e.activation(out=mo_t, in_=hxt, func=AF.Square, accum_out=sm[:, 0:1]), reads=[hk], writes=['actb', 'sm'])
            T.op('act', lambda e: e.activation(out=sm[:, 0:1], in_=sm[:, 0:1], func=AF.Sqrt, scale=1.0 / D, bias=epsT[:, 0:1]), reads=['sm', 'epsT'], writes=['sm'])
            T.op('dve', lambda e: e.reciprocal(out=sm[:, 0:1], in_=sm[:, 0:1]), reads=['sm'], writes=['sm'])
            T.op('dve', lambda e, hxt=hxt: e.scalar_tensor_tensor(out=hxt, in0=hxt, scalar=sm[:, 0:1], in1=finbc, op0=ALU.mult, op1=ALU.mult),
                 reads=[hk, 'sm', 'wst0'], writes=[hk])
            T.dma('sp', lambda e, hxt=hxt, r0=r0: e.dma_start(out=out_d[r0:r0 + 128, :], in_=hxt), reads=[hk], writes=['out_d'])
    T.pop()
    T.finish()
    return nc


def hy_consts():
    N = 16384
    bf = ml_dtypes.bfloat16
    n1 = np.arange(64)[:, None]; k1 = np.arange(65)[None, :]
    f1 = np.concatenate([np.cos(2 * np.pi * n1 * k1 / 128), -np.sin(2 * np.pi * n1 * k1 / 128)], 1)
    n2 = np.arange(128)[:, None]
    twf = np.concatenate([np.cos(2 * np.pi * n2 * k1 / N), -np.sin(2 * np.pi * n2 * k1 / N)], 1)
    a = np.arange(128)[:, None]; b = np.arange(128)[None, :]
    C = np.cos(2 * np.pi * a * b / 128); Sn = np.sin(2 * np.pi * a * b / 128)
    csn = np.stack([C, Sn, -Sn], 1)
    csh = np.zeros((128, 2, 2, 128))
    for h in range(2):
        csh[:, 0, h, 0:64] = C[:, h * 64:(h + 1) * 64]; csh[:, 0, h, 64:128] = Sn[:, h * 64:(h + 1) * 64]
        csh[:, 1, h, 0:64] = -Sn[:, h * 64:(h + 1) * 64]; csh[:, 1, h, 64:128] = C[:, h * 64:(h + 1) * 64]
    kk = np.arange(65)[:, None]; nn = np.arange(128)[None, :]
    twi = np.stack([np.cos(2 * np.pi * nn * kk / N), np.sin(2 * np.pi * nn * kk / N)], 1)
    w = np.full((65, 1), 2.0); w[0] = 1.0; w[64] = 1.0
    m1 = np.arange(64)[None, :]
    eri = np.stack([w * np.cos(2 * np.pi * m1 * kk / 128) / N, -w * np.sin(2 * np.pi * m1 * kk / 128) / N], 1)
    L = S
    t = np.linspace(0.0, 1.0, L, dtype=np.float32)[:, None]
    wv = (2.0 * np.pi * np.arange(L, dtype=np.float32)[:, None] / L).astype(np.float32)
    f = np.linspace(1e-4, 15, 16, dtype=np.float32)[None, :]
    z = np.concatenate([t, np.cos(f * wv), -np.sin(f * wv)], -1).astype(np.float32)
    import math
    max_decay = math.log(1e-2) / 0.3; min_decay = math.log(1e-2) / 1.5
    deltas = np.abs(np.linspace(min_decay, max_decay, 1024, dtype=np.float32))
    sidx = np.arange(L)
    perm = 128 * (sidx % 64) + sidx // 64
    dec = np.exp(-t * deltas[None, :])[perm]
    z = z[perm]
    return {
        "c_zT": np.ascontiguousarray(z.T), "c_dec": np.ascontiguousarray(dec.T).reshape(8, 128, L).astype(bf),
        "c_identb": np.eye(128).astype(bf), "c_f1": f1.astype(bf), "c_twf": twf.astype(np.float32),
        "c_csn": csn.astype(bf), "c_csh": csh.astype(bf), "c_twi": twi.astype(np.float32), "c_eri": eri.astype(bf),
    }


def make_in_map(b, inp, h=0):
    g = lambda k: np.asarray(inp[k], np.float32)
    cvec = np.stack([g('c')[b], g('c_ctx')], 0)
    m = {
        "x": np.ascontiguousarray(g('x')[b]),
        "ctx": np.ascontiguousarray(g('ctx')[b]),
        "cT": np.ascontiguousarray(cvec.reshape(2, 16, 128).transpose(2, 1, 0)),
        "w_mod": np.ascontiguousarray(g('w_mod')[0]),
        "bmodT": fm(g('b_mod')[0], 96),
        "n1gT": fm(g('norm1_g')[0], 16),
        "n2gT": fm(g('norm2_g')[0], 16),
        "final_g": np.ascontiguousarray(g('final_g').reshape(1, D)),
        "w_in": np.ascontiguousarray(g('w_in')[0]),
        "lcwT": np.ascontiguousarray(g('lru_conv_w')[0].reshape(4, 8, 128).transpose(2, 1, 0)),
        "lcbT": fm(g('lru_conv_b')[0], 8),
        "lru_wa": np.ascontiguousarray(g('lru_wa')[0]),
        "lru_wi": np.ascontiguousarray(g('lru_wi')[0]),
        "lbaT": np.ascontiguousarray(g('lru_ba')[0].reshape(2, 8, 128).transpose(2, 0, 1)),
        "lbiT": np.ascontiguousarray(g('lru_bi')[0].reshape(2, 8, 128).transpose(2, 0, 1)),
        "llamT": np.ascontiguousarray(g('lru_lambda')[0].reshape(2, 8, 128).transpose(2, 0, 1)),
        "identf": np.eye(128, dtype=np.float32),
        "xh": np.ascontiguousarray(g('x')[b][h * SL:(h + 1) * SL]),
        "offs": (h * SL + 512 * np.arange(8, dtype=np.int32)).reshape(1, 8).astype(np.int32),
        "hcwT": np.ascontiguousarray(g('hy_conv_w')[0].reshape(3, 24, 128).transpose(2, 1, 0)),
        "hcbT": fm(g('hy_conv_b')[0], 24),
        "hy_w1": np.ascontiguousarray(g('hy_w1')[0]), "hy_w2": np.ascontiguousarray(g('hy_w2')[0]), "hy_w3": np.ascontiguousarray(g('hy_w3')[0]),
        "hy_bT": np.ascontiguousarray(np.stack([g('hy_b1')[0], g('hy_b2')[0], g('hy_b3')[0], g('hy_freq')[0]], 1)),
        "hy_w4": np.ascontiguousarray(g('hy_w4')[0]), "hy_d": np.ascontiguousarray(g('hy_d')[0]),
        "w_out": np.ascontiguousarray(g('w_out')[0]),
        "w_gu": np.ascontiguousarray(g('exp_w_gu')[0]), "w_dn": np.ascontiguousarray(g('exp_w_down')[0]),
        "bguT": np.ascontiguousarray(g('exp_b_gu')[0].reshape(32, 32, 128).transpose(2, 0, 1)),
        "b_dn": np.ascontiguousarray(g('exp_b_down')[0]),
        "router_w": np.ascontiguousarray(g('router_w')[0]), "router_b": np.ascontiguousarray(g('router_b')[0].reshape(1, 32)),
        "gnT": np.ascontiguousarray(np.concatenate([fm(g('gn_lru')[0], 8), fm(g('gn_hy')[0], 8)], 1)),
    }
    m.update(hy_consts())
    return m


def kernel(**inputs):
    nc = build(dbg=False)
    shared = {}
    in_maps = []
    for c in range(NCORES):
        b, h = c // 2, c % 2
        if h == 0:
            shared = make_in_map(b, inputs, 0)
            in_maps.append(shared)
        else:
            m = dict(shared)
            m["xh"] = np.ascontiguousarray(np.asarray(inputs['x'], np.float32)[b][SL:2 * SL])
            m["offs"] = (SL + 512 * np.arange(8, dtype=np.int32)).reshape(1, 8).astype(np.int32)
            in_maps.append(m)
    res = run_bass_kernel_spmd(nc, in_maps, core_ids=list(range(NCORES)))
    out = np.empty((4, S, D), np.float32)
    for c in range(NCORES):
        out[c // 2, (c % 2) * SL:(c % 2 + 1) * SL] = np.asarray(res.results[c]["out"], np.float32)
    return out
```
